# Optimizing a Trainium2 kernel written in Bass

```python
import math
import jax, jax.numpy as jnp
from jax import lax
import numpy as np

D_MODEL = 1024
BATCH = 8
SEQ = 4096
DEPTH = 2

D_MIX = D_MODEL
SSD_INNER = D_MODEL // 2
SSD_HEAD_DIM = 64
SSD_HEADS = SSD_INNER // SSD_HEAD_DIM
SSD_GROUPS = 2
SSD_HEADS_PER_GROUP = SSD_HEADS // SSD_GROUPS
SSD_STATE = 64
SSD_CONV = 4
SSD_CHUNK = 128
SSD_CONV_DIM = SSD_INNER + 2 * SSD_GROUPS * SSD_STATE
DT_MIN = 0.001
DT_MAX = 0.1
MLA_HEADS = 4
MLA_Q_RANK = D_MODEL // 4
MLA_KV_RANK = D_MODEL // 8
MLA_NOPE = 64
MLA_ROPE = 32
MLA_V = 64
MLA_OUT = MLA_HEADS * MLA_V
MLA_SCALE = (MLA_NOPE + MLA_ROPE) ** -0.5
MLA_Q_BLOCK = 128
ROPE_THETA = 10000.0
GM_GROUPS = 4
GM_GROUP_DIM = 64
GM_WIDTH = GM_GROUPS * GM_GROUP_DIM
GM_CHUNK = 128
IN_SIZES = (SSD_INNER, SSD_INNER, SSD_GROUPS * SSD_STATE, SSD_GROUPS * SSD_STATE, SSD_HEADS,
            MLA_Q_RANK, MLA_KV_RANK, MLA_ROPE, GM_WIDTH, GM_WIDTH)
D_IN = sum(IN_SIZES)
D_FF = 2816
N_EXPERTS = 8
TOP_K = 2
DN_ALPHA = (2 * DEPTH) ** 0.25
DN_BETA = (8 * DEPTH) ** -0.25
LN_EPS = 1e-5
RMS_EPS = 1e-6
ADA_SCALE = 0.3

kernel_name = "hymba_ssd_mla_gmlp_moe_deepnorm_adaln"


def layer_norm(x, g, b):
    xf = x.astype(jnp.float32)
    mu = jnp.mean(xf, axis=-1, keepdims=True)
    var = jnp.mean(jnp.square(xf - mu), axis=-1, keepdims=True)
    return ((xf - mu) * lax.rsqrt(var + LN_EPS)).astype(x.dtype) * g + b


def rms_norm(x, g):
    xf = x.astype(jnp.float32)
    return (xf * lax.rsqrt(jnp.mean(jnp.square(xf), axis=-1, keepdims=True) + RMS_EPS)).astype(x.dtype) * g


def modulation(c, w, b):
    m = jnp.einsum("bd,de->be", jax.nn.silu(c), w) + b
    shift, scale, gate = jnp.split(m, 3, axis=-1)
    return shift[:, None], scale[:, None], gate[:, None]


def causal_depthwise_conv(x, w, b):
    k, ch = w.shape
    y = lax.conv_general_dilated(x, w[:, None, :], window_strides=(1,), padding=[(k - 1, 0)],
                                 dimension_numbers=("NWC", "WIO", "NWC"), feature_group_count=ch)
    return y + b


def segsum(a):
    n = a.shape[-1]
    cs = jnp.cumsum(a, axis=-1)
    diff = cs[..., :, None] - cs[..., None, :]
    return jnp.where(jnp.tril(jnp.ones((n, n), dtype=bool)), diff, -jnp.inf)


def ssd_scan(xs, bm, cm, dt, a_log, d_skip):
    b, t, _ = xs.shape
    nc, l = t // SSD_CHUNK, SSD_CHUNK
    g, r, p, n = SSD_GROUPS, SSD_HEADS_PER_GROUP, SSD_HEAD_DIM, SSD_STATE
    x = xs.reshape(b, nc, l, g, r, p)
    bc = bm.reshape(b, nc, l, g, n).astype(jnp.float32)
    cc = cm.reshape(b, nc, l, g, n).astype(jnp.float32)
    dtc = dt.reshape(b, nc, l, g, r)
    a_head = -jnp.exp(a_log.astype(jnp.float32)).reshape(g, r)
    a = jnp.transpose(dtc * a_head, (0, 3, 4, 1, 2))
    a_cs = jnp.cumsum(a, axis=-1)
    xdt = x.astype(jnp.float32) * dtc[..., None]
    decay_in = jnp.exp(segsum(a))
    cb = jnp.einsum("bclgn,bcsgn->bcgls", cc, bc)
    y_diag = jnp.einsum("bcgls,bgrcls,bcsgrp->bclgrp", cb, decay_in, xdt)
    decay_states = jnp.exp(a_cs[..., -1:] - a_cs)
    states = jnp.einsum("bcsgn,bgrcs,bcsgrp->bcgrpn", bc, decay_states, xdt)
    states = jnp.concatenate([jnp.zeros_like(states[:, :1]), states], axis=1)
    chunk_a = jnp.pad(a_cs[..., -1], ((0, 0), (0, 0), (0, 0), (1, 0)))
    decay_chunk = jnp.exp(segsum(chunk_a))
    states = jnp.einsum("bgrzc,bcgrpn->bzgrpn", decay_chunk, states)[:, :-1]
    y_off = jnp.einsum("bclgn,bcgrpn,bgrcl->bclgrp", cc, states, jnp.exp(a_cs))
    y = y_diag + y_off + x.astype(jnp.float32) * d_skip.astype(jnp.float32).reshape(g, r)[:, :, None]
    return y.reshape(b, t, SSD_INNER).astype(xs.dtype)


def gated_group_rmsnorm(y, z, w):
    gy = y * jax.nn.silu(z)
    b, t, _ = gy.shape
    gf = gy.reshape(b, t, SSD_GROUPS, SSD_INNER // SSD_GROUPS).astype(jnp.float32)
    gf = gf * lax.rsqrt(jnp.mean(jnp.square(gf), axis=-1, keepdims=True) + RMS_EPS)
    return gf.reshape(b, t, SSD_INNER).astype(y.dtype) * w


def rope_tables(t):
    inv = ROPE_THETA ** (-jnp.arange(0, MLA_ROPE, 2, dtype=jnp.float32) / MLA_ROPE)
    ang = jnp.arange(t, dtype=jnp.float32)[:, None] * inv[None, :]
    return jnp.cos(ang), jnp.sin(ang)


def apply_rope(x, cos, sin):
    x1, x2 = jnp.split(x.astype(jnp.float32), 2, axis=-1)
    return jnp.concatenate([x1 * cos - x2 * sin, x1 * sin + x2 * cos], axis=-1).astype(x.dtype)


def mla_attention(q_nope, q_pe, k_nope, k_pe, v):
    b, t, h, _ = q_nope.shape
    nb = t // MLA_Q_BLOCK
    qn = jnp.moveaxis(q_nope.reshape(b, nb, MLA_Q_BLOCK, h, MLA_NOPE), 1, 0)
    qp = jnp.moveaxis(q_pe.reshape(b, nb, MLA_Q_BLOCK, h, MLA_ROPE), 1, 0)
    kpos = jnp.arange(t)

    def block(args):
        qn_b, qp_b, i = args
        s = (jnp.einsum("bqhd,bkhd->bhqk", qn_b, k_nope)
             + jnp.einsum("bqhd,bkd->bhqk", qp_b, k_pe)).astype(jnp.float32) * MLA_SCALE
        qpos = i * MLA_Q_BLOCK + jnp.arange(MLA_Q_BLOCK)
        s = jnp.where(kpos[None, :] <= qpos[:, None], s, -jnp.inf)
        pr = jax.nn.softmax(s, axis=-1).astype(v.dtype)
        return jnp.einsum("bhqk,bkhd->bqhd", pr, v)

    out = lax.map(block, (qn, qp, jnp.arange(nb)))
    return jnp.moveaxis(out, 0, 1).reshape(b, t, h * MLA_V)


def spatial_gating(gu, gv, ln_g, ln_b, w_s, b_s):
    gu = jax.nn.gelu(gu, approximate=False)
    gv = layer_norm(jax.nn.gelu(gv, approximate=False), ln_g, ln_b)
    b, t, _ = gv.shape
    vc = gv.reshape(b, t // GM_CHUNK, GM_CHUNK, GM_GROUPS, GM_GROUP_DIM)
    w = w_s * jnp.tril(jnp.ones((GM_CHUNK, GM_CHUNK), dtype=w_s.dtype))
    s = jnp.einsum("gts,bcsgd->bctgd", w, vc) + jnp.transpose(b_s)[:, :, None]
    return gu * s.reshape(b, t, GM_WIDTH)


def hybrid_mixer(h, cos, sin, w_in, conv_w, conv_b, dt_bias, a_log, d_skip, ssd_norm_w,
                 q_norm, w_qb, kv_norm, w_kvb, gm_ln_g, gm_ln_b, gm_w_s, gm_b_s, w_out):
    b, t, _ = h.shape
    proj = jnp.einsum("btd,de->bte", h, w_in)
    split_at = np.cumsum(IN_SIZES)[:-1].tolist()
    z, xs, bm, cm, dt, q_lat, kv_lat, k_pe, gu, gv = jnp.split(proj, split_at, axis=-1)
    xbc = jax.nn.silu(causal_depthwise_conv(jnp.concatenate([xs, bm, cm], axis=-1), conv_w, conv_b))
    xs, bm, cm = jnp.split(xbc, [SSD_INNER, SSD_INNER + SSD_GROUPS * SSD_STATE], axis=-1)
    dt = jax.nn.softplus(dt.astype(jnp.float32) + dt_bias.astype(jnp.float32))
    y_ssd = gated_group_rmsnorm(ssd_scan(xs, bm, cm, dt, a_log, d_skip), z, ssd_norm_w)
    q = jnp.einsum("btr,re->bte", rms_norm(q_lat, q_norm), w_qb).reshape(b, t, MLA_HEADS, MLA_NOPE + MLA_ROPE)
    q_nope, q_pe = jnp.split(q, [MLA_NOPE], axis=-1)
    q_pe = apply_rope(q_pe, cos[:, None, :], sin[:, None, :])
    kv = jnp.einsum("btr,re->bte", rms_norm(kv_lat, kv_norm), w_kvb).reshape(b, t, MLA_HEADS, MLA_NOPE + MLA_V)
    k_nope, v_heads = jnp.split(kv, [MLA_NOPE], axis=-1)
    k_pe = apply_rope(k_pe, cos, sin)
    y_mla = mla_attention(q_nope, q_pe, k_nope, k_pe, v_heads)
    y_gm = spatial_gating(gu, gv, gm_ln_g, gm_ln_b, gm_w_s, gm_b_s)
    y = jnp.concatenate([y_ssd, y_mla.astype(y_ssd.dtype), y_gm.astype(y_ssd.dtype)], axis=-1)
    return jnp.einsum("bte,ed->btd", y, w_out)


def swiglu(h, w1, w3, w2):
    a = jnp.einsum("btd,df->btf", h, w1)
    g = jnp.einsum("btd,df->btf", h, w3)
    return jnp.einsum("btf,fd->btd", jax.nn.silu(a) * g, w2)


def moe_swiglu(h, w_router, w1, w3, w2):
    logits = jnp.einsum("btd,de->bte", h, w_router).astype(jnp.float32)
    top_v, top_i = lax.top_k(logits, TOP_K)
    gates = jax.nn.softmax(top_v, axis=-1)
    combine = jnp.sum(jax.nn.one_hot(top_i, N_EXPERTS, dtype=jnp.float32) * gates[..., None], axis=-2)
    combine = combine.astype(h.dtype)
    out = jnp.zeros_like(h)
    for e in range(N_EXPERTS):
        out = out + combine[..., e:e + 1] * swiglu(h, w1[e], w3[e], w2[e])
    return out


def setup_inputs(seed: int = 0) -> dict:
    key = jax.random.key(seed)
    ks = iter(jax.random.split(key, 40))
    f32 = jnp.float32

    def nrm(shape, scale):
        return jax.random.normal(next(ks), shape, f32) * scale

    def gain(shape):
        return 1.0 + nrm(shape, 0.02)

    def bias(shape):
        return nrm(shape, 0.02)

    n_dense = (DEPTH + 1) // 2
    n_moe = DEPTH // 2
    u = jax.random.uniform(next(ks), (DEPTH, SSD_HEADS), f32)
    dt0 = jnp.exp(u * (math.log(DT_MAX) - math.log(DT_MIN)) + math.log(DT_MIN))
    dt_bias = dt0 + jnp.log(-jnp.expm1(-dt0))
    a_log = jnp.log(jax.random.uniform(next(ks), (DEPTH, SSD_HEADS), f32, 1.0, 16.0))
    return {
        "x": nrm((BATCH, SEQ, D_MODEL), 1.0),
        "c": nrm((BATCH, D_MODEL), 1.0),
        "ln0_g": gain((D_MODEL,)),
        "ln0_b": bias((D_MODEL,)),
        "ada_w": nrm((DEPTH, 2, D_MODEL, 3 * D_MODEL), ADA_SCALE * D_MODEL ** -0.5),
        "ada_b": bias((DEPTH, 2, 3 * D_MODEL)),
        "post_ln_g": gain((DEPTH, 2, D_MODEL)),
        "post_ln_b": bias((DEPTH, 2, D_MODEL)),
        "w_in": nrm((DEPTH, D_MODEL, D_IN), D_MODEL ** -0.5),
        "ssd_conv_w": nrm((DEPTH, SSD_CONV, SSD_CONV_DIM), SSD_CONV ** -0.5),
        "ssd_conv_b": bias((DEPTH, SSD_CONV_DIM)),
        "ssd_dt_bias": dt_bias,
        "ssd_a_log": a_log,
        "ssd_d": gain((DEPTH, SSD_HEADS)),
        "ssd_norm_w": gain((DEPTH, SSD_INNER)),
        "mla_q_norm": gain((DEPTH, MLA_Q_RANK)),
        "mla_w_qb": nrm((DEPTH, MLA_Q_RANK, MLA_HEADS * (MLA_NOPE + MLA_ROPE)), MLA_Q_RANK ** -0.5),
        "mla_kv_norm": gain((DEPTH, MLA_KV_RANK)),
        "mla_w_kvb": nrm((DEPTH, MLA_KV_RANK, MLA_HEADS * (MLA_NOPE + MLA_V)), MLA_KV_RANK ** -0.5),
        "gm_ln_g": gain((DEPTH, GM_WIDTH)),
        "gm_ln_b": bias((DEPTH, GM_WIDTH)),
        "gm_w_s": nrm((DEPTH, GM_GROUPS, GM_CHUNK, GM_CHUNK), GM_CHUNK ** -0.5),
        "gm_b_s": 1.0 + nrm((DEPTH, GM_GROUPS, GM_CHUNK), 0.1),
        "w_out": nrm((DEPTH, D_MIX, D_MODEL), DN_BETA * D_MIX ** -0.5),
        "ffn_w1": nrm((n_dense, D_MODEL, D_FF), D_MODEL ** -0.5),
        "ffn_w3": nrm((n_dense, D_MODEL, D_FF), D_MODEL ** -0.5),
        "ffn_w2": nrm((n_dense, D_FF, D_MODEL), DN_BETA * D_FF ** -0.5),
        "moe_router": nrm((n_moe, D_MODEL, N_EXPERTS), D_MODEL ** -0.5),
        "moe_w1": nrm((n_moe, N_EXPERTS, D_MODEL, D_FF), D_MODEL ** -0.5),
        "moe_w3": nrm((n_moe, N_EXPERTS, D_MODEL, D_FF), D_MODEL ** -0.5),
        "moe_w2": nrm((n_moe, N_EXPERTS, D_FF, D_MODEL), DN_BETA * D_FF ** -0.5),
    }


def reference(x, c, ln0_g, ln0_b, ada_w, ada_b, post_ln_g, post_ln_b, w_in, ssd_conv_w, ssd_conv_b,
              ssd_dt_bias, ssd_a_log, ssd_d, ssd_norm_w, mla_q_norm, mla_w_qb, mla_kv_norm, mla_w_kvb,
              gm_ln_g, gm_ln_b, gm_w_s, gm_b_s, w_out, ffn_w1, ffn_w3, ffn_w2,
              moe_router, moe_w1, moe_w3, moe_w2):
    cos, sin = rope_tables(x.shape[1])
    x = layer_norm(x, ln0_g, ln0_b)
    for layer in range(DEPTH):
        shift, scale, gate = modulation(c, ada_w[layer, 0], ada_b[layer, 0])
        h = x * (1.0 + scale) + shift
        y = hybrid_mixer(h, cos, sin, w_in[layer], ssd_conv_w[layer], ssd_conv_b[layer],
                         ssd_dt_bias[layer], ssd_a_log[layer], ssd_d[layer], ssd_norm_w[layer],
                         mla_q_norm[layer], mla_w_qb[layer], mla_kv_norm[layer], mla_w_kvb[layer],
                         gm_ln_g[layer], gm_ln_b[layer], gm_w_s[layer], gm_b_s[layer], w_out[layer])
        x = layer_norm(DN_ALPHA * x + (1.0 + gate) * y, post_ln_g[layer, 0], post_ln_b[layer, 0])
        shift, scale, gate = modulation(c, ada_w[layer, 1], ada_b[layer, 1])
        h = x * (1.0 + scale) + shift
        i = layer // 2
        if layer % 2 == 0:
            y = swiglu(h, ffn_w1[i], ffn_w3[i], ffn_w2[i])
        else:
            y = moe_swiglu(h, moe_router[i], moe_w1[i], moe_w3[i], moe_w2[i])
        x = layer_norm(DN_ALPHA * x + (1.0 + gate) * y, post_ln_g[layer, 1], post_ln_b[layer, 1])
    return x
```

```python
import math
from contextlib import ExitStack

import numpy as np
import concourse.bass as bass
import concourse.mybir as mybir
from concourse.bass_utils import run_bass_kernel_spmd

F32 = mybir.dt.float32
BF16 = mybir.dt.bfloat16
I32 = mybir.dt.int32
AF = mybir.ActivationFunctionType
ALU = mybir.AluOpType
AX = mybir.AxisListType

T = 4096
D = 1024
DFF = 2816
NE = 8
NCH = DFF // 128
DEPTH = 2
DN_ALPHA = (2 * DEPTH) ** 0.25
LN_EPS = 1e-5
RMS_EPS = 1e-6
D_IN = 2216


class Sched:
    NDSEM = 6

    def __init__(self, nc):
        self.nc = nc
        self.engs = {"pe": nc.tensor, "act": nc.scalar, "dve": nc.vector, "pool": nc.gpsimd, "sp": nc.sync}
        self.sem = {k: nc.alloc_semaphore("prog_" + k) for k in ("pe", "act", "dve", "pool")}
        self.cnt = {k: 0 for k in self.sem}
        self.seen = {k: {} for k in self.engs}
        self.lastw = {}
        self.rd = {}
        self.dsem = {}
        self.dcnt = {}
        self.dnext = {}
        for q in ("sp", "act", "pool"):
            for i in range(self.NDSEM):
                key = ("d", q, i)
                self.dsem[key] = nc.alloc_semaphore(f"dma_{q}{i}")
                self.dcnt[key] = 0
            self.dnext[q] = 0
        self.n_inst = 0
        self.pe_open = False
        self.hold = False

    def can_yield(self):
        return not (self.pe_open or self.hold)

    def _semh(self, key):
        return self.sem[key] if isinstance(key, str) else self.dsem[key]

    def wait(self, eng, ev):
        key, val = ev
        if val <= 0:
            return
        if key == eng and eng in ("pe",):
            return
        if self.seen[eng].get(key, 0) >= val:
            return
        self.engs[eng].wait_ge(self._semh(key), val)
        self.seen[eng][key] = val
        self.n_inst += 1

    def _deps(self, reads, writes):
        evs = []
        for r in reads:
            if r in self.lastw:
                evs.append(self.lastw[r])
        for w in writes:
            if w in self.lastw:
                evs.append(self.lastw[w])
            for k, v in self.rd.get(w, {}).items():
                evs.append((k, v))
        return evs

    def _record(self, ev, reads, writes):
        for r in reads:
            d = self.rd.setdefault(r, {})
            d[ev[0]] = max(d.get(ev[0], 0), ev[1])
        for w in writes:
            self.lastw[w] = ev
            self.rd[w] = {}

    PSUM_TOK = frozenset([f"b{i}" for i in range(8)] + ["bankb"])

    def op(self, eng, fn, reads=(), writes=(), inc=True):
        pr = [r for r in reads if r in self.PSUM_TOK]
        if pr:
            reads = [r for r in reads if r not in self.PSUM_TOK]
            writes = list(writes) + pr
        for ev in self._deps(reads, writes):
            self.wait(eng, ev)
        ins = fn()
        self.n_inst += 1
        if eng == "pe":
            self.pe_open = not inc
        if inc:
            ins.then_inc(self.sem[eng], 1)
            self.cnt[eng] += 1
            ev = (eng, self.cnt[eng])
        else:
            ev = (eng, self.cnt[eng] + 1)
        self._record(ev, reads, writes)
        return ins

    def dma(self, q, out, in_, reads=(), writes=()):
        i = self.dnext[q] % self.NDSEM
        self.dnext[q] += 1
        key = ("d", q, i)
        if self.dcnt[key] > 0:
            self.wait(q, (key, self.dcnt[key]))
        for ev in self._deps(reads, writes):
            self.wait(q, ev)
        ins = self.engs[q].dma_start(out=out, in_=in_)
        ins.then_inc(self.dsem[key], 16)
        self.n_inst += 1
        self.dcnt[key] += 16
        ev = (key, self.dcnt[key])
        self._record(ev, reads, writes)
        return ins

    def finish(self):
        for key, v in self.dcnt.items():
            if v > 0:
                self.wait("sp", (key, v))
        for k, v in self.cnt.items():
            self.wait("sp", (k, v))


class Ctx:
    def __init__(self, nc):
        self.nc = nc
        self.S = Sched(nc)
        self.stack = ExitStack()
        self.bank = [nc.alloc_psum_tensor(f"bank{i}", [128, 512], F32) for i in range(7)]
        self.bankb = nc.alloc_psum_tensor("bankb", [128, 1024], BF16)

    def chk(self, n):
        if getattr(self, "sstop", 99) <= n:
            raise StopIteration

    def sb(self, name, shape, dtype):
        return self.nc.alloc_sbuf_tensor("sb_" + name, list(shape), dtype)


def setup_consts(C, ident_f_d, cpack_d=None):
    nc, S = C.nc, C.S
    C.identf = C.sb("identf", [128, 128], F32)
    C.identb = C.sb("identb", [128, 128], BF16)
    S.dma("sp", C.identf[:], ident_f_d, writes=["identf"])
    if cpack_d is not None:
        C.cpack = C.sb("cpack", [128, 384], F32)
        S.dma("sp", C.cpack[:], cpack_d, writes=["cpack"])
        C.triu = C.cpack[:, 0:128]
        C.mneg = C.cpack[:, 128:256]
        C.ones = C.cpack[:, 256:384]
        C.triub = C.sb("triub", [128, 128], BF16)
        S.op("dve", lambda: nc.vector.tensor_copy(C.triub[:], C.triu), reads=["cpack"], writes=["triub"])
    C.eps_ln = C.sb("eps_ln", [128, 1], F32)
    C.eps_rms = C.sb("eps_rms", [128, 1], F32)
    S.op("pool", lambda: nc.gpsimd.memset(C.eps_ln[:], LN_EPS), writes=["eps_ln"])
    S.op("pool", lambda: nc.gpsimd.memset(C.eps_rms[:], RMS_EPS), writes=["eps_rms"])
    S.op("dve", lambda: nc.vector.tensor_copy(C.identb[:], C.identf[:]), reads=["identf"], writes=["identb"])


def ln_stats(C, u_ap, tok_u, mv, tok_mv, rstd, tok_rstd, st):
    nc, S = C.nc, C.S
    S.op("dve", lambda: nc.vector.bn_stats(st[:, 0, :], u_ap[:, 0:512]), reads=[tok_u], writes=["st0"])
    S.op("dve", lambda: nc.vector.bn_stats(st[:, 1, :], u_ap[:, 512:1024]), reads=[tok_u], writes=["st1"])
    S.op("dve", lambda: nc.vector.bn_aggr(mv, st[:].rearrange("p a b -> p (a b)")), reads=["st0", "st1"], writes=[tok_mv])
    S.op("act", lambda: nc.scalar.activation(out=rstd, in_=mv[:, 1:2], func=AF.Ln, bias=C.eps_ln[:, 0:1]),
         reads=[tok_mv, "eps_ln"], writes=[tok_rstd])
    S.op("act", lambda: nc.scalar.activation(out=rstd, in_=rstd, func=AF.Exp, scale=-0.5),
         reads=[tok_rstd], writes=[tok_rstd])


def barrier(C):
    S = C.S
    evs = [(k, v) for k, v in S.cnt.items()] + [(k, v) for k, v in S.dcnt.items()]
    for eng in ("pe", "act", "dve", "pool", "sp"):
        for ev in evs:
            S.wait(eng, ev)


def load_bcast(C, tile, dram_vec, tok):
    C.S.dma("sp", tile[:], dram_vec.partition_broadcast(128), writes=[tok])


def ln_front(C, ph, u_d, r0, vec, want_f32=False):
    nc, S = C.nc, C.S
    i = ph["i"] = ph.get("i", 0) + 1
    b = 0 if ph.get("single") else i % 2
    ut, xn, hb, hf = ph["ut"][b], ph["xn"][b], ph["hb"][b], ph["hf"]
    mv, rstd, st = ph["mv"][b], ph["rstd"][b], ph["st"]
    tx = f"ut{b}" if ph["xn"] is ph["ut"] else f"xn{b}"
    ph["tx"] = tx
    S.dma("sp", ut[:], u_d[r0:r0 + 128, :], writes=[f"ut{b}"])
    ln_stats(C, ut[:], f"ut{b}", mv[:], f"mv{b}", rstd[:], f"rstd{b}", st)
    S.op("dve", lambda: nc.vector.tensor_scalar(xn[:], ut[:], mv[:, 0:1], rstd[:, 0:1], ALU.subtract, ALU.mult),
         reads=[f"ut{b}", f"mv{b}", f"rstd{b}"], writes=[tx])
    if hf is None:
        S.op("pool", lambda: nc.gpsimd.tensor_tensor(hb[:], xn[:], vec["P1"][:], ALU.mult), reads=[tx, "P1"], writes=[f"hb{b}"])
        S.op("pool", lambda: nc.gpsimd.tensor_tensor(hb[:], hb[:], vec["P2"][:], ALU.add), reads=[f"hb{b}", "P2"], writes=[f"hb{b}"])
        return b
    S.op("pool", lambda: nc.gpsimd.tensor_tensor(hf[:], xn[:], vec["P1"][:], ALU.mult), reads=[tx, "P1"], writes=["hf"])
    if want_f32:
        S.op("dve", lambda: nc.vector.tensor_tensor(hf[:], hf[:], vec["P2"][:], ALU.add), reads=["hf", "P2"], writes=["hf"])
        S.op("act", lambda: nc.scalar.copy(hb[:], hf[:]), reads=["hf"], writes=[f"hb{b}"])
    else:
        S.op("pool", lambda: nc.gpsimd.tensor_tensor(hb[:], hf[:], vec["P2"][:], ALU.add), reads=["hf", "P2"], writes=[f"hb{b}"])
    return b


def transpose_to(C, src, src_tok, dst3, dst_tok, col0):
    nc, S = C.nc, C.S
    for k in range(8):
        S.op("pe", lambda: nc.tensor.transpose(C.bankb[:, k * 128:(k + 1) * 128], src[:, k * 128:(k + 1) * 128], C.identb[:]),
             reads=[src_tok, "identb"], writes=["bankb"], inc=(k == 7))
    S.op("act", lambda: nc.scalar.copy(dst3[:, :, col0:col0 + 128], C.bankb[:].rearrange("p (k t) -> p k t", k=8)),
         reads=["bankb"], writes=[dst_tok])


def ffn_sublayer(C, u_in_d, u_out_d, vec, experts, Tn, router_d=None, name="ffn"):
    nc, S = C.nc, C.S
    HT = min(2048, Tn)
    NS = HT // 128
    moe = router_d is not None
    with ExitStack() as es:
        def sb(nm, shape, dt):
            return es.enter_context(nc.sbuf_tensor(f"{name}_{nm}", list(shape), dt))
        hT = sb("hT", [128, 8, HT], BF16)
        yacc = sb("yacc", [128, NS, 1024], F32)
        ph = {"ut": [sb(f"ut{i}", [128, 1024], F32) for i in range(2)],
              "xn": [sb(f"xn{i}", [128, 1024], F32) for i in range(2)],
              "hb": [sb(f"hb{i}", [128, 1024], BF16) for i in range(2)],
              "hf": sb("hf", [128, 1024], F32),
              "mv": [sb(f"mv{i}", [128, 2], F32) for i in range(2)],
              "rstd": [sb(f"rstd{i}", [128, 1], F32) for i in range(2)],
              "st": sb("st", [128, 2, 6], F32)}
        GC = 4
        groups = [list(range(c, min(c + GC, NCH))) for c in range(0, NCH, GC)]
        NWB = 2
        w1g = [sb(f"w1g{i}", [128, 8, GC * 128], BF16) for i in range(NWB)]
        w3g = [sb(f"w3g{i}", [128, 8, GC * 128], BF16) for i in range(NWB)]
        w2g = [sb(f"w2g{i}", [128, GC, 1024], BF16) for i in range(NWB)]
        NAB = 4
        sil = [sb(f"sil{i}", [128, 256], BF16) for i in range(NAB)]
        actT = [sb(f"actT{i}", [128, 256], BF16) for i in range(NAB)]
        if moe:
            hTf = sb("hTf", [128, 8, 128], F32)
            wr = sb("wr", [128, 8, NE], F32)
            lg = sb("lg", [128, NE], F32)
            m8 = sb("m8", [128, 8], F32)
            gts = sb("gts", [128, 2], F32)
            dlt = sb("dlt", [128, 2], F32)
            cm1 = sb("cm1", [128, NE], F32)
            comb = sb("comb", [128, NS, NE], F32)
            S.dma("sp", wr[:], router_d.rearrange("(k p) e -> p k e", p=128), writes=["wr"])
        abank = [C.bank[0], C.bank[1], C.bank[2]]
        accb = [C.bank[3], C.bank[4], C.bank[5], C.bank[6]]
        wld = 0
        abi = 0
        for half in range(Tn // HT):
            t0 = half * HT
            for s in range(NS):
                b = ln_front(C, ph, u_in_d, t0 + s * 128, vec, want_f32=moe)
                xn = ph["xn"][b]
                S.op("dve", lambda: nc.vector.tensor_tensor(yacc[:, s, :], xn[:], vec["Q1"][:], ALU.mult),
                     reads=[ph["tx"], "Q1"], writes=[f"yacc{s}"])
                S.op("pool", lambda: nc.gpsimd.tensor_tensor(yacc[:, s, :], yacc[:, s, :], vec["Q2"][:], ALU.add),
                     reads=[f"yacc{s}", "Q2"], writes=[f"yacc{s}"])
                transpose_to(C, ph["hb"][b], f"hb{b}", hT, f"hT{s // 2}", s * 128)
                if moe:
                    hf = ph["hf"]
                    for q in range(2):
                        for kk in range(4):
                            k = q * 4 + kk
                            S.op("pe", lambda: nc.tensor.transpose(C.bank[q][:, kk * 128:(kk + 1) * 128], hf[:, k * 128:(k + 1) * 128], C.identf[:]),
                                 reads=["hf", "identf"], writes=[f"b{q}"], inc=(kk == 3))
                        S.op("dve", lambda: nc.vector.tensor_copy(hTf[:, q * 4:(q + 1) * 4, :], C.bank[q][:].rearrange("p (k t) -> p k t", k=4)),
                             reads=[f"b{q}"], writes=["hTf"])
                    for k in range(8):
                        S.op("pe", lambda: nc.tensor.matmul(C.bank[2][:, 0:NE], hTf[:, k, :], wr[:, k, :], start=(k == 0), stop=(k == 7)),
                             reads=["hTf", "wr"], writes=["b2"], inc=(k == 7))
                    S.op("dve", lambda: nc.vector.tensor_copy(lg[:], C.bank[2][:, 0:NE]), reads=["b2"], writes=["lg"])
                    S.op("dve", lambda: nc.vector.max(m8[:], lg[:]), reads=["lg"], writes=["m8"])
                    S.op("dve", lambda: nc.vector.tensor_tensor(dlt[:, 0:1], m8[:, 0:1], m8[:, 1:2], ALU.subtract), reads=["m8"], writes=["dlt"])
                    S.op("dve", lambda: nc.vector.tensor_tensor(dlt[:, 1:2], m8[:, 1:2], m8[:, 0:1], ALU.subtract), reads=["m8", "dlt"], writes=["dlt"])
                    S.op("act", lambda: nc.scalar.activation(out=gts[:], in_=dlt[:], func=AF.Sigmoid), reads=["dlt"], writes=["gts"])
                    S.op("dve", lambda: nc.vector.tensor_scalar(cm1[:], lg[:], m8[:, 0:1], gts[:, 0:1], ALU.is_equal, ALU.mult),
                         reads=["lg", "m8", "gts"], writes=["cm1"])
                    S.op("dve", lambda: nc.vector.tensor_scalar(comb[:, s, :], lg[:], m8[:, 1:2], gts[:, 1:2], ALU.is_equal, ALU.mult),
                         reads=["lg", "m8", "gts"], writes=[f"comb{s}"])
                    S.op("dve", lambda: nc.vector.tensor_tensor(comb[:, s, :], comb[:, s, :], cm1[:], ALU.add),
                         reads=[f"comb{s}", "cm1"], writes=[f"comb{s}"])
            for e, (w1_d, w3_d, w2_d) in enumerate(experts):
                w1v = w1_d.rearrange("(k p) f -> p k f", p=128)
                w3v = w3_d.rearrange("(k p) f -> p k f", p=128)
                w2v = w2_d.rearrange("(c p) d -> p c d", p=128)
                for grp in groups:
                    wb = wld % NWB
                    wld += 1
                    ng = len(grp)
                    f0 = grp[0] * 128
                    S.dma("pool", w1g[wb][:, :, 0:ng * 128], w1v[:, :, f0:f0 + ng * 128], writes=[f"w1g{wb}"])
                    S.dma("pool", w3g[wb][:, :, 0:ng * 128], w3v[:, :, f0:f0 + ng * 128], writes=[f"w3g{wb}"])
                    S.dma("pool", w2g[wb][:, 0:ng, :], w2v[:, grp[0]:grp[0] + ng, :], writes=[f"w2g{wb}"])
                    for cc in range(ng):
                        S.op("pool", lambda: nc.gpsimd.tensor_tensor(w2g[wb][:, cc, :], w2g[wb][:, cc, :], vec["Q3b"][:], ALU.mult),
                             reads=[f"w2g{wb}", "Q3b"], writes=[f"w2g{wb}"])
                    seq = [(j, cc) for j in range(HT // 256) for cc in range(ng)]
                    SKEW = 2
                    st_ = {}

                    def up(j, cc):
                        nonlocal abi
                        c0 = j * 256
                        ab = abank[abi % 3]
                        abt = f"b{abi % 3}"
                        bf = abi % NAB
                        abi += 1
                        st_[(j, cc)] = bf
                        for k in range(8):
                            S.op("pe", lambda: nc.tensor.matmul(ab[:, 0:256], w1g[wb][:, k, cc * 128:(cc + 1) * 128], hT[:, k, c0:c0 + 256],
                                                                start=(k == 0), stop=(k == 7)),
                                 reads=[f"w1g{wb}", f"hT{j}"], writes=[abt], inc=False)
                        for k in range(8):
                            S.op("pe", lambda: nc.tensor.matmul(ab[:, 256:512], w3g[wb][:, k, cc * 128:(cc + 1) * 128], hT[:, k, c0:c0 + 256],
                                                                start=(k == 0), stop=(k == 7)),
                                 reads=[f"w3g{wb}", f"hT{j}"], writes=[abt], inc=(k == 7))
                        S.op("act", lambda: nc.scalar.activation(out=sil[bf][:], in_=ab[:, 0:256], func=AF.Silu),
                             reads=[abt], writes=[f"sil{bf}"])
                        S.op("dve", lambda: nc.vector.tensor_tensor(actT[bf][:], ab[:, 256:512], sil[bf][:], ALU.mult),
                             reads=[abt, f"sil{bf}"], writes=[f"actT{bf}"])

                    def down(j, cc):
                        bf = st_[(j, cc)]
                        for s2 in range(2):
                            for hlf in range(2):
                                bi = s2 * 2 + hlf
                                S.op("pe", lambda: nc.tensor.matmul(accb[bi][:, :], actT[bf][:, s2 * 128:(s2 + 1) * 128],
                                                                    w2g[wb][:, cc, hlf * 512:(hlf + 1) * 512],
                                                                    start=(cc == 0), stop=(cc == ng - 1)),
                                     reads=[f"actT{bf}", f"w2g{wb}"], writes=[f"b{3 + bi}"], inc=(cc == ng - 1 or bi == 3))
                        if cc == ng - 1:
                            for s2 in range(2):
                                s = j * 2 + s2
                                for hlf in range(2):
                                    bi = s2 * 2 + hlf
                                    ysl = yacc[:, s, hlf * 512:(hlf + 1) * 512]
                                    if moe:
                                        S.op("dve", lambda: nc.vector.scalar_tensor_tensor(ysl, accb[bi][:, :], comb[:, s, e:e + 1], ysl, ALU.mult, ALU.add),
                                             reads=[f"b{3 + bi}", f"comb{s}", f"yacc{s}"], writes=[f"yacc{s}"])
                                    else:
                                        S.op("dve", lambda: nc.vector.tensor_tensor(ysl, accb[bi][:, :], ysl, ALU.add),
                                             reads=[f"b{3 + bi}", f"yacc{s}"], writes=[f"yacc{s}"])

                    for idx in range(len(seq) + SKEW):
                        if idx < len(seq):
                            up(*seq[idx])
                        if idx >= SKEW:
                            down(*seq[idx - SKEW])
            for s in range(NS):
                S.dma("sp", u_out_d[t0 + s * 128:t0 + (s + 1) * 128, :], yacc[:, s, :], reads=[f"yacc{s}"])
        barrier(C)


MLA_SCALE = 96 ** -0.5


def mixer_sublayer(C, u_in_d, u_out_d, vec, W, Tn, name="mix", TQ=256):
    nc, S = C.nc, C.S
    V_, A_, G_, P_ = nc.vector, nc.scalar, nc.gpsimd, nc.tensor
    NT = Tn // TQ
    NSUB = TQ // 128
    NKT = Tn // 128

    def dve(fn, r=(), w=()): return S.op("dve", fn, r, w)
    def act(fn, r=(), w=()): return S.op("act", fn, r, w)
    def pool(fn, r=(), w=()): return S.op("pool", fn, r, w)
    def pe(fn, r=(), w=(), inc=True): return S.op("pe", fn, r, w, inc=inc)

    rb = {"i": 0}

    def nextbank():
        i = rb["i"] % 3
        rb["i"] += 1
        return C.bank[i], f"b{i}"

    fb = {"i": 0}

    def fbank():
        i = (0, 1)[fb["i"] % 2]
        fb["i"] += 1
        return C.bank[i], f"b{i}"

    bb = {"i": 0}

    def bbank():
        i = (2, 5)[bb["i"] % 2]
        bb["i"] += 1
        return C.bank[i], f"b{i}"

    with ExitStack() as es:
        def sb(nm, shape, dt):
            return es.enter_context(nc.sbuf_tensor(f"{name}_{nm}", list(shape), dt))

        win = sb("win", [128, 8, D_IN], BF16)
        w_in_v = W["w_in"].rearrange("(k p) e -> p k e", p=128)
        for k in range(8):
            S.dma("pool", win[:, k, :], w_in_v[:, k, :], writes=[f"win{k}"])
        WIN = [f"win{k}" for k in range(8)]
        wkA = sb("wkA", [128, 8, 96], BF16)
        wkB = sb("wkB", [128, 8, 96], BF16)
        pool(lambda: G_.memset(wkA[:], 0.0), w=["wkA"])
        pool(lambda: G_.memset(wkB[:], 0.0), w=["wkB"])
        pool(lambda: G_.tensor_copy(wkA[:, :, 64:96], win[:, :, 1672:1704]), r=WIN, w=["wkA"])
        pool(lambda: G_.tensor_copy(wkB[:, :, 64:80], win[:, :, 1688:1704]), r=WIN, w=["wkB"])
        pool(lambda: G_.tensor_copy(wkB[:, :, 80:96], win[:, :, 1672:1688]), r=WIN, w=["wkB"])
        wout = sb("wout", [128, 8, 1024], BF16)
        w_out_v = W["w_out"].rearrange("(k p) e -> p k e", p=128)
        for k in range(8):
            S.dma("pool", wout[:, k, :], w_out_v[:, k, :], writes=[f"wout{k}"])
            pool(lambda: G_.tensor_tensor(wout[:, k, :], wout[:, k, :], vec["Q3b"][:], ALU.mult), r=[f"wout{k}", "Q3b"], w=[f"wout{k}"])
        WOUT = [f"wout{k}" for k in range(8)]
        rest = sb("rest", [128, 1024], F32)
        tmpw = rest[:].rearrange("p (a b) -> p a b", a=2)
        qn = sb("qn", [128, 2], F32)
        kvn_g = sb("kvn_g", [128, 1], F32)
        with nc.allow_non_contiguous_dma(reason="tiny param vectors"):
            S.dma("sp", qn[:], W["q_norm"].rearrange("(b p) -> p b", p=128), writes=["qn"])
            S.dma("sp", kvn_g[:], W["kv_norm"].rearrange("(p o) -> p o", o=1), writes=["kvn_g"])
        S.dma("sp", tmpw[:, :, 0:384], W["w_qb"].rearrange("(b p) e -> p b e", p=128), writes=["rest"])
        wqb = sb("wqb", [128, 2, 384], BF16)
        wqs = sb("wqs", [128, 2, 384], BF16)
        for b in range(2):
            dve(lambda: V_.tensor_scalar(wqb[:, b, :], tmpw[:, b, 0:384], qn[:, b:b + 1], None, ALU.mult), r=["rest", "qn"], w=["wqb"])
        pool(lambda: G_.memset(wqs[:], 0.0), w=["wqs"])
        for hd in range(4):
            o = hd * 96
            pool(lambda: G_.tensor_copy(wqs[:, :, o + 64:o + 80], wqb[:, :, o + 80:o + 96]), r=["wqb"], w=["wqs"])
            pool(lambda: G_.tensor_copy(wqs[:, :, o + 80:o + 96], wqb[:, :, o + 64:o + 80]), r=["wqb"], w=["wqs"])
        S.dma("sp", tmpw[:, 0, :], W["w_kvb"], reads=["wqb"], writes=["rest"])
        wkv = sb("wkv", [128, 512], BF16)
        dve(lambda: V_.tensor_scalar(wkv[:], tmpw[:, 0, :], kvn_g[:, 0:1], None, ALU.mult), r=["rest", "kvn_g"], w=["wkv"])
        wkv4 = wkv[:].rearrange("p (h c) -> p h c", c=128)
        cwr = sb("cwr", [5, 768], F32)
        S.dma("sp", cwr[0:4, :], W["conv_w"], writes=["cwr"])
        S.dma("sp", cwr[4:5, :], W["conv_b"].rearrange("(o c) -> o c", o=1), writes=["cwr"])
        cw = sb("cw", [128, 6, 5], F32)
        bk, bt = nextbank()
        for blk in range(6):
            pe(lambda: P_.transpose(bk[:, blk * 5:blk * 5 + 5], cwr[0:5, blk * 128:(blk + 1) * 128], C.identf[0:5, 0:5]),
               r=["cwr", "identf"], w=[bt], inc=(blk == 5))
        dve(lambda: V_.tensor_copy(cw[:].rearrange("p a b -> p (a b)"), bk[:, 0:30]), r=[bt], w=["cw"])
        Abc = sb("Abc", [128, 8], F32)
        dtb = sb("dtb", [128, 8], F32)
        Dbc = sb("Dbc", [128, 8], F32)
        load_bcast(C, Abc, W["a_log"], "Abc")
        load_bcast(C, dtb, W["dt_bias"], "dtb")
        load_bcast(C, Dbc, W["ssd_d"], "Dbc")
        act(lambda: A_.activation(out=Abc[:], in_=Abc[:], func=AF.Exp), r=["Abc"], w=["Abc"])
        dve(lambda: V_.tensor_scalar(Abc[:], Abc[:], -1.0, None, ALU.mult), r=["Abc"], w=["Abc"])
        nw = sb("nw", [128, 512], F32)
        load_bcast(C, nw, W["ssd_norm_w"], "nw")
        gmg = sb("gmg", [128, 256], F32)
        gmb = sb("gmb", [128, 256], F32)
        load_bcast(C, gmg, W["gm_ln_g"], "gmg")
        load_bcast(C, gmb, W["gm_ln_b"], "gmb")
        bs = sb("bs", [128, 4], F32)
        with nc.allow_non_contiguous_dma(reason="tiny param vectors"):
            S.dma("sp", bs[:], W["gm_b_s"].rearrange("g t -> t g"), writes=["bs"])
        WT = sb("WT", [128, 4, 128], BF16)
        for g in range(4):
            S.dma("sp", tmpw[:, 1, 0:128], W["gm_w_s"][g], writes=["rest"])
            bk, bt = nextbank()
            pe(lambda: P_.transpose(bk[:, 0:128], tmpw[:, 1, 0:128], C.identf[:]), r=["rest", "identf"], w=[bt])
            dve(lambda: V_.tensor_tensor(WT[:, g, :], bk[:, 0:128], C.triu[:], ALU.mult), r=[bt, "cpack"], w=["WT"])

        KT = [sb(f"KT{hd}", [96, Tn], BF16) for hd in range(4)]
        Vt = sb("Vt", [128, NKT, 4, 65], BF16)
        pool(lambda: G_.memset(Vt[:, :, :, 64:65], 1.0), w=["Vones"])
        Srun = sb("Srun", [128, 4, 64], F32)
        Sbf = sb("Sbf", [128, 4, 64], BF16)
        pool(lambda: G_.memset(Srun[:], 0.0), w=["Srun"])
        pool(lambda: G_.memset(Sbf[:], 0.0), w=["Sbf"])
        halo = sb("halo", [128, 6, 3], F32)
        pool(lambda: G_.memset(halo[:], 0.0), w=["halo"])
        xct = [sb(f"xct{i}", [128, 3 + TQ], F32) for i in range(2)]

        _ut = sb("ut0", [128, 1024], F32)
        _hb = sb("hb0", [128, 1024], BF16)
        ph = {"ut": [_ut, _ut], "hb": [_hb, _hb], "hf": None, "single": True,
              "mv": [sb(f"mv{i}", [128, 2], F32) for i in range(2)],
              "rstd": [sb(f"rstd{i}", [128, 1], F32) for i in range(2)],
              "st": sb("st", [128, 2, 6], F32)}
        ph["xn"] = ph["ut"]
        hT = sb("hT", [128, 8, TQ], BF16)
        zsD = [sb(f"zs{i}", [128, NSUB, 512], BF16) for i in range(2)]
        ggD = [sb(f"gg{i}", [128, NSUB, 512], BF16) for i in range(2)]
        dtrD = [sb(f"dtr{i}", [128, NSUB, 8], F32) for i in range(2)]
        xaD = [sb(f"xa{i}", [128, 6, TQ], BF16) for i in range(2)]
        cacc = sb("cacc", [128, TQ], F32)
        qlT = sb("qlT", [128, 2, TQ], BF16)
        sqT = sb("sqT", [128, 2, TQ], BF16)
        rq = sb("rq", [128, TQ], F32)
        rkv = sb("rkv", [128, TQ], F32)
        kvnT = sb("kvnT", [128, TQ], BF16)
        QTD = [sb(f"QT{i}", [96, 4, TQ], BF16) for i in range(2)]
        ccs = sb("ccs", [96, 2, TQ], F32)
        qt1 = sb("qt1", [96, TQ], F32)
        qt2 = sb("qt2", [96, TQ], F32)
        NPT = 3
        pt = [sb(f"pt{i}", [128, TQ], BF16) for i in range(NPT)]
        rcp = sb("rcp", [128, NSUB], F32)
        ycat = [sb(f"ycat{i}", [128, 1024], BF16) for i in range(NSUB)]
        yT = sb("yT", [128, 8, 128], BF16)
        dif = sb("dif", [128, 8, 128], F32)
        axT = dif
        eA = sb("eA", [128, 8, 128], F32)
        Mt = sb("Mt", [128, 8, 128], BF16)
        CsT = sb("CsT", [128, 4, 128], BF16)
        xtok = sb("xtok", [128, 512], BF16)
        xdt = sb("xdt", [128, 512], BF16)
        xdw = sb("xdw", [128, 512], BF16)
        Btok = sb("Btok", [128, 128], BF16)
        dts = sb("dts", [128, 8], F32)
        t8a = sb("t8a", [128, 8], F32)
        t8b = sb("t8b", [128, 8], F32)
        av = sb("av", [128, 8], F32)
        acs = sb("acs", [128, 8], F32)
        dend = sb("dend", [128, 8], F32)
        eAl = sb("eAl", [128, 8], F32)
        ys = sb("ys", [128, 512], F32)
        gst = sb("gst", [128, 2, 6], F32)
        gmv = sb("gmv", [128, 2, 2], F32)
        grs = sb("grs", [128, 2], F32)
        vnb = sb("vnb", [128, 256], BF16)
        vnf = sb("vnf", [128, 256], F32)
        outt = rest
        ones_b = sb("ones_b", [128, 128], BF16)
        dve(lambda: V_.tensor_copy(ones_b[:], C.ones[:]), r=["cpack"], w=["ones_b"])
        print("mixer sbuf bytes remaining", nc.sbuf_bytes_remaining)

        def front(j):
            par = j % 2
            zs, gg, dtr, xa, QT = zsD[par], ggD[par], dtrD[par], xaD[par], QTD[par]
            T0 = j * TQ
            if S.can_yield(): yield
            S.dma("sp", ccs[:, 0, :], W["rope"][0, :, T0:T0 + TQ], writes=["cc"])
            if S.can_yield(): yield
            S.dma("sp", ccs[:, 1, :], W["rope"][1, :, T0:T0 + TQ], writes=["ss"])
            for s in range(NSUB):
                if S.can_yield(): yield
                b = ln_front(C, ph, u_in_d, T0 + s * 128, vec)
                xn = ph["xn"][b]
                if S.can_yield(): yield
                dve(lambda: V_.tensor_tensor(rest[:], xn[:], vec["Q1"][:], ALU.mult), r=[ph["tx"], "Q1"], w=["rest"])
                if S.can_yield(): yield
                pool(lambda: G_.tensor_tensor(rest[:], rest[:], vec["Q2"][:], ALU.add), r=["rest", "Q2"], w=["rest"])
                if S.can_yield(): yield
                S.dma("sp", u_out_d[T0 + s * 128:T0 + (s + 1) * 128, :], rest[:], reads=["rest"], writes=[f"uo{j}_{s}"])
                if S.can_yield(): yield
                transpose_to(C, ph["hb"][b], f"hb{b}", hT, "hT", s * 128)
            for s in range(NSUB):
                lh = lambda k: hT[:, k, s * 128:(s + 1) * 128]
                bk, bt = fbank()
                for k in range(8):
                    if S.can_yield(): yield
                    pe(lambda: P_.matmul(bk[:, :], lh(k), win[:, k, 1704:2216], start=(k == 0), stop=(k == 7)), r=["hT", WIN[k]], w=[bt], inc=(k == 7))
                if S.can_yield(): yield
                act(lambda: A_.activation(out=gg[:, s, :], in_=bk[:, :], func=AF.Gelu), r=[bt], w=[f"gg{par}_{s}"])
            for s in range(NSUB):
                lh = lambda k: hT[:, k, s * 128:(s + 1) * 128]
                bk, bt = fbank()
                for k in range(8):
                    if S.can_yield(): yield
                    pe(lambda: P_.matmul(bk[:, 0:8], lh(k), win[:, k, 1280:1288], start=(k == 0), stop=(k == 7)), r=["hT", WIN[k]], w=[bt], inc=(k == 7))
                if S.can_yield(): yield
                dve(lambda: V_.tensor_tensor(dtr[:, s, :], bk[:, 0:8], dtb[:], ALU.add), r=[bt, "dtb"], w=[f"dtr{par}_{s}"])
            for s in range(NSUB):
                lh = lambda k: hT[:, k, s * 128:(s + 1) * 128]
                bk, bt = fbank()
                for k in range(8):
                    if S.can_yield(): yield
                    pe(lambda: P_.matmul(bk[:, :], lh(k), win[:, k, 0:512], start=(k == 0), stop=(k == 7)), r=["hT", WIN[k]], w=[bt], inc=(k == 7))
                if S.can_yield(): yield
                for hz in range(512 // TQ):
                    zsl = slice(hz * TQ, (hz + 1) * TQ)
                    if S.can_yield(): yield
                    act(lambda: A_.activation(out=cacc[:], in_=bk[:, zsl], func=AF.Exp, scale=-1.0), r=[bt], w=["cacc"])
                    if S.can_yield(): yield
                    act(lambda: A_.activation(out=cacc[:], in_=cacc[:], func=AF.Ln, bias=C.ones[:, 0:1]), r=["cacc", "cpack"], w=["cacc"])
                    act(lambda: A_.activation(out=cacc[:], in_=cacc[:], func=AF.Exp, scale=-1.0), r=["cacc"], w=["cacc"])
                    dve(lambda: V_.tensor_tensor(zs[:, s, zsl], bk[:, zsl], cacc[:], ALU.mult), r=[bt, "cacc"], w=[f"zs{par}_{s}"])
            for blk in range(6):
                c0 = 512 + blk * 128
                bk, bt = fbank()
                for k in range(8):
                    if S.can_yield(): yield
                    pe(lambda: P_.matmul(bk[:, 0:TQ], win[:, k, c0:c0 + 128], hT[:, k, :], start=(k == 0), stop=(k == 7)), r=["hT", WIN[k]], w=[bt], inc=(k == 7))
                xb = xct[blk % 2]
                xbt = f"xct{blk % 2}"
                if S.can_yield(): yield
                act(lambda: A_.copy(xb[:, 3:3 + TQ], bk[:, 0:TQ]), r=[bt], w=[xbt])
                if S.can_yield(): yield
                pool(lambda: G_.tensor_copy(xb[:, 0:3], halo[:, blk, :]), r=["halo"], w=[xbt])
                if S.can_yield(): yield
                dve(lambda: V_.tensor_scalar(cacc[:], xb[:, 3:3 + TQ], cw[:, blk, 3:4], cw[:, blk, 4:5], ALU.mult, ALU.add),
                    r=[xbt, "cw"], w=["cacc"])
                for kk in range(3):
                    if S.can_yield(): yield
                    dve(lambda: V_.scalar_tensor_tensor(cacc[:], xb[:, kk:kk + TQ], cw[:, blk, kk:kk + 1], cacc[:], ALU.mult, ALU.add),
                        r=[xbt, "cw", "cacc"], w=["cacc"])
                if S.can_yield(): yield
                pool(lambda: G_.tensor_copy(halo[:, blk, :], xb[:, TQ:TQ + 3]), r=[xbt], w=["halo"])
                if S.can_yield(): yield
                act(lambda: A_.activation(out=xb[:, 0:TQ], in_=cacc[:], func=AF.Exp, scale=-1.0), r=["cacc", "halo"], w=[xbt])
                if S.can_yield(): yield
                act(lambda: A_.activation(out=xb[:, 0:TQ], in_=xb[:, 0:TQ], func=AF.Ln, bias=C.ones[:, 0:1]), r=[xbt, "cpack"], w=[xbt])
                act(lambda: A_.activation(out=xb[:, 0:TQ], in_=xb[:, 0:TQ], func=AF.Exp, scale=-1.0), r=[xbt], w=[xbt])
                dve(lambda: V_.tensor_tensor(xa[:, blk, :], cacc[:], xb[:, 0:TQ], ALU.mult), r=["cacc", xbt], w=[f"xa{par}_{blk}"])
            for b2 in range(2):
                c0 = 1288 + b2 * 128
                bk, bt = fbank()
                for k in range(8):
                    if S.can_yield(): yield
                    pe(lambda: P_.matmul(bk[:, 0:TQ], win[:, k, c0:c0 + 128], hT[:, k, :], start=(k == 0), stop=(k == 7)), r=["hT", WIN[k]], w=[bt], inc=(k == 7))
                if S.can_yield(): yield
                act(lambda: A_.copy(qlT[:, b2, :], bk[:, 0:TQ]), r=[bt], w=["qlT"])
                if S.can_yield(): yield
                act(lambda: A_.activation(out=sqT[:, b2, :], in_=bk[:, 0:TQ], func=AF.Square), r=[bt], w=["sqT"])
            bk, bt = fbank()
            for b2 in range(2):
                if S.can_yield(): yield
                pe(lambda: P_.matmul(bk[:, 0:TQ], ones_b[:], sqT[:, b2, :], start=(b2 == 0), stop=(b2 == 1)), r=["ones_b", "sqT"], w=[bt], inc=(b2 == 1))
            if S.can_yield(): yield
            act(lambda: A_.activation(out=rq[:], in_=bk[:, 0:TQ], func=AF.Ln, scale=1.0 / 256, bias=C.eps_rms[:, 0:1]), r=[bt, "eps_rms"], w=["rq"])
            if S.can_yield(): yield
            act(lambda: A_.activation(out=rq[:], in_=rq[:], func=AF.Exp, scale=-0.5), r=["rq"], w=["rq"])
            bk, bt = fbank()
            for k in range(8):
                if S.can_yield(): yield
                pe(lambda: P_.matmul(bk[:, 0:TQ], win[:, k, 1544:1672], hT[:, k, :], start=(k == 0), stop=(k == 7)), r=["hT", WIN[k]], w=[bt], inc=(k == 7))
            if S.can_yield(): yield
            act(lambda: A_.activation(out=sqT[:, 0, :], in_=bk[:, 0:TQ], func=AF.Square), r=[bt], w=["sqT"])
            bk2, bt2 = fbank()
            if S.can_yield(): yield
            pe(lambda: P_.matmul(bk2[:, 0:TQ], ones_b[:], sqT[:, 0, :], start=True, stop=True), r=["ones_b", "sqT"], w=[bt2])
            if S.can_yield(): yield
            act(lambda: A_.activation(out=rkv[:], in_=bk2[:, 0:TQ], func=AF.Ln, scale=1.0 / 128, bias=C.eps_rms[:, 0:1]), r=[bt2, "eps_rms"], w=["rkv"])
            if S.can_yield(): yield
            act(lambda: A_.activation(out=rkv[:], in_=rkv[:], func=AF.Exp, scale=-0.5), r=["rkv"], w=["rkv"])
            if S.can_yield(): yield
            dve(lambda: V_.tensor_tensor(kvnT[:], bk[:, 0:TQ], rkv[:], ALU.mult), r=[bt, "rkv"], w=["kvnT"])
            bk, bt = fbank()
            for k in range(8):
                if S.can_yield(): yield
                pe(lambda: P_.matmul(bk[0:96, 0:TQ], wkA[:, k, :], hT[:, k, :], start=(k == 0), stop=(k == 7)), r=["hT", "wkA"], w=[bt], inc=(k == 7))
            bk2, bt2 = fbank()
            for k in range(8):
                if S.can_yield(): yield
                pe(lambda: P_.matmul(bk2[0:96, 0:TQ], wkB[:, k, :], hT[:, k, :], start=(k == 0), stop=(k == 7)), r=["hT", "wkB"], w=[bt2], inc=(k == 7))
            if S.can_yield(): yield
            dve(lambda: V_.tensor_tensor(qt1[64:96, :], bk[64:96, 0:TQ], ccs[64:96, 0, :], ALU.mult), r=[bt, "cc"], w=["qt1"])
            if S.can_yield(): yield
            dve(lambda: V_.tensor_tensor(qt2[64:96, :], bk2[64:96, 0:TQ], ccs[64:96, 1, :], ALU.mult), r=[bt2, "ss"], w=["qt2"])
            for hd in range(4):
                if S.can_yield(): yield
                pool(lambda: G_.tensor_tensor(KT[hd][64:96, T0:T0 + TQ], qt1[64:96, :], qt2[64:96, :], ALU.add), r=["qt1", "qt2"], w=[f"KT{hd}_{j}"])
            for hd in range(4):
                bk, bt = fbank()
                if S.can_yield(): yield
                pe(lambda: P_.matmul(bk[0:64, 0:TQ], wkv[:, hd * 128:hd * 128 + 64], kvnT[:], start=True, stop=True), r=["wkv", "kvnT"], w=[bt])
                if S.can_yield(): yield
                act(lambda: A_.copy(KT[hd][0:64, T0:T0 + TQ], bk[0:64, 0:TQ]), r=[bt], w=[f"KT{hd}_{j}"])
            for s in range(NSUB):
                bk, bt = fbank()
                for hd in range(4):
                    if S.can_yield(): yield
                    pe(lambda: P_.matmul(bk[:, hd * 64:(hd + 1) * 64], kvnT[:, s * 128:(s + 1) * 128], wkv[:, hd * 128 + 64:hd * 128 + 128], start=True, stop=True),
                       r=["wkv", "kvnT"], w=[bt], inc=(hd == 3))
                if S.can_yield(): yield
                act(lambda: A_.copy(Vt[:, j * NSUB + s, :, 0:64], bk[:, 0:256].rearrange("p (h c) -> p h c", c=64)), r=[bt], w=[f"V{j * NSUB + s}"])
            for hd in range(4):
                bk, bt = fbank()
                for b2 in range(2):
                    if S.can_yield(): yield
                    pe(lambda: P_.matmul(bk[0:96, 0:TQ], wqb[:, b2, hd * 96:(hd + 1) * 96], qlT[:, b2, :], start=(b2 == 0), stop=(b2 == 1)), r=["wqb", "qlT"], w=[bt], inc=(b2 == 1))
                bk2, bt2 = fbank()
                for b2 in range(2):
                    if S.can_yield(): yield
                    pe(lambda: P_.matmul(bk2[0:96, 0:TQ], wqs[:, b2, hd * 96:(hd + 1) * 96], qlT[:, b2, :], start=(b2 == 0), stop=(b2 == 1)), r=["wqs", "qlT"], w=[bt2], inc=(b2 == 1))
                if S.can_yield(): yield
                dve(lambda: V_.tensor_tensor(qt1[:], bk[0:96, 0:TQ], ccs[:, 0, :], ALU.mult), r=[bt, "cc"], w=["qt1"])
                if S.can_yield(): yield
                dve(lambda: V_.tensor_tensor(qt2[:], bk2[0:96, 0:TQ], ccs[:, 1, :], ALU.mult), r=[bt2, "ss"], w=["qt2"])
                if S.can_yield(): yield
                pool(lambda: G_.tensor_tensor(qt1[:], qt1[:], qt2[:], ALU.add), r=["qt1", "qt2"], w=["qt1"])
                if S.can_yield(): yield
                dve(lambda: V_.tensor_tensor(QT[:, hd, :], qt1[:], rq[0:96, :], ALU.mult), r=["qt1", "rq"], w=[f"QT{par}_{hd}"])


        def back(j):
            par = j % 2
            zs, gg, dtr, xa, QT = zsD[par], ggD[par], dtrD[par], xaD[par], QTD[par]
            T0 = j * TQ
            nkt = NSUB * (j + 1)
            sv = {}
            def ssd1(s):
                ch = j * NSUB + s
                csl = slice(s * 128, (s + 1) * 128)
                if S.can_yield(): yield
                dve(lambda: V_.tensor_scalar(t8a[:], dtr[:, s, :], -1.0, None, ALU.mult), r=[f"dtr{par}_{s}"], w=["t8a"])
                if S.can_yield(): yield
                dve(lambda: V_.tensor_tensor(t8a[:], t8a[:], dtr[:, s, :], ALU.max), r=["t8a", f"dtr{par}_{s}"], w=["t8a"])
                if S.can_yield(): yield
                act(lambda: A_.activation(out=t8a[:], in_=t8a[:], func=AF.Exp, scale=-1.0), r=["t8a"], w=["t8a"])
                if S.can_yield(): yield
                act(lambda: A_.activation(out=t8a[:], in_=t8a[:], func=AF.Ln, bias=C.ones[:, 0:1]), r=["t8a", "cpack"], w=["t8a"])
                if S.can_yield(): yield
                dve(lambda: V_.tensor_scalar(t8b[:], dtr[:, s, :], 0.0, None, ALU.max), r=[f"dtr{par}_{s}"], w=["t8b"])
                if S.can_yield(): yield
                dve(lambda: V_.tensor_tensor(dts[:], t8a[:], t8b[:], ALU.add), r=["t8a", "t8b"], w=["dts"])
                if S.can_yield(): yield
                dve(lambda: V_.tensor_tensor(av[:], dts[:], Abc[:], ALU.mult), r=["dts", "Abc"], w=["av"])
                bk, bt = bbank()
                if S.can_yield(): yield
                pe(lambda: P_.matmul(bk[:, 0:8], C.triu[:], av[:], start=True, stop=True), r=["cpack", "av"], w=[bt], inc=False)
                if S.can_yield(): yield
                pe(lambda: P_.matmul(bk[:, 8:16], C.ones[:], av[:], start=True, stop=True), r=["cpack", "av"], w=[bt])
                if S.can_yield(): yield
                dve(lambda: V_.tensor_copy(acs[:], bk[:, 0:8]), r=[bt], w=["acs"])
                if S.can_yield(): yield
                dve(lambda: V_.tensor_tensor(dend[:], bk[:, 8:16], acs[:], ALU.subtract), r=[bt, "acs"], w=["dend"])
                if S.can_yield(): yield
                act(lambda: A_.activation(out=dend[:], in_=dend[:], func=AF.Exp), r=["dend"], w=["dend"])
                if S.can_yield(): yield
                act(lambda: A_.activation(out=eAl[:], in_=bk[:, 8:16], func=AF.Exp), r=[bt], w=["eAl"])
                if S.can_yield(): yield
                dve(lambda: V_.tensor_tensor(axT[:], C.triu[:].unsqueeze(1).broadcast_to([128, 8, 128]), av[:].unsqueeze(2).broadcast_to([128, 8, 128]), ALU.mult),
                    r=["cpack", "av"], w=["dif0", "dif1"])

            def ssd2(s):
                ch = j * NSUB + s
                csl = slice(s * 128, (s + 1) * 128)
                bkA, btA = bbank()
                bkB, btB = bbank()
                if S.can_yield(): yield
                pe(lambda: P_.matmul(bkA[:, :], C.ones[:], axT[:, 0:4, :].rearrange("p a b -> p (a b)"), start=True, stop=True), r=["cpack", "dif0"], w=[btA])
                if S.can_yield(): yield
                pe(lambda: P_.matmul(bkB[:, :], C.ones[:], axT[:, 4:8, :].rearrange("p a b -> p (a b)"), start=True, stop=True), r=["cpack", "dif1"], w=[btB])
                for hh, (bkx, btx) in enumerate(((bkA, btA), (bkB, btB))):
                    d2 = dif[:, hh * 4:hh * 4 + 4, :].rearrange("p a b -> p (a b)")
                    e2 = eA[:, hh * 4:hh * 4 + 4, :].rearrange("p a b -> p (a b)")
                    if S.can_yield(): yield
                    act(lambda: A_.activation(out=e2, in_=bkx[:, :], func=AF.Exp), r=[btx], w=[f"eA{hh}"])
                    hs = slice(hh * 4, hh * 4 + 4)
                    if S.can_yield(): yield
                    dve(lambda: V_.tensor_tensor(dif[:, hs, :], bkx[:, :].rearrange("p (a b) -> p a b", a=4), acs[:, hs].unsqueeze(2).broadcast_to([128, 4, 128]), ALU.subtract),
                        r=[btx, "acs"], w=[f"dif{hh}"])
                    if S.can_yield(): yield
                    pool(lambda: G_.tensor_tensor(dif[:, hs, :], dif[:, hs, :], C.mneg[:].unsqueeze(1).broadcast_to([128, 4, 128]), ALU.add),
                         r=[f"dif{hh}", "cpack"], w=[f"dif{hh}"])
                    if S.can_yield(): yield
                    act(lambda: A_.activation(out=d2, in_=d2, func=AF.Exp), r=[f"dif{hh}"], w=[f"dif{hh}"])
                cbk = [bbank(), bbank()]
                for g in range(2):
                    gs = slice(g * 64, (g + 1) * 64)
                    if S.can_yield(): yield
                    pe(lambda: P_.matmul(cbk[g][0][:, 0:128], xa[gs, 4, csl], xa[gs, 5, csl], start=True, stop=True), r=[f"xa{par}_4", f"xa{par}_5"], w=[cbk[g][1]])
                for g in range(2):
                    gs = slice(g * 64, (g + 1) * 64)
                    if S.can_yield(): yield
                    dve(lambda: V_.tensor_tensor(Mt[:, g * 4:(g + 1) * 4, :], cbk[g][0][:, 0:128].unsqueeze(1).broadcast_to([128, 4, 128]), dif[:, g * 4:(g + 1) * 4, :], ALU.mult),
                        r=[cbk[g][1], f"dif{g}"], w=[f"Mt{g}"])
                    if S.can_yield(): yield
                    dve(lambda: V_.tensor_tensor(CsT[gs, :, :], xa[gs, 5, csl].unsqueeze(1).broadcast_to([64, 4, 128]), eA[gs, g * 4:(g + 1) * 4, :], ALU.mult),
                        r=[f"xa{par}_5", f"eA{g}"], w=[f"CsT{g}"])
                S.hold = True
                for blk in range(4):
                    if S.can_yield(): yield
                    pe(lambda: P_.transpose(C.bankb[:, blk * 128:(blk + 1) * 128], xa[:, blk, csl], C.identb[:]), r=[f"xa{par}_{blk}", "identb"], w=["bankb"], inc=False)
                if S.can_yield(): yield
                pe(lambda: P_.transpose(C.bankb[:, 512:640], xa[:, 4, csl], C.identb[:]), r=[f"xa{par}_4", "identb"], w=["bankb"])
                if S.can_yield(): yield
                act(lambda: A_.copy(xtok[:], C.bankb[:, 0:512]), r=["bankb"], w=["xtok"])
                if S.can_yield(): yield
                act(lambda: A_.copy(Btok[:], C.bankb[:, 512:640]), r=["bankb"], w=["Btok"])
                S.hold = False
                v3 = lambda t_: t_[:].rearrange("p (h c) -> p h c", c=64)
                b3 = lambda t_: t_[:].unsqueeze(2).broadcast_to([128, 8, 64])
                if S.can_yield(): yield
                dve(lambda: V_.tensor_tensor(v3(xdt), v3(xtok), b3(dts), ALU.mult), r=["xtok", "dts"], w=["xdt"])
                if S.can_yield(): yield
                pool(lambda: G_.tensor_tensor(v3(xdw), v3(xdt), b3(dend), ALU.mult), r=["xdt", "dend"], w=["xdw"])
                if S.can_yield(): yield
                dve(lambda: V_.tensor_tensor(v3(ys), v3(xtok), b3(Dbc), ALU.mult), r=["xtok", "Dbc"], w=["ys"])

            def ssd3(s):
                ch = j * NSUB + s
                csl = slice(s * 128, (s + 1) * 128)
                bk, bt = bbank()
                for h in range(8):
                    g, r_ = h // 4, h % 4
                    gs = slice(g * 64, (g + 1) * 64)
                    if S.can_yield(): yield
                    pe(lambda: P_.matmul(bk[:, h * 64:(h + 1) * 64], Mt[:, h, :], xdt[:, h * 64:(h + 1) * 64], start=True, stop=(ch == 0)),
                       r=[f"Mt{g}", "xdt"], w=[bt], inc=False)
                    if ch > 0:
                        if S.can_yield(): yield
                        pe(lambda: P_.matmul(bk[:, h * 64:(h + 1) * 64], CsT[gs, r_, :], Sbf[gs, r_, :], start=False, stop=True),
                           r=[f"CsT{g}", "Sbf"], w=[bt], inc=False)
                bk2, bt2 = bbank()
                if S.can_yield(): yield
                pe(lambda: P_.matmul(bk2[:, :], Btok[:], xdw[:], start=True, stop=True), r=["Btok", "xdw"], w=[bt2])
                for h in range(8):
                    g, r_ = h // 4, h % 4
                    gs = slice(g * 64, (g + 1) * 64)
                    if S.can_yield(): yield
                    dve(lambda: V_.scalar_tensor_tensor(Srun[gs, r_, :], Srun[gs, r_, :], eAl[gs, h:h + 1], bk2[gs, h * 64:(h + 1) * 64], ALU.mult, ALU.add),
                        r=["Srun", "eAl", bt2], w=["Srun"])
                if S.can_yield(): yield
                act(lambda: A_.copy(Sbf[:], Srun[:]), r=["Srun"], w=["Sbf"])
                if S.can_yield(): yield
                dve(lambda: V_.tensor_tensor(ys[:], bk[:, :], ys[:], ALU.add), r=[bt, "ys"], w=["ys"])
                if S.can_yield(): yield
                pool(lambda: G_.tensor_tensor(ys[:], ys[:], zs[:, s, :], ALU.mult), r=["ys", f"zs{par}_{s}"], w=["ys"])
                for g in range(2):
                    if S.can_yield(): yield
                    dve(lambda: V_.bn_stats(gst[:, g, :], ys[:, g * 256:(g + 1) * 256]), r=["ys"], w=[f"gst{g}"])
                    if S.can_yield(): yield
                    dve(lambda: V_.bn_aggr(gmv[:, g, :], gst[:, g, :]), r=[f"gst{g}"], w=[f"gmv{g}"])
                    if S.can_yield(): yield
                    dve(lambda: V_.tensor_tensor(grs[:, g:g + 1], gmv[:, g, 0:1], gmv[:, g, 0:1], ALU.mult), r=[f"gmv{g}"], w=["grs"])
                    if S.can_yield(): yield
                    dve(lambda: V_.tensor_tensor(grs[:, g:g + 1], grs[:, g:g + 1], gmv[:, g, 1:2], ALU.add), r=["grs", f"gmv{g}"], w=["grs"])
                if S.can_yield(): yield
                act(lambda: A_.activation(out=grs[:], in_=grs[:], func=AF.Ln, bias=C.eps_rms[:, 0:1]), r=["grs", "eps_rms"], w=["grs"])
                if S.can_yield(): yield
                act(lambda: A_.activation(out=grs[:], in_=grs[:], func=AF.Exp, scale=-0.5), r=["grs"], w=["grs"])
                for g in range(2):
                    if S.can_yield(): yield
                    dve(lambda: V_.scalar_tensor_tensor(ycat[s][:, g * 256:(g + 1) * 256], ys[:, g * 256:(g + 1) * 256], grs[:, g:g + 1], nw[:, g * 256:(g + 1) * 256],
                                                        ALU.mult, ALU.mult), r=["ys", "grs", "nw"], w=[f"ycat{s}"])

            def gmlp(s):
                if S.can_yield(): yield
                dve(lambda: V_.bn_stats(gst[:, 0, :], gg[:, s, 256:512]), r=[f"gg{par}_{s}"], w=["gst0"])
                if S.can_yield(): yield
                dve(lambda: V_.bn_aggr(gmv[:, 0, :], gst[:, 0, :]), r=["gst0"], w=["gmv0"])
                if S.can_yield(): yield
                act(lambda: A_.activation(out=grs[:, 0:1], in_=gmv[:, 0, 1:2], func=AF.Ln, bias=C.eps_ln[:, 0:1]), r=["gmv0", "eps_ln"], w=["grs"])
                if S.can_yield(): yield
                act(lambda: A_.activation(out=grs[:, 0:1], in_=grs[:, 0:1], func=AF.Exp, scale=-0.5), r=["grs"], w=["grs"])
                if S.can_yield(): yield
                dve(lambda: V_.tensor_scalar(vnf[:], gg[:, s, 256:512], gmv[:, 0, 0:1], grs[:, 0:1], ALU.subtract, ALU.mult), r=[f"gg{par}_{s}", "gmv0", "grs"], w=["vnf"])
                if S.can_yield(): yield
                pool(lambda: G_.tensor_tensor(vnf[:], vnf[:], gmg[:], ALU.mult), r=["vnf", "gmg"], w=["vnf"])
                if S.can_yield(): yield
                pool(lambda: G_.tensor_tensor(vnb[:], vnf[:], gmb[:], ALU.add), r=["vnf", "gmb"], w=["vnb"])
                bk, bt = bbank()
                for g in range(4):
                    if S.can_yield(): yield
                    pe(lambda: P_.matmul(bk[:, g * 64:(g + 1) * 64], WT[:, g, :], vnb[:, g * 64:(g + 1) * 64], start=True, stop=True), r=["WT", "vnb"], w=[bt], inc=(g == 3))
                for g in range(4):
                    if S.can_yield(): yield
                    dve(lambda: V_.scalar_tensor_tensor(ycat[s][:, 768 + g * 64:768 + (g + 1) * 64], bk[:, g * 64:(g + 1) * 64], bs[:, g:g + 1], gg[:, s, g * 64:(g + 1) * 64],
                                                        ALU.add, ALU.mult), r=[bt, "bs", f"gg{par}_{s}"], w=[f"ycat{s}"])


            def attn(hd):
                ob, obt = C.bank[6], "b6"
                o3 = ob[:, 0:NSUB * 65].rearrange("p (q c) -> p q c", c=65)
                for kt in range(nkt):
                    r_ = max(0, kt - NSUB * j)
                    q0 = r_ * 128
                    sbk, sbt = (C.bank[3], "b3") if (kt % 2 == 0) else (C.bank[4], "b4")
                    if S.can_yield(): yield
                    pe(lambda: P_.matmul(sbk[:, q0:TQ], KT[hd][0:96, kt * 128:(kt + 1) * 128], QT[:, hd, q0:TQ], start=True, stop=True),
                       r=[f"KT{hd}_{kt // NSUB}", f"QT{par}_{hd}"], w=[sbt])
                    pb = pt[(hd * nkt + kt) % NPT]
                    pbt = f"pt{(hd * nkt + kt) % NPT}"
                    if S.can_yield(): yield
                    act(lambda: A_.activation(out=pb[:, q0:TQ], in_=sbk[:, q0:TQ], func=AF.Exp, scale=MLA_SCALE), r=[sbt], w=[pbt])
                    if kt >= NSUB * j:
                        if S.can_yield(): yield
                        pool(lambda: G_.tensor_tensor(pb[:, q0:q0 + 128], pb[:, q0:q0 + 128], C.triub[:], ALU.mult), r=[pbt, "triub"], w=[pbt])
                    for qs in range(r_, NSUB):
                        last = (kt == NSUB * j + qs)
                        if S.can_yield(): yield
                        pe(lambda: P_.matmul(o3[:, qs, :], pb[:, qs * 128:(qs + 1) * 128], Vt[:, kt, hd, :], start=(kt == 0 and qs == 0), stop=last, skip_group_check=True),
                           r=[pbt, f"V{kt}", "Vones"], w=[obt], inc=(qs == NSUB - 1))
                if S.can_yield(): yield
                dve(lambda: V_.reciprocal(rcp[:], o3[:, :, 64]), r=[obt], w=["rcp"])
                for qs in range(NSUB):
                    if S.can_yield(): yield
                    dve(lambda: V_.tensor_scalar(ycat[qs][:, 512 + hd * 64:512 + (hd + 1) * 64], o3[:, qs, 0:64], rcp[:, qs:qs + 1], None, ALU.mult),
                        r=[obt, "rcp"], w=[f"ycat{qs}"])


            for s in range(NSUB):
                yield from ssd1(s)
                yield from gmlp(s)
                yield from attn(2 * s)
                yield from ssd2(s)
                yield from attn(2 * s + 1)
                yield from ssd3(s)
            for s in range(NSUB):
                S.hold = True
                for k in range(8):
                    pe(lambda: P_.transpose(C.bankb[:, k * 128:(k + 1) * 128], ycat[s][:, k * 128:(k + 1) * 128], C.identb[:]), r=[f"ycat{s}", "identb"], w=["bankb"], inc=(k == 7))
                if S.can_yield(): yield
                act(lambda: A_.copy(yT[:].rearrange("p k t -> p (k t)"), C.bankb[:]), r=["bankb"], w=["yT"])
                S.hold = False
                if S.can_yield(): yield
                S.dma("sp", outt[:], u_out_d[T0 + s * 128:T0 + (s + 1) * 128, :], reads=[f"uo{j}_{s}"], writes=["rest"])
                for hlf in range(2):
                    bk, bt = bbank()
                    for k in range(8):
                        if S.can_yield(): yield
                        pe(lambda: P_.matmul(bk[:, :], yT[:, k, :], wout[:, k, hlf * 512:(hlf + 1) * 512], start=(k == 0), stop=(k == 7)), r=["yT", WOUT[k]], w=[bt], inc=(k == 7))
                    if S.can_yield(): yield
                    dve(lambda: V_.tensor_tensor(outt[:, hlf * 512:(hlf + 1) * 512], bk[:, :], outt[:, hlf * 512:(hlf + 1) * 512], ALU.add), r=[bt, "rest"], w=["rest"])
                if S.can_yield(): yield
                S.dma("sp", u_out_d[T0 + s * 128:T0 + (s + 1) * 128, :], outt[:], reads=["rest"], writes=[f"uo{j}_{s}"])

        def merge(ga, gb, na, nb_):
            gens, tot, done, live = [ga, gb], [max(na, 1), max(nb_, 1)], [0, 0], [ga is not None, gb is not None]
            while live[0] or live[1]:
                if live[0] and live[1]:
                    gi = 0 if done[0] * tot[1] <= done[1] * tot[0] else 1
                else:
                    gi = 0 if live[0] else 1
                try:
                    next(gens[gi])
                    done[gi] += 1
                except StopIteration:
                    live[gi] = False

        for j in range(NT + 1):
            merge(front(j) if j < NT else None, back(j - 1) if j >= 1 else None, 150, 190 + 24 * NSUB // 2 * max(j - 1, 0))
        barrier(C)


def modulation_vectors(C, c_d, ada_w_d, ada_b_d, gin_d, bin_d, vec, name):
    nc, S = C.nc, C.S
    V_, A_, G_, P_ = nc.vector, nc.scalar, nc.gpsimd, nc.tensor
    with ExitStack() as es:
        def sb(nm, shape, dt):
            return es.enter_context(nc.sbuf_tensor(f"{name}_{nm}", list(shape), dt))
        sc = sb("sc", [128, 8], F32)
        scb = sb("scb", [128, 8, 128], F32)
        wblk = [sb(f"wblk{i}", [128, 8, 512], F32) for i in range(2)]
        mrow = sb("mrow", [128, 3072], F32)
        brow = sb("brow", [128, 3072], F32)
        gt = sb("gt", [128, 1024], F32)
        bt_ = sb("bt", [128, 1024], F32)
        with nc.allow_non_contiguous_dma(reason="tiny conditioning vector"):
            S.dma("sp", sc[:], c_d.rearrange("(k p) -> p k", p=128), writes=["sc"])
        S.op("act", lambda: A_.activation(out=sc[:], in_=sc[:], func=AF.Silu), reads=["sc"], writes=["sc"])
        for k in range(8):
            S.op("dve", lambda: V_.tensor_scalar(scb[:, k, :], C.ones[:], sc[:, k:k + 1], None, ALU.mult), reads=["sc", "cpack"], writes=["scb"])
        S.dma("sp", brow[:], ada_b_d.partition_broadcast(128), writes=["brow"])
        S.dma("sp", gt[:], gin_d.partition_broadcast(128), writes=["gt"])
        S.dma("sp", bt_[:], bin_d.partition_broadcast(128), writes=["bt"])
        wv = ada_w_d.rearrange("(k p) e -> p k e", p=128)
        for cb in range(6):
            wb = wblk[cb % 2]
            S.dma("sp", wb[:], wv[:, :, cb * 512:(cb + 1) * 512], writes=[f"wblk{cb % 2}"])
            bk, bt = C.bank[cb % 2], f"b{cb % 2}"
            for k in range(8):
                S.op("pe", lambda: P_.matmul(bk[:, :], scb[:, k, :], wb[:, k, :], start=(k == 0), stop=(k == 7)),
                     reads=["scb", f"wblk{cb % 2}"], writes=[bt], inc=(k == 7))
            S.op("dve", lambda: V_.tensor_tensor(mrow[:, cb * 512:(cb + 1) * 512], bk[:, :], brow[:, cb * 512:(cb + 1) * 512], ALU.add),
                 reads=[bt, "brow"], writes=["mrow"])
        shift, scale, gate = mrow[:, 0:1024], mrow[:, 1024:2048], mrow[:, 2048:3072]
        S.op("dve", lambda: V_.tensor_scalar(scale, scale, 1.0, None, ALU.add), reads=["mrow"], writes=["mrow"])
        S.op("dve", lambda: V_.tensor_scalar(gate, gate, 1.0, None, ALU.add), reads=["mrow"], writes=["mrow"])
        S.op("dve", lambda: V_.tensor_tensor(vec["P1"][:], gt[:], scale, ALU.mult), reads=["gt", "mrow"], writes=["P1"])
        S.op("dve", lambda: V_.tensor_tensor(vec["P2"][:], bt_[:], scale, ALU.mult), reads=["bt", "mrow"], writes=["P2"])
        S.op("dve", lambda: V_.tensor_tensor(vec["P2"][:], vec["P2"][:], shift, ALU.add), reads=["P2", "mrow"], writes=["P2"])
        S.op("dve", lambda: V_.tensor_scalar(vec["Q1"][:], gt[:], DN_ALPHA, None, ALU.mult), reads=["gt"], writes=["Q1"])
        S.op("dve", lambda: V_.tensor_scalar(vec["Q2"][:], bt_[:], DN_ALPHA, None, ALU.mult), reads=["bt"], writes=["Q2"])
        S.op("dve", lambda: V_.tensor_copy(vec["Q3b"][:], gate), reads=["mrow"], writes=["Q3b"])
        barrier(C)


def final_ln(C, u_d, out_d, g_d, b_d, Tn, name="fin"):
    nc, S = C.nc, C.S
    with ExitStack() as es:
        def sb(nm, shape, dt):
            return es.enter_context(nc.sbuf_tensor(f"{name}_{nm}", list(shape), dt))
        gt = sb("gt", [128, 1024], F32)
        bt_ = sb("bt", [128, 1024], F32)
        S.dma("sp", gt[:], g_d.partition_broadcast(128), writes=["fgt"])
        S.dma("sp", bt_[:], b_d.partition_broadcast(128), writes=["fbt"])
        ut = [sb(f"ut{i}", [128, 1024], F32) for i in range(3)]
        mv = [sb(f"mv{i}", [128, 2], F32) for i in range(3)]
        rstd = [sb(f"rstd{i}", [128, 1], F32) for i in range(3)]
        st = sb("st", [128, 2, 6], F32)
        for s in range(Tn // 128):
            b = s % 3
            S.dma("sp", ut[b][:], u_d[s * 128:(s + 1) * 128, :], writes=[f"fut{b}"])
            ln_stats(C, ut[b][:], f"fut{b}", mv[b][:], f"fmv{b}", rstd[b][:], f"frstd{b}", st)
            S.op("dve", lambda: nc.vector.tensor_scalar(ut[b][:], ut[b][:], mv[b][:, 0:1], rstd[b][:, 0:1], ALU.subtract, ALU.mult),
                 reads=[f"fut{b}", f"fmv{b}", f"frstd{b}"], writes=[f"fut{b}"])
            S.op("pool", lambda: nc.gpsimd.tensor_tensor(ut[b][:], ut[b][:], gt[:], ALU.mult), reads=[f"fut{b}", "fgt"], writes=[f"fut{b}"])
            S.op("dve", lambda: nc.vector.tensor_tensor(ut[b][:], ut[b][:], bt_[:], ALU.add), reads=[f"fut{b}", "fbt"], writes=[f"fut{b}"])
            S.dma("sp", out_d[s * 128:(s + 1) * 128, :], ut[b][:], reads=[f"fut{b}"])
        barrier(C)


W_SHAPES = {
    "ln0_g": [D], "ln0_b": [D], "ada_w": [2, 2, D, 3 * D], "ada_b": [2, 2, 3 * D], "post_ln_g": [2, 2, D], "post_ln_b": [2, 2, D],
    "w_in": [2, D, D_IN], "ssd_conv_w": [2, 4, 768], "ssd_conv_b": [2, 768], "ssd_dt_bias": [2, 8], "ssd_a_log": [2, 8], "ssd_d": [2, 8],
    "ssd_norm_w": [2, 512], "mla_q_norm": [2, 256], "mla_w_qb": [2, 256, 384], "mla_kv_norm": [2, 128], "mla_w_kvb": [2, 128, 512],
    "gm_ln_g": [2, 256], "gm_ln_b": [2, 256], "gm_w_s": [2, 4, 128, 128], "gm_b_s": [2, 4, 128], "w_out": [2, D, D],
    "ffn_w1": [1, D, DFF], "ffn_w3": [1, D, DFF], "ffn_w2": [1, DFF, D], "moe_router": [1, D, NE],
    "moe_w1": [1, NE, D, DFF], "moe_w3": [1, NE, D, DFF], "moe_w2": [1, NE, DFF, D],
}


def host_consts(Tn):
    i = np.arange(128)
    triu = (i[:, None] <= i[None, :]).astype(np.float32)
    mneg = np.where(i[None, :] >= i[:, None], 0.0, -30000.0).astype(np.float32)
    cpack = np.concatenate([triu, mneg, np.ones((128, 128), np.float32)], axis=1)
    inv = (10000.0 ** (-np.arange(0, 32, 2, dtype=np.float32) / 32)).astype(np.float32)
    ang = (np.arange(Tn, dtype=np.float32)[:, None] * inv[None, :]).astype(np.float32)
    cos, sin = np.cos(ang).T.astype(np.float32), np.sin(ang).T.astype(np.float32)
    rope = np.zeros((2, 96, Tn), np.float32)
    rope[0, 0:64] = 1.0
    rope[0, 64:80] = cos
    rope[0, 80:96] = cos
    rope[1, 64:80] = -sin
    rope[1, 80:96] = sin
    return {"ident": np.eye(128, dtype=np.float32), "cpack": cpack, "rope": rope}


def build_program(Tn=T, n_sub=4):
    nc = bass.Bass("TRN2", target_bir_lowering=False)
    C = Ctx(nc)
    S = C.S
    x_d = nc.dram_tensor("x", [Tn, D], F32, kind="ExternalInput").ap()
    c_d = nc.dram_tensor("c", [D], F32, kind="ExternalInput").ap()
    Wd = {n: nc.dram_tensor(n, sh, F32, kind="ExternalInput").ap() for n, sh in W_SHAPES.items()}
    id_d = nc.dram_tensor("ident", [128, 128], F32, kind="ExternalInput").ap()
    cp_d = nc.dram_tensor("cpack", [128, 384], F32, kind="ExternalInput").ap()
    rope_d = nc.dram_tensor("rope", [2, 96, Tn], F32, kind="ExternalInput").ap()
    out_d = nc.dram_tensor("out", [Tn, D], F32, kind="ExternalOutput").ap()
    scr = [nc.dram_tensor(f"uscr{i}", [Tn, D], F32).ap() for i in range(2)]
    setup_consts(C, id_d, cp_d)
    vec = {n: C.sb("v" + n, [128, 1024], F32) for n in ["P1", "P2", "Q1", "Q2"]}
    vec["Q3b"] = C.sb("vQ3b", [128, 1024], BF16)
    u_in = x_d
    for i in range(n_sub):
        l, sub = i // 2, i % 2
        if i == 0:
            gin, bin_ = Wd["ln0_g"], Wd["ln0_b"]
        else:
            pl, ps = (i - 1) // 2, (i - 1) % 2
            gin, bin_ = Wd["post_ln_g"][pl, ps], Wd["post_ln_b"][pl, ps]
        modulation_vectors(C, c_d, Wd["ada_w"][l, sub], Wd["ada_b"][l, sub], gin, bin_, vec, f"mod{i}")
        u_out = scr[i % 2]
        if sub == 0:
            W = {"w_in": Wd["w_in"][l], "conv_w": Wd["ssd_conv_w"][l], "conv_b": Wd["ssd_conv_b"][l], "dt_bias": Wd["ssd_dt_bias"][l],
                 "a_log": Wd["ssd_a_log"][l], "ssd_d": Wd["ssd_d"][l], "ssd_norm_w": Wd["ssd_norm_w"][l], "q_norm": Wd["mla_q_norm"][l],
                 "w_qb": Wd["mla_w_qb"][l], "kv_norm": Wd["mla_kv_norm"][l], "w_kvb": Wd["mla_w_kvb"][l], "gm_ln_g": Wd["gm_ln_g"][l],
                 "gm_ln_b": Wd["gm_ln_b"][l], "gm_w_s": Wd["gm_w_s"][l], "gm_b_s": Wd["gm_b_s"][l], "w_out": Wd["w_out"][l], "rope": rope_d}
            mixer_sublayer(C, u_in, u_out, vec, W, Tn, name=f"mix{l}")
        elif l % 2 == 0:
            ffn_sublayer(C, u_in, u_out, vec, [(Wd["ffn_w1"][l // 2], Wd["ffn_w3"][l // 2], Wd["ffn_w2"][l // 2])], Tn, name=f"ffn{l}")
        else:
            m = l // 2
            experts = [(Wd["moe_w1"][m, e], Wd["moe_w3"][m, e], Wd["moe_w2"][m, e]) for e in range(NE)]
            ffn_sublayer(C, u_in, u_out, vec, experts, Tn, router_d=Wd["moe_router"][m], name=f"moe{l}")
        u_in = u_out
    pl, ps = (n_sub - 1) // 2, (n_sub - 1) % 2
    final_ln(C, u_in, out_d, Wd["post_ln_g"][pl, ps], Wd["post_ln_b"][pl, ps], Tn)
    S.finish()
    return nc, C


_CACHE = {}


def kernel(**inputs):
    x = np.asarray(inputs["x"], dtype=np.float32)
    B, Tn, _ = x.shape
    if Tn not in _CACHE:
        _CACHE[Tn] = build_program(Tn)
    nc, _ = _CACHE[Tn]
    consts = host_consts(Tn)
    shared = {n: np.ascontiguousarray(np.asarray(inputs[n], dtype=np.float32)) for n in W_SHAPES}
    shared.update(consts)
    c = np.asarray(inputs["c"], dtype=np.float32)
    in_maps = []
    for b in range(B):
        m = dict(shared)
        m["x"] = np.ascontiguousarray(x[b])
        m["c"] = np.ascontiguousarray(c[b])
        in_maps.append(m)
    res = run_bass_kernel_spmd(nc, in_maps, core_ids=list(range(B)))
    return np.stack([np.asarray(r["out"], dtype=np.float32) for r in res.results], axis=0)
```

```python
import math
from contextlib import ExitStack

import numpy as np
import concourse.bass as bass
import concourse.mybir as mybir
from concourse.bass_utils import run_bass_kernel_spmd

F32 = mybir.dt.float32
BF16 = mybir.dt.bfloat16
I32 = mybir.dt.int32
AF = mybir.ActivationFunctionType
ALU = mybir.AluOpType
AX = mybir.AxisListType

T = 4096
D = 1024
DFF = 2816
NE = 8
NCH = DFF // 128
DEPTH = 2
DN_ALPHA = (2 * DEPTH) ** 0.25
LN_EPS = 1e-5
RMS_EPS = 1e-6
D_IN = 2216


class Sched:
    NDSEM = 6

    def __init__(self, nc):
        self.nc = nc
        self.engs = {"pe": nc.tensor, "act": nc.scalar, "dve": nc.vector, "pool": nc.gpsimd, "sp": nc.sync}
        self.sem = {k: nc.alloc_semaphore("prog_" + k) for k in ("pe", "act", "dve", "pool")}
        self.cnt = {k: 0 for k in self.sem}
        self.seen = {k: {} for k in self.engs}
        self.lastw = {}
        self.rd = {}
        self.dsem = {}
        self.dcnt = {}
        self.dnext = {}
        for q in ("sp", "act", "pool"):
            for i in range(self.NDSEM):
                key = ("d", q, i)
                self.dsem[key] = nc.alloc_semaphore(f"dma_{q}{i}")
                self.dcnt[key] = 0
            self.dnext[q] = 0
        self.n_inst = 0
        self.pe_open = False
        self.hold = False

    def can_yield(self):
        return not (self.pe_open or self.hold)

    def _semh(self, key):
        return self.sem[key] if isinstance(key, str) else self.dsem[key]

    def wait(self, eng, ev):
        key, val = ev
        if val <= 0:
            return
        if key == eng and eng in ("pe",):
            return
        if self.seen[eng].get(key, 0) >= val:
            return
        self.engs[eng].wait_ge(self._semh(key), val)
        self.seen[eng][key] = val
        self.n_inst += 1

    def _deps(self, reads, writes):
        evs = []
        for r in reads:
            if r in self.lastw:
                evs.append(self.lastw[r])
        for w in writes:
            if w in self.lastw:
                evs.append(self.lastw[w])
            for k, v in self.rd.get(w, {}).items():
                evs.append((k, v))
        return evs

    def _record(self, ev, reads, writes):
        for r in reads:
            d = self.rd.setdefault(r, {})
            d[ev[0]] = max(d.get(ev[0], 0), ev[1])
        for w in writes:
            self.lastw[w] = ev
            self.rd[w] = {}

    PSUM_TOK = frozenset([f"b{i}" for i in range(8)] + ["bankb"])

    def op(self, eng, fn, reads=(), writes=(), inc=True):
        pr = [r for r in reads if r in self.PSUM_TOK]
        if pr:
            reads = [r for r in reads if r not in self.PSUM_TOK]
            writes = list(writes) + pr
        for ev in self._deps(reads, writes):
            self.wait(eng, ev)
        ins = fn()
        self.n_inst += 1
        if eng == "pe":
            self.pe_open = not inc
        if inc:
            ins.then_inc(self.sem[eng], 1)
            self.cnt[eng] += 1
            ev = (eng, self.cnt[eng])
        else:
            ev = (eng, self.cnt[eng] + 1)
        self._record(ev, reads, writes)
        return ins

    def dma(self, q, out, in_, reads=(), writes=()):
        i = self.dnext[q] % self.NDSEM
        self.dnext[q] += 1
        key = ("d", q, i)
        if self.dcnt[key] > 0:
            self.wait(q, (key, self.dcnt[key]))
        for ev in self._deps(reads, writes):
            self.wait(q, ev)
        ins = self.engs[q].dma_start(out=out, in_=in_)
        ins.then_inc(self.dsem[key], 16)
        self.n_inst += 1
        self.dcnt[key] += 16
        ev = (key, self.dcnt[key])
        self._record(ev, reads, writes)
        return ins

    def finish(self):
        for key, v in self.dcnt.items():
            if v > 0:
                self.wait("sp", (key, v))
        for k, v in self.cnt.items():
            self.wait("sp", (k, v))


class Ctx:
    def __init__(self, nc):
        self.nc = nc
        self.S = Sched(nc)
        self.stack = ExitStack()
        self.bank = [nc.alloc_psum_tensor(f"bank{i}", [128, 512], F32) for i in range(7)]
        self.bankb = nc.alloc_psum_tensor("bankb", [128, 1024], BF16)

    def chk(self, n):
        if getattr(self, "sstop", 99) <= n:
            raise StopIteration

    def sb(self, name, shape, dtype):
        return self.nc.alloc_sbuf_tensor("sb_" + name, list(shape), dtype)


def setup_consts(C, ident_f_d, cpack_d=None):
    nc, S = C.nc, C.S
    C.identf = C.sb("identf", [128, 128], F32)
    C.identb = C.sb("identb", [128, 128], BF16)
    S.dma("sp", C.identf[:], ident_f_d, writes=["identf"])
    if cpack_d is not None:
        C.cpack = C.sb("cpack", [128, 384], F32)
        S.dma("sp", C.cpack[:], cpack_d, writes=["cpack"])
        C.triu = C.cpack[:, 0:128]
        C.mneg = C.cpack[:, 128:256]
        C.ones = C.cpack[:, 256:384]
        C.triub = C.sb("triub", [128, 128], BF16)
        S.op("dve", lambda: nc.vector.tensor_copy(C.triub[:], C.triu), reads=["cpack"], writes=["triub"])
    C.eps_ln = C.sb("eps_ln", [128, 1], F32)
    C.eps_rms = C.sb("eps_rms", [128, 1], F32)
    S.op("pool", lambda: nc.gpsimd.memset(C.eps_ln[:], LN_EPS), writes=["eps_ln"])
    S.op("pool", lambda: nc.gpsimd.memset(C.eps_rms[:], RMS_EPS), writes=["eps_rms"])
    S.op("dve", lambda: nc.vector.tensor_copy(C.identb[:], C.identf[:]), reads=["identf"], writes=["identb"])


def ln_stats(C, u_ap, tok_u, mv, tok_mv, rstd, tok_rstd, st):
    nc, S = C.nc, C.S
    S.op("dve", lambda: nc.vector.bn_stats(st[:, 0, :], u_ap[:, 0:512]), reads=[tok_u], writes=["st0"])
    S.op("dve", lambda: nc.vector.bn_stats(st[:, 1, :], u_ap[:, 512:1024]), reads=[tok_u], writes=["st1"])
    S.op("dve", lambda: nc.vector.bn_aggr(mv, st[:].rearrange("p a b -> p (a b)")), reads=["st0", "st1"], writes=[tok_mv])
    S.op("act", lambda: nc.scalar.activation(out=rstd, in_=mv[:, 1:2], func=AF.Ln, bias=C.eps_ln[:, 0:1]),
         reads=[tok_mv, "eps_ln"], writes=[tok_rstd])
    S.op("act", lambda: nc.scalar.activation(out=rstd, in_=rstd, func=AF.Exp, scale=-0.5),
         reads=[tok_rstd], writes=[tok_rstd])


def barrier(C):
    S = C.S
    evs = [(k, v) for k, v in S.cnt.items()] + [(k, v) for k, v in S.dcnt.items()]
    for eng in ("pe", "act", "dve", "pool", "sp"):
        for ev in evs:
            S.wait(eng, ev)


def load_bcast(C, tile, dram_vec, tok):
    C.S.dma("sp", tile[:], dram_vec.partition_broadcast(128), writes=[tok])


def ln_front(C, ph, u_d, r0, vec, want_f32=False):
    nc, S = C.nc, C.S
    i = ph["i"] = ph.get("i", 0) + 1
    b = 0 if ph.get("single") else i % 2
    ut, xn, hb, hf = ph["ut"][b], ph["xn"][b], ph["hb"][b], ph["hf"]
    mv, rstd, st = ph["mv"][b], ph["rstd"][b], ph["st"]
    tx = f"ut{b}" if ph["xn"] is ph["ut"] else f"xn{b}"
    ph["tx"] = tx
    S.dma("sp", ut[:], u_d[r0:r0 + 128, :], writes=[f"ut{b}"])
    ln_stats(C, ut[:], f"ut{b}", mv[:], f"mv{b}", rstd[:], f"rstd{b}", st)
    S.op("dve", lambda: nc.vector.tensor_scalar(xn[:], ut[:], mv[:, 0:1], rstd[:, 0:1], ALU.subtract, ALU.mult),
         reads=[f"ut{b}", f"mv{b}", f"rstd{b}"], writes=[tx])
    if hf is None:
        S.op("pool", lambda: nc.gpsimd.tensor_tensor(hb[:], xn[:], vec["P1"][:], ALU.mult), reads=[tx, "P1"], writes=[f"hb{b}"])
        S.op("pool", lambda: nc.gpsimd.tensor_tensor(hb[:], hb[:], vec["P2"][:], ALU.add), reads=[f"hb{b}", "P2"], writes=[f"hb{b}"])
        return b
    S.op("pool", lambda: nc.gpsimd.tensor_tensor(hf[:], xn[:], vec["P1"][:], ALU.mult), reads=[tx, "P1"], writes=["hf"])
    if want_f32:
        S.op("dve", lambda: nc.vector.tensor_tensor(hf[:], hf[:], vec["P2"][:], ALU.add), reads=["hf", "P2"], writes=["hf"])
        S.op("act", lambda: nc.scalar.copy(hb[:], hf[:]), reads=["hf"], writes=[f"hb{b}"])
    else:
        S.op("pool", lambda: nc.gpsimd.tensor_tensor(hb[:], hf[:], vec["P2"][:], ALU.add), reads=["hf", "P2"], writes=[f"hb{b}"])
    return b


def transpose_to(C, src, src_tok, dst3, dst_tok, col0):
    nc, S = C.nc, C.S
    for k in range(8):
        S.op("pe", lambda: nc.tensor.transpose(C.bankb[:, k * 128:(k + 1) * 128], src[:, k * 128:(k + 1) * 128], C.identb[:]),
             reads=[src_tok, "identb"], writes=["bankb"], inc=(k == 7))
    S.op("act", lambda: nc.scalar.copy(dst3[:, :, col0:col0 + 128], C.bankb[:].rearrange("p (k t) -> p k t", k=8)),
         reads=["bankb"], writes=[dst_tok])


def ffn_sublayer(C, u_in_d, u_out_d, vec, experts, Tn, router_d=None, name="ffn", final=None):
    nc, S = C.nc, C.S
    HT = min(2048, Tn)
    NS = HT // 128
    moe = router_d is not None
    with ExitStack() as es:
        def sb(nm, shape, dt):
            return es.enter_context(nc.sbuf_tensor(f"{name}_{nm}", list(shape), dt))
        hT = sb("hT", [128, 8, HT], BF16)
        yacc = sb("yacc", [128, NS, 1024], F32)
        ph = {"ut": [sb(f"ut{i}", [128, 1024], F32) for i in range(2)],
              "xn": [sb(f"xn{i}", [128, 1024], F32) for i in range(2)],
              "hb": [sb(f"hb{i}", [128, 1024], BF16) for i in range(2)],
              "hf": sb("hf", [128, 1024], F32),
              "mv": [sb(f"mv{i}", [128, 2], F32) for i in range(2)],
              "rstd": [sb(f"rstd{i}", [128, 1], F32) for i in range(2)],
              "st": sb("st", [128, 2, 6], F32)}
        GC = 4
        groups = [list(range(c, min(c + GC, NCH))) for c in range(0, NCH, GC)]
        NWB = 2
        w1g = [sb(f"w1g{i}", [128, 8, GC * 128], BF16) for i in range(NWB)]
        w3g = [sb(f"w3g{i}", [128, 8, GC * 128], BF16) for i in range(NWB)]
        w2g = [sb(f"w2g{i}", [128, GC, 1024], BF16) for i in range(NWB)]
        NAB = 4
        sil = [sb(f"sil{i}", [128, 256], BF16) for i in range(NAB)]
        actT = [sb(f"actT{i}", [128, 256], BF16) for i in range(NAB)]
        if moe:
            hTf = sb("hTf", [128, 8, 128], F32)
            wr = sb("wr", [128, 8, NE], F32)
            lg = sb("lg", [128, NE], F32)
            m8 = sb("m8", [128, 8], F32)
            gts = sb("gts", [128, 2], F32)
            dlt = sb("dlt", [128, 2], F32)
            cm1 = sb("cm1", [128, NE], F32)
            comb = sb("comb", [128, NS, NE], F32)
            S.dma("sp", wr[:], router_d.rearrange("(k p) e -> p k e", p=128), writes=["wr"])
        abank = [C.bank[0], C.bank[1], C.bank[2]]
        accb = [C.bank[3], C.bank[4], C.bank[5], C.bank[6]]
        wld = 0
        abi = 0
        for half in range(Tn // HT):
            t0 = half * HT
            for s in range(NS):
                b = ln_front(C, ph, u_in_d, t0 + s * 128, vec, want_f32=moe)
                xn = ph["xn"][b]
                S.op("dve", lambda: nc.vector.tensor_tensor(yacc[:, s, :], xn[:], vec["Q1"][:], ALU.mult),
                     reads=[ph["tx"], "Q1"], writes=[f"yacc{s}"])
                S.op("pool", lambda: nc.gpsimd.tensor_tensor(yacc[:, s, :], yacc[:, s, :], vec["Q2"][:], ALU.add),
                     reads=[f"yacc{s}", "Q2"], writes=[f"yacc{s}"])
                transpose_to(C, ph["hb"][b], f"hb{b}", hT, f"hT{s // 2}", s * 128)
                if moe:
                    hf = ph["hf"]
                    for q in range(2):
                        for kk in range(4):
                            k = q * 4 + kk
                            S.op("pe", lambda: nc.tensor.transpose(C.bank[q][:, kk * 128:(kk + 1) * 128], hf[:, k * 128:(k + 1) * 128], C.identf[:]),
                                 reads=["hf", "identf"], writes=[f"b{q}"], inc=(kk == 3))
                        S.op("dve", lambda: nc.vector.tensor_copy(hTf[:, q * 4:(q + 1) * 4, :], C.bank[q][:].rearrange("p (k t) -> p k t", k=4)),
                             reads=[f"b{q}"], writes=["hTf"])
                    for k in range(8):
                        S.op("pe", lambda: nc.tensor.matmul(C.bank[2][:, 0:NE], hTf[:, k, :], wr[:, k, :], start=(k == 0), stop=(k == 7)),
                             reads=["hTf", "wr"], writes=["b2"], inc=(k == 7))
                    S.op("dve", lambda: nc.vector.tensor_copy(lg[:], C.bank[2][:, 0:NE]), reads=["b2"], writes=["lg"])
                    S.op("dve", lambda: nc.vector.max(m8[:], lg[:]), reads=["lg"], writes=["m8"])
                    S.op("dve", lambda: nc.vector.tensor_tensor(dlt[:, 0:1], m8[:, 0:1], m8[:, 1:2], ALU.subtract), reads=["m8"], writes=["dlt"])
                    S.op("dve", lambda: nc.vector.tensor_tensor(dlt[:, 1:2], m8[:, 1:2], m8[:, 0:1], ALU.subtract), reads=["m8", "dlt"], writes=["dlt"])
                    S.op("act", lambda: nc.scalar.activation(out=gts[:], in_=dlt[:], func=AF.Sigmoid), reads=["dlt"], writes=["gts"])
                    S.op("dve", lambda: nc.vector.tensor_scalar(cm1[:], lg[:], m8[:, 0:1], gts[:, 0:1], ALU.is_equal, ALU.mult),
                         reads=["lg", "m8", "gts"], writes=["cm1"])
                    S.op("dve", lambda: nc.vector.tensor_scalar(comb[:, s, :], lg[:], m8[:, 1:2], gts[:, 1:2], ALU.is_equal, ALU.mult),
                         reads=["lg", "m8", "gts"], writes=[f"comb{s}"])
                    S.op("dve", lambda: nc.vector.tensor_tensor(comb[:, s, :], comb[:, s, :], cm1[:], ALU.add),
                         reads=[f"comb{s}", "cm1"], writes=[f"comb{s}"])
            for e, (w1_d, w3_d, w2_d) in enumerate(experts):
                w1v = w1_d.rearrange("(k p) f -> p k f", p=128)
                w3v = w3_d.rearrange("(k p) f -> p k f", p=128)
                w2v = w2_d.rearrange("(c p) d -> p c d", p=128)
                for grp in groups:
                    wb = wld % NWB
                    wld += 1
                    ng = len(grp)
                    f0 = grp[0] * 128
                    S.dma("pool", w1g[wb][:, :, 0:ng * 128], w1v[:, :, f0:f0 + ng * 128], writes=[f"w1g{wb}"])
                    S.dma("pool", w3g[wb][:, :, 0:ng * 128], w3v[:, :, f0:f0 + ng * 128], writes=[f"w3g{wb}"])
                    S.dma("pool", w2g[wb][:, 0:ng, :], w2v[:, grp[0]:grp[0] + ng, :], writes=[f"w2g{wb}"])
                    for cc in range(ng):
                        S.op("pool", lambda: nc.gpsimd.tensor_tensor(w2g[wb][:, cc, :], w2g[wb][:, cc, :], vec["Q3b"][:], ALU.mult),
                             reads=[f"w2g{wb}", "Q3b"], writes=[f"w2g{wb}"])
                    seq = [(j, cc) for j in range(HT // 256) for cc in range(ng)]
                    SKEW = 2
                    st_ = {}

                    def up(j, cc):
                        nonlocal abi
                        c0 = j * 256
                        ab = abank[abi % 3]
                        abt = f"b{abi % 3}"
                        bf = abi % NAB
                        abi += 1
                        st_[(j, cc)] = bf
                        for k in range(8):
                            S.op("pe", lambda: nc.tensor.matmul(ab[:, 0:256], w1g[wb][:, k, cc * 128:(cc + 1) * 128], hT[:, k, c0:c0 + 256],
                                                                start=(k == 0), stop=(k == 7)),
                                 reads=[f"w1g{wb}", f"hT{j}"], writes=[abt], inc=False)
                        for k in range(8):
                            S.op("pe", lambda: nc.tensor.matmul(ab[:, 256:512], w3g[wb][:, k, cc * 128:(cc + 1) * 128], hT[:, k, c0:c0 + 256],
                                                                start=(k == 0), stop=(k == 7)),
                                 reads=[f"w3g{wb}", f"hT{j}"], writes=[abt], inc=(k == 7))
                        S.op("act", lambda: nc.scalar.activation(out=sil[bf][:], in_=ab[:, 0:256], func=AF.Silu),
                             reads=[abt], writes=[f"sil{bf}"])
                        S.op("dve", lambda: nc.vector.tensor_tensor(actT[bf][:], ab[:, 256:512], sil[bf][:], ALU.mult),
                             reads=[abt, f"sil{bf}"], writes=[f"actT{bf}"])

                    def down(j, cc):
                        bf = st_[(j, cc)]
                        for s2 in range(2):
                            for hlf in range(2):
                                bi = s2 * 2 + hlf
                                S.op("pe", lambda: nc.tensor.matmul(accb[bi][:, :], actT[bf][:, s2 * 128:(s2 + 1) * 128],
                                                                    w2g[wb][:, cc, hlf * 512:(hlf + 1) * 512],
                                                                    start=(cc == 0), stop=(cc == ng - 1)),
                                     reads=[f"actT{bf}", f"w2g{wb}"], writes=[f"b{3 + bi}"], inc=(cc == ng - 1 or bi == 3))
                        if cc == ng - 1:
                            for s2 in range(2):
                                s = j * 2 + s2
                                for hlf in range(2):
                                    bi = s2 * 2 + hlf
                                    ysl = yacc[:, s, hlf * 512:(hlf + 1) * 512]
                                    if moe:
                                        S.op("dve", lambda: nc.vector.scalar_tensor_tensor(ysl, accb[bi][:, :], comb[:, s, e:e + 1], ysl, ALU.mult, ALU.add),
                                             reads=[f"b{3 + bi}", f"comb{s}", f"yacc{s}"], writes=[f"yacc{s}"])
                                    else:
                                        S.op("dve", lambda: nc.vector.tensor_tensor(ysl, accb[bi][:, :], ysl, ALU.add),
                                             reads=[f"b{3 + bi}", f"yacc{s}"], writes=[f"yacc{s}"])

                    for idx in range(len(seq) + SKEW):
                        if idx < len(seq):
                            up(*seq[idx])
                        if idx >= SKEW:
                            down(*seq[idx - SKEW])
            for s in range(NS):
                if final is not None:
                    fg, fb = final
                    b = s % 2
                    ln_stats(C, yacc[:, s, :], f"yacc{s}", ph["mv"][b][:], f"mv{b}", ph["rstd"][b][:], f"rstd{b}", ph["st"])
                    S.op("dve", lambda: nc.vector.tensor_scalar(yacc[:, s, :], yacc[:, s, :], ph["mv"][b][:, 0:1], ph["rstd"][b][:, 0:1], ALU.subtract, ALU.mult),
                         reads=[f"yacc{s}", f"mv{b}", f"rstd{b}"], writes=[f"yacc{s}"])
                    S.op("pool", lambda: nc.gpsimd.tensor_tensor(yacc[:, s, :], yacc[:, s, :], fg[:], ALU.mult), reads=[f"yacc{s}", "fing"], writes=[f"yacc{s}"])
                    S.op("dve", lambda: nc.vector.tensor_tensor(yacc[:, s, :], yacc[:, s, :], fb[:], ALU.add), reads=[f"yacc{s}", "finb"], writes=[f"yacc{s}"])
                S.dma("sp", u_out_d[t0 + s * 128:t0 + (s + 1) * 128, :], yacc[:, s, :], reads=[f"yacc{s}"])
        barrier(C)


MLA_SCALE = 96 ** -0.5


def mixer_sublayer(C, u_in_d, u_out_d, vec, W, Tn, name="mix", TQ=256):
    nc, S = C.nc, C.S
    V_, A_, G_, P_ = nc.vector, nc.scalar, nc.gpsimd, nc.tensor
    NT = Tn // TQ
    NSUB = TQ // 128
    NKT = Tn // 128

    def dve(fn, r=(), w=()): return S.op("dve", fn, r, w)
    def act(fn, r=(), w=()): return S.op("act", fn, r, w)
    def pool(fn, r=(), w=()): return S.op("pool", fn, r, w)
    def pe(fn, r=(), w=(), inc=True): return S.op("pe", fn, r, w, inc=inc)

    rb = {"i": 0}

    def nextbank():
        i = rb["i"] % 3
        rb["i"] += 1
        return C.bank[i], f"b{i}"

    fb = {"i": 0}

    def fbank():
        i = (0, 1)[fb["i"] % 2]
        fb["i"] += 1
        return C.bank[i], f"b{i}"

    bb = {"i": 0}

    def bbank():
        i = (2, 5)[bb["i"] % 2]
        bb["i"] += 1
        return C.bank[i], f"b{i}"

    with ExitStack() as es:
        def sb(nm, shape, dt):
            return es.enter_context(nc.sbuf_tensor(f"{name}_{nm}", list(shape), dt))

        win = sb("win", [128, 8, D_IN], BF16)
        w_in_v = W["w_in"].rearrange("(k p) e -> p k e", p=128)
        for k in range(8):
            S.dma("pool", win[:, k, :], w_in_v[:, k, :], writes=[f"win{k}"])
        WIN = [f"win{k}" for k in range(8)]
        wkA = sb("wkA", [128, 8, 96], BF16)
        wkB = sb("wkB", [128, 8, 96], BF16)
        pool(lambda: G_.memset(wkA[:], 0.0), w=["wkA"])
        pool(lambda: G_.memset(wkB[:], 0.0), w=["wkB"])
        pool(lambda: G_.tensor_copy(wkA[:, :, 64:96], win[:, :, 1672:1704]), r=WIN, w=["wkA"])
        pool(lambda: G_.tensor_copy(wkB[:, :, 64:80], win[:, :, 1688:1704]), r=WIN, w=["wkB"])
        pool(lambda: G_.tensor_copy(wkB[:, :, 80:96], win[:, :, 1672:1688]), r=WIN, w=["wkB"])
        wout = sb("wout", [128, 8, 1024], BF16)
        w_out_v = W["w_out"].rearrange("(k p) e -> p k e", p=128)
        for k in range(8):
            S.dma("pool", wout[:, k, :], w_out_v[:, k, :], writes=[f"wout{k}"])
            pool(lambda: G_.tensor_tensor(wout[:, k, :], wout[:, k, :], vec["Q3b"][:], ALU.mult), r=[f"wout{k}", "Q3b"], w=[f"wout{k}"])
        WOUT = [f"wout{k}" for k in range(8)]
        rest = sb("rest", [128, 1024], F32)
        tmpw = rest[:].rearrange("p (a b) -> p a b", a=2)
        qn = sb("qn", [128, 2], F32)
        kvn_g = sb("kvn_g", [128, 1], F32)
        with nc.allow_non_contiguous_dma(reason="tiny param vectors"):
            S.dma("sp", qn[:], W["q_norm"].rearrange("(b p) -> p b", p=128), writes=["qn"])
            S.dma("sp", kvn_g[:], W["kv_norm"].rearrange("(p o) -> p o", o=1), writes=["kvn_g"])
        S.dma("sp", tmpw[:, :, 0:384], W["w_qb"].rearrange("(b p) e -> p b e", p=128), writes=["rest"])
        wqb = sb("wqb", [128, 2, 384], BF16)
        wqs = sb("wqs", [128, 2, 384], BF16)
        for b in range(2):
            dve(lambda: V_.tensor_scalar(wqb[:, b, :], tmpw[:, b, 0:384], qn[:, b:b + 1], None, ALU.mult), r=["rest", "qn"], w=["wqb"])
        pool(lambda: G_.memset(wqs[:], 0.0), w=["wqs"])
        for hd in range(4):
            o = hd * 96
            pool(lambda: G_.tensor_copy(wqs[:, :, o + 64:o + 80], wqb[:, :, o + 80:o + 96]), r=["wqb"], w=["wqs"])
            pool(lambda: G_.tensor_copy(wqs[:, :, o + 80:o + 96], wqb[:, :, o + 64:o + 80]), r=["wqb"], w=["wqs"])
        S.dma("sp", tmpw[:, 0, :], W["w_kvb"], reads=["wqb"], writes=["rest"])
        wkv = sb("wkv", [128, 512], BF16)
        dve(lambda: V_.tensor_scalar(wkv[:], tmpw[:, 0, :], kvn_g[:, 0:1], None, ALU.mult), r=["rest", "kvn_g"], w=["wkv"])
        wkv4 = wkv[:].rearrange("p (h c) -> p h c", c=128)
        cwr = sb("cwr", [5, 768], F32)
        S.dma("sp", cwr[0:4, :], W["conv_w"], writes=["cwr"])
        S.dma("sp", cwr[4:5, :], W["conv_b"].rearrange("(o c) -> o c", o=1), writes=["cwr"])
        cw = sb("cw", [128, 6, 5], F32)
        bk, bt = nextbank()
        for blk in range(6):
            pe(lambda: P_.transpose(bk[:, blk * 5:blk * 5 + 5], cwr[0:5, blk * 128:(blk + 1) * 128], C.identf[0:5, 0:5]),
               r=["cwr", "identf"], w=[bt], inc=(blk == 5))
        dve(lambda: V_.tensor_copy(cw[:].rearrange("p a b -> p (a b)"), bk[:, 0:30]), r=[bt], w=["cw"])
        Abc = sb("Abc", [128, 8], F32)
        dtb = sb("dtb", [128, 8], F32)
        Dbc = sb("Dbc", [128, 8], F32)
        load_bcast(C, Abc, W["a_log"], "Abc")
        load_bcast(C, dtb, W["dt_bias"], "dtb")
        load_bcast(C, Dbc, W["ssd_d"], "Dbc")
        act(lambda: A_.activation(out=Abc[:], in_=Abc[:], func=AF.Exp), r=["Abc"], w=["Abc"])
        dve(lambda: V_.tensor_scalar(Abc[:], Abc[:], -1.0, None, ALU.mult), r=["Abc"], w=["Abc"])
        nw = sb("nw", [128, 512], F32)
        load_bcast(C, nw, W["ssd_norm_w"], "nw")
        gmg = sb("gmg", [128, 256], F32)
        gmb = sb("gmb", [128, 256], F32)
        load_bcast(C, gmg, W["gm_ln_g"], "gmg")
        load_bcast(C, gmb, W["gm_ln_b"], "gmb")
        bs = sb("bs", [128, 4], F32)
        with nc.allow_non_contiguous_dma(reason="tiny param vectors"):
            S.dma("sp", bs[:], W["gm_b_s"].rearrange("g t -> t g"), writes=["bs"])
        WT = sb("WT", [128, 4, 128], BF16)
        for g in range(4):
            S.dma("sp", tmpw[:, 1, 0:128], W["gm_w_s"][g], writes=["rest"])
            bk, bt = nextbank()
            pe(lambda: P_.transpose(bk[:, 0:128], tmpw[:, 1, 0:128], C.identf[:]), r=["rest", "identf"], w=[bt])
            dve(lambda: V_.tensor_tensor(WT[:, g, :], bk[:, 0:128], C.triu[:], ALU.mult), r=[bt, "cpack"], w=["WT"])

        KT = [sb(f"KT{hd}", [96, Tn], BF16) for hd in range(4)]
        Vt = sb("Vt", [128, NKT, 4, 65], BF16)
        pool(lambda: G_.memset(Vt[:, :, :, 64:65], 1.0), w=["Vones"])
        Srun = sb("Srun", [128, 4, 64], F32)
        Sbf = sb("Sbf", [128, 4, 64], BF16)
        pool(lambda: G_.memset(Srun[:], 0.0), w=["Srun"])
        pool(lambda: G_.memset(Sbf[:], 0.0), w=["Sbf"])
        halo = sb("halo", [128, 6, 3], F32)
        pool(lambda: G_.memset(halo[:], 0.0), w=["halo"])
        xct = [sb(f"xct{i}", [128, 3 + TQ], F32) for i in range(2)]

        _ut = sb("ut0", [128, 1024], F32)
        _hb = sb("hb0", [128, 1024], BF16)
        ph = {"ut": [_ut, _ut], "hb": [_hb, _hb], "hf": None, "single": True,
              "mv": [sb(f"mv{i}", [128, 2], F32) for i in range(2)],
              "rstd": [sb(f"rstd{i}", [128, 1], F32) for i in range(2)],
              "st": sb("st", [128, 2, 6], F32)}
        ph["xn"] = ph["ut"]
        hT = sb("hT", [128, 8, TQ], BF16)
        zsD = [sb(f"zs{i}", [128, NSUB, 512], BF16) for i in range(2)]
        ggD = [sb(f"gg{i}", [128, NSUB, 512], BF16) for i in range(2)]
        dtrD = [sb(f"dtr{i}", [128, NSUB, 8], F32) for i in range(2)]
        xaD = [sb(f"xa{i}", [128, 6, TQ], BF16) for i in range(2)]
        cacc = sb("cacc", [128, TQ], F32)
        qlT = sb("qlT", [128, 2, TQ], BF16)
        sqT = sb("sqT", [128, 2, TQ], BF16)
        rq = sb("rq", [128, TQ], F32)
        rkv = sb("rkv", [128, TQ], F32)
        kvnT = sb("kvnT", [128, TQ], BF16)
        QTD = [sb(f"QT{i}", [96, 4, TQ], BF16) for i in range(2)]
        ccs = sb("ccs", [96, 2, TQ], F32)
        qt1 = sb("qt1", [96, TQ], F32)
        qt2 = sb("qt2", [96, TQ], F32)
        NPT = 3
        pt = [sb(f"pt{i}", [128, TQ], BF16) for i in range(NPT)]
        rcp = sb("rcp", [128, NSUB], F32)
        ycat = [sb(f"ycat{i}", [128, 1024], BF16) for i in range(NSUB)]
        yT = sb("yT", [128, 8, 128], BF16)
        dif = sb("dif", [128, 8, 128], F32)
        axT = dif
        eA = sb("eA", [128, 8, 128], F32)
        Mt = sb("Mt", [128, 8, 128], BF16)
        CsT = sb("CsT", [128, 4, 128], BF16)
        xtok = sb("xtok", [128, 512], BF16)
        xdt = sb("xdt", [128, 512], BF16)
        xdw = sb("xdw", [128, 512], BF16)
        Btok = sb("Btok", [128, 128], BF16)
        dts = sb("dts", [128, 8], F32)
        t8a = sb("t8a", [128, 8], F32)
        t8b = sb("t8b", [128, 8], F32)
        av = sb("av", [128, 8], F32)
        acs = sb("acs", [128, 8], F32)
        dend = sb("dend", [128, 8], F32)
        eAl = sb("eAl", [128, 8], F32)
        ys = sb("ys", [128, 512], F32)
        gst = sb("gst", [128, 2, 6], F32)
        gmv = sb("gmv", [128, 2, 2], F32)
        grs = sb("grs", [128, 2], F32)
        vnb = sb("vnb", [128, 256], BF16)
        vnf = sb("vnf", [128, 256], F32)
        outt = rest
        ones_b = sb("ones_b", [128, 128], BF16)
        dve(lambda: V_.tensor_copy(ones_b[:], C.ones[:]), r=["cpack"], w=["ones_b"])
        print("mixer sbuf bytes remaining", nc.sbuf_bytes_remaining)

        def front(j):
            par = j % 2
            zs, gg, dtr, xa, QT = zsD[par], ggD[par], dtrD[par], xaD[par], QTD[par]
            T0 = j * TQ
            if S.can_yield(): yield
            S.dma("sp", ccs[:, 0, :], W["rope"][0, :, T0:T0 + TQ], writes=["cc"])
            if S.can_yield(): yield
            S.dma("sp", ccs[:, 1, :], W["rope"][1, :, T0:T0 + TQ], writes=["ss"])
            for s in range(NSUB):
                if S.can_yield(): yield
                b = ln_front(C, ph, u_in_d, T0 + s * 128, vec)
                xn = ph["xn"][b]
                if S.can_yield(): yield
                dve(lambda: V_.tensor_tensor(rest[:], xn[:], vec["Q1"][:], ALU.mult), r=[ph["tx"], "Q1"], w=["rest"])
                if S.can_yield(): yield
                pool(lambda: G_.tensor_tensor(rest[:], rest[:], vec["Q2"][:], ALU.add), r=["rest", "Q2"], w=["rest"])
                if S.can_yield(): yield
                S.dma("sp", u_out_d[T0 + s * 128:T0 + (s + 1) * 128, :], rest[:], reads=["rest"], writes=[f"uo{j}_{s}"])
                if S.can_yield(): yield
                transpose_to(C, ph["hb"][b], f"hb{b}", hT, "hT", s * 128)
            for s in range(NSUB):
                lh = lambda k: hT[:, k, s * 128:(s + 1) * 128]
                bk, bt = fbank()
                for k in range(8):
                    if S.can_yield(): yield
                    pe(lambda: P_.matmul(bk[:, :], lh(k), win[:, k, 1704:2216], start=(k == 0), stop=(k == 7)), r=["hT", WIN[k]], w=[bt], inc=(k == 7))
                if S.can_yield(): yield
                act(lambda: A_.activation(out=gg[:, s, :], in_=bk[:, :], func=AF.Gelu), r=[bt], w=[f"gg{par}_{s}"])
            for s in range(NSUB):
                lh = lambda k: hT[:, k, s * 128:(s + 1) * 128]
                bk, bt = fbank()
                for k in range(8):
                    if S.can_yield(): yield
                    pe(lambda: P_.matmul(bk[:, 0:8], lh(k), win[:, k, 1280:1288], start=(k == 0), stop=(k == 7)), r=["hT", WIN[k]], w=[bt], inc=(k == 7))
                if S.can_yield(): yield
                dve(lambda: V_.tensor_tensor(dtr[:, s, :], bk[:, 0:8], dtb[:], ALU.add), r=[bt, "dtb"], w=[f"dtr{par}_{s}"])
            for s in range(NSUB):
                lh = lambda k: hT[:, k, s * 128:(s + 1) * 128]
                bk, bt = fbank()
                for k in range(8):
                    if S.can_yield(): yield
                    pe(lambda: P_.matmul(bk[:, :], lh(k), win[:, k, 0:512], start=(k == 0), stop=(k == 7)), r=["hT", WIN[k]], w=[bt], inc=(k == 7))
                if S.can_yield(): yield
                for hz in range(512 // TQ):
                    zsl = slice(hz * TQ, (hz + 1) * TQ)
                    if S.can_yield(): yield
                    act(lambda: A_.activation(out=cacc[:], in_=bk[:, zsl], func=AF.Exp, scale=-1.0), r=[bt], w=["cacc"])
                    if S.can_yield(): yield
                    act(lambda: A_.activation(out=cacc[:], in_=cacc[:], func=AF.Ln, bias=C.ones[:, 0:1]), r=["cacc", "cpack"], w=["cacc"])
                    act(lambda: A_.activation(out=cacc[:], in_=cacc[:], func=AF.Exp, scale=-1.0), r=["cacc"], w=["cacc"])
                    dve(lambda: V_.tensor_tensor(zs[:, s, zsl], bk[:, zsl], cacc[:], ALU.mult), r=[bt, "cacc"], w=[f"zs{par}_{s}"])
            for blk in range(6):
                c0 = 512 + blk * 128
                bk, bt = fbank()
                for k in range(8):
                    if S.can_yield(): yield
                    pe(lambda: P_.matmul(bk[:, 0:TQ], win[:, k, c0:c0 + 128], hT[:, k, :], start=(k == 0), stop=(k == 7)), r=["hT", WIN[k]], w=[bt], inc=(k == 7))
                xb = xct[blk % 2]
                xbt = f"xct{blk % 2}"
                if S.can_yield(): yield
                act(lambda: A_.copy(xb[:, 3:3 + TQ], bk[:, 0:TQ]), r=[bt], w=[xbt])
                if S.can_yield(): yield
                pool(lambda: G_.tensor_copy(xb[:, 0:3], halo[:, blk, :]), r=["halo"], w=[xbt])
                if S.can_yield(): yield
                dve(lambda: V_.tensor_scalar(cacc[:], xb[:, 3:3 + TQ], cw[:, blk, 3:4], cw[:, blk, 4:5], ALU.mult, ALU.add),
                    r=[xbt, "cw"], w=["cacc"])
                for kk in range(3):
                    if S.can_yield(): yield
                    dve(lambda: V_.scalar_tensor_tensor(cacc[:], xb[:, kk:kk + TQ], cw[:, blk, kk:kk + 1], cacc[:], ALU.mult, ALU.add),
                        r=[xbt, "cw", "cacc"], w=["cacc"])
                if S.can_yield(): yield
                pool(lambda: G_.tensor_copy(halo[:, blk, :], xb[:, TQ:TQ + 3]), r=[xbt], w=["halo"])
                if S.can_yield(): yield
                act(lambda: A_.activation(out=xb[:, 0:TQ], in_=cacc[:], func=AF.Exp, scale=-1.0), r=["cacc", "halo"], w=[xbt])
                if S.can_yield(): yield
                act(lambda: A_.activation(out=xb[:, 0:TQ], in_=xb[:, 0:TQ], func=AF.Ln, bias=C.ones[:, 0:1]), r=[xbt, "cpack"], w=[xbt])
                act(lambda: A_.activation(out=xb[:, 0:TQ], in_=xb[:, 0:TQ], func=AF.Exp, scale=-1.0), r=[xbt], w=[xbt])
                dve(lambda: V_.tensor_tensor(xa[:, blk, :], cacc[:], xb[:, 0:TQ], ALU.mult), r=["cacc", xbt], w=[f"xa{par}_{blk}"])
            for b2 in range(2):
                c0 = 1288 + b2 * 128
                bk, bt = fbank()
                for k in range(8):
                    if S.can_yield(): yield
                    pe(lambda: P_.matmul(bk[:, 0:TQ], win[:, k, c0:c0 + 128], hT[:, k, :], start=(k == 0), stop=(k == 7)), r=["hT", WIN[k]], w=[bt], inc=(k == 7))
                if S.can_yield(): yield
                act(lambda: A_.copy(qlT[:, b2, :], bk[:, 0:TQ]), r=[bt], w=["qlT"])
                if S.can_yield(): yield
                act(lambda: A_.activation(out=sqT[:, b2, :], in_=bk[:, 0:TQ], func=AF.Square), r=[bt], w=["sqT"])
            bk, bt = fbank()
            for b2 in range(2):
                if S.can_yield(): yield
                pe(lambda: P_.matmul(bk[:, 0:TQ], ones_b[:], sqT[:, b2, :], start=(b2 == 0), stop=(b2 == 1)), r=["ones_b", "sqT"], w=[bt], inc=(b2 == 1))
            if S.can_yield(): yield
            act(lambda: A_.activation(out=rq[:], in_=bk[:, 0:TQ], func=AF.Ln, scale=1.0 / 256, bias=C.eps_rms[:, 0:1]), r=[bt, "eps_rms"], w=["rq"])
            if S.can_yield(): yield
            act(lambda: A_.activation(out=rq[:], in_=rq[:], func=AF.Exp, scale=-0.5), r=["rq"], w=["rq"])
            bk, bt = fbank()
            for k in range(8):
                if S.can_yield(): yield
                pe(lambda: P_.matmul(bk[:, 0:TQ], win[:, k, 1544:1672], hT[:, k, :], start=(k == 0), stop=(k == 7)), r=["hT", WIN[k]], w=[bt], inc=(k == 7))
            if S.can_yield(): yield
            act(lambda: A_.activation(out=sqT[:, 0, :], in_=bk[:, 0:TQ], func=AF.Square), r=[bt], w=["sqT"])
            bk2, bt2 = fbank()
            if S.can_yield(): yield
            pe(lambda: P_.matmul(bk2[:, 0:TQ], ones_b[:], sqT[:, 0, :], start=True, stop=True), r=["ones_b", "sqT"], w=[bt2])
            if S.can_yield(): yield
            act(lambda: A_.activation(out=rkv[:], in_=bk2[:, 0:TQ], func=AF.Ln, scale=1.0 / 128, bias=C.eps_rms[:, 0:1]), r=[bt2, "eps_rms"], w=["rkv"])
            if S.can_yield(): yield
            act(lambda: A_.activation(out=rkv[:], in_=rkv[:], func=AF.Exp, scale=-0.5), r=["rkv"], w=["rkv"])
            if S.can_yield(): yield
            dve(lambda: V_.tensor_tensor(kvnT[:], bk[:, 0:TQ], rkv[:], ALU.mult), r=[bt, "rkv"], w=["kvnT"])
            bk, bt = fbank()
            for k in range(8):
                if S.can_yield(): yield
                pe(lambda: P_.matmul(bk[0:96, 0:TQ], wkA[:, k, :], hT[:, k, :], start=(k == 0), stop=(k == 7)), r=["hT", "wkA"], w=[bt], inc=(k == 7))
            bk2, bt2 = fbank()
            for k in range(8):
                if S.can_yield(): yield
                pe(lambda: P_.matmul(bk2[0:96, 0:TQ], wkB[:, k, :], hT[:, k, :], start=(k == 0), stop=(k == 7)), r=["hT", "wkB"], w=[bt2], inc=(k == 7))
            if S.can_yield(): yield
            dve(lambda: V_.tensor_tensor(qt1[64:96, :], bk[64:96, 0:TQ], ccs[64:96, 0, :], ALU.mult), r=[bt, "cc"], w=["qt1"])
            if S.can_yield(): yield
            dve(lambda: V_.tensor_tensor(qt2[64:96, :], bk2[64:96, 0:TQ], ccs[64:96, 1, :], ALU.mult), r=[bt2, "ss"], w=["qt2"])
            for hd in range(4):
                if S.can_yield(): yield
                pool(lambda: G_.tensor_tensor(KT[hd][64:96, T0:T0 + TQ], qt1[64:96, :], qt2[64:96, :], ALU.add), r=["qt1", "qt2"], w=[f"KT{hd}_{j}"])
            for hd in range(4):
                bk, bt = fbank()
                if S.can_yield(): yield
                pe(lambda: P_.matmul(bk[0:64, 0:TQ], wkv[:, hd * 128:hd * 128 + 64], kvnT[:], start=True, stop=True), r=["wkv", "kvnT"], w=[bt])
                if S.can_yield(): yield
                act(lambda: A_.copy(KT[hd][0:64, T0:T0 + TQ], bk[0:64, 0:TQ]), r=[bt], w=[f"KT{hd}_{j}"])
            for s in range(NSUB):
                bk, bt = fbank()
                for hd in range(4):
                    if S.can_yield(): yield
                    pe(lambda: P_.matmul(bk[:, hd * 64:(hd + 1) * 64], kvnT[:, s * 128:(s + 1) * 128], wkv[:, hd * 128 + 64:hd * 128 + 128], start=True, stop=True),
                       r=["wkv", "kvnT"], w=[bt], inc=(hd == 3))
                if S.can_yield(): yield
                act(lambda: A_.copy(Vt[:, j * NSUB + s, :, 0:64], bk[:, 0:256].rearrange("p (h c) -> p h c", c=64)), r=[bt], w=[f"V{j * NSUB + s}"])
            for hd in range(4):
                bk, bt = fbank()
                for b2 in range(2):
                    if S.can_yield(): yield
                    pe(lambda: P_.matmul(bk[0:96, 0:TQ], wqb[:, b2, hd * 96:(hd + 1) * 96], qlT[:, b2, :], start=(b2 == 0), stop=(b2 == 1)), r=["wqb", "qlT"], w=[bt], inc=(b2 == 1))
                bk2, bt2 = fbank()
                for b2 in range(2):
                    if S.can_yield(): yield
                    pe(lambda: P_.matmul(bk2[0:96, 0:TQ], wqs[:, b2, hd * 96:(hd + 1) * 96], qlT[:, b2, :], start=(b2 == 0), stop=(b2 == 1)), r=["wqs", "qlT"], w=[bt2], inc=(b2 == 1))
                if S.can_yield(): yield
                dve(lambda: V_.tensor_tensor(qt1[:], bk[0:96, 0:TQ], ccs[:, 0, :], ALU.mult), r=[bt, "cc"], w=["qt1"])
                if S.can_yield(): yield
                dve(lambda: V_.tensor_tensor(qt2[:], bk2[0:96, 0:TQ], ccs[:, 1, :], ALU.mult), r=[bt2, "ss"], w=["qt2"])
                if S.can_yield(): yield
                pool(lambda: G_.tensor_tensor(qt1[:], qt1[:], qt2[:], ALU.add), r=["qt1", "qt2"], w=["qt1"])
                if S.can_yield(): yield
                dve(lambda: V_.tensor_tensor(QT[:, hd, :], qt1[:], rq[0:96, :], ALU.mult), r=["qt1", "rq"], w=[f"QT{par}_{hd}"])


        def back(j):
            par = j % 2
            zs, gg, dtr, xa, QT = zsD[par], ggD[par], dtrD[par], xaD[par], QTD[par]
            T0 = j * TQ
            nkt = NSUB * (j + 1)
            sv = {}
            def ssd1(s):
                ch = j * NSUB + s
                csl = slice(s * 128, (s + 1) * 128)
                if S.can_yield(): yield
                dve(lambda: V_.tensor_scalar(t8a[:], dtr[:, s, :], -1.0, None, ALU.mult), r=[f"dtr{par}_{s}"], w=["t8a"])
                if S.can_yield(): yield
                dve(lambda: V_.tensor_tensor(t8a[:], t8a[:], dtr[:, s, :], ALU.max), r=["t8a", f"dtr{par}_{s}"], w=["t8a"])
                if S.can_yield(): yield
                act(lambda: A_.activation(out=t8a[:], in_=t8a[:], func=AF.Exp, scale=-1.0), r=["t8a"], w=["t8a"])
                if S.can_yield(): yield
                act(lambda: A_.activation(out=t8a[:], in_=t8a[:], func=AF.Ln, bias=C.ones[:, 0:1]), r=["t8a", "cpack"], w=["t8a"])
                if S.can_yield(): yield
                dve(lambda: V_.tensor_scalar(t8b[:], dtr[:, s, :], 0.0, None, ALU.max), r=[f"dtr{par}_{s}"], w=["t8b"])
                if S.can_yield(): yield
                dve(lambda: V_.tensor_tensor(dts[:], t8a[:], t8b[:], ALU.add), r=["t8a", "t8b"], w=["dts"])
                if S.can_yield(): yield
                dve(lambda: V_.tensor_tensor(av[:], dts[:], Abc[:], ALU.mult), r=["dts", "Abc"], w=["av"])
                bk, bt = bbank()
                if S.can_yield(): yield
                pe(lambda: P_.matmul(bk[:, 0:8], C.triu[:], av[:], start=True, stop=True), r=["cpack", "av"], w=[bt], inc=False)
                if S.can_yield(): yield
                pe(lambda: P_.matmul(bk[:, 8:16], C.ones[:], av[:], start=True, stop=True), r=["cpack", "av"], w=[bt])
                if S.can_yield(): yield
                dve(lambda: V_.tensor_copy(acs[:], bk[:, 0:8]), r=[bt], w=["acs"])
                if S.can_yield(): yield
                dve(lambda: V_.tensor_tensor(dend[:], bk[:, 8:16], acs[:], ALU.subtract), r=[bt, "acs"], w=["dend"])
                if S.can_yield(): yield
                act(lambda: A_.activation(out=dend[:], in_=dend[:], func=AF.Exp), r=["dend"], w=["dend"])
                if S.can_yield(): yield
                act(lambda: A_.activation(out=eAl[:], in_=bk[:, 8:16], func=AF.Exp), r=[bt], w=["eAl"])
                if S.can_yield(): yield
                dve(lambda: V_.tensor_tensor(axT[:], C.triu[:].unsqueeze(1).broadcast_to([128, 8, 128]), av[:].unsqueeze(2).broadcast_to([128, 8, 128]), ALU.mult),
                    r=["cpack", "av"], w=["dif0", "dif1"])

            def ssd2(s):
                ch = j * NSUB + s
                csl = slice(s * 128, (s + 1) * 128)
                bkA, btA = bbank()
                bkB, btB = bbank()
                if S.can_yield(): yield
                pe(lambda: P_.matmul(bkA[:, :], C.ones[:], axT[:, 0:4, :].rearrange("p a b -> p (a b)"), start=True, stop=True), r=["cpack", "dif0"], w=[btA])
                if S.can_yield(): yield
                pe(lambda: P_.matmul(bkB[:, :], C.ones[:], axT[:, 4:8, :].rearrange("p a b -> p (a b)"), start=True, stop=True), r=["cpack", "dif1"], w=[btB])
                for hh, (bkx, btx) in enumerate(((bkA, btA), (bkB, btB))):
                    d2 = dif[:, hh * 4:hh * 4 + 4, :].rearrange("p a b -> p (a b)")
                    e2 = eA[:, hh * 4:hh * 4 + 4, :].rearrange("p a b -> p (a b)")
                    if S.can_yield(): yield
                    act(lambda: A_.activation(out=e2, in_=bkx[:, :], func=AF.Exp), r=[btx], w=[f"eA{hh}"])
                    hs = slice(hh * 4, hh * 4 + 4)
                    if S.can_yield(): yield
                    dve(lambda: V_.tensor_tensor(dif[:, hs, :], bkx[:, :].rearrange("p (a b) -> p a b", a=4), acs[:, hs].unsqueeze(2).broadcast_to([128, 4, 128]), ALU.subtract),
                        r=[btx, "acs"], w=[f"dif{hh}"])
                    if S.can_yield(): yield
                    pool(lambda: G_.tensor_tensor(dif[:, hs, :], dif[:, hs, :], C.mneg[:].unsqueeze(1).broadcast_to([128, 4, 128]), ALU.add),
                         r=[f"dif{hh}", "cpack"], w=[f"dif{hh}"])
                    if S.can_yield(): yield
                    act(lambda: A_.activation(out=d2, in_=d2, func=AF.Exp), r=[f"dif{hh}"], w=[f"dif{hh}"])
                cbk = [bbank(), bbank()]
                for g in range(2):
                    gs = slice(g * 64, (g + 1) * 64)
                    if S.can_yield(): yield
                    pe(lambda: P_.matmul(cbk[g][0][:, 0:128], xa[gs, 4, csl], xa[gs, 5, csl], start=True, stop=True), r=[f"xa{par}_4", f"xa{par}_5"], w=[cbk[g][1]])
                for g in range(2):
                    gs = slice(g * 64, (g + 1) * 64)
                    if S.can_yield(): yield
                    dve(lambda: V_.tensor_tensor(Mt[:, g * 4:(g + 1) * 4, :], cbk[g][0][:, 0:128].unsqueeze(1).broadcast_to([128, 4, 128]), dif[:, g * 4:(g + 1) * 4, :], ALU.mult),
                        r=[cbk[g][1], f"dif{g}"], w=[f"Mt{g}"])
                    if S.can_yield(): yield
                    dve(lambda: V_.tensor_tensor(CsT[gs, :, :], xa[gs, 5, csl].unsqueeze(1).broadcast_to([64, 4, 128]), eA[gs, g * 4:(g + 1) * 4, :], ALU.mult),
                        r=[f"xa{par}_5", f"eA{g}"], w=[f"CsT{g}"])
                S.hold = True
                for blk in range(4):
                    if S.can_yield(): yield
                    pe(lambda: P_.transpose(C.bankb[:, blk * 128:(blk + 1) * 128], xa[:, blk, csl], C.identb[:]), r=[f"xa{par}_{blk}", "identb"], w=["bankb"], inc=False)
                if S.can_yield(): yield
                pe(lambda: P_.transpose(C.bankb[:, 512:640], xa[:, 4, csl], C.identb[:]), r=[f"xa{par}_4", "identb"], w=["bankb"])
                if S.can_yield(): yield
                act(lambda: A_.copy(xtok[:], C.bankb[:, 0:512]), r=["bankb"], w=["xtok"])
                if S.can_yield(): yield
                act(lambda: A_.copy(Btok[:], C.bankb[:, 512:640]), r=["bankb"], w=["Btok"])
                S.hold = False
                v3 = lambda t_: t_[:].rearrange("p (h c) -> p h c", c=64)
                b3 = lambda t_: t_[:].unsqueeze(2).broadcast_to([128, 8, 64])
                if S.can_yield(): yield
                dve(lambda: V_.tensor_tensor(v3(xdt), v3(xtok), b3(dts), ALU.mult), r=["xtok", "dts"], w=["xdt"])
                if S.can_yield(): yield
                pool(lambda: G_.tensor_tensor(v3(xdw), v3(xdt), b3(dend), ALU.mult), r=["xdt", "dend"], w=["xdw"])
                if S.can_yield(): yield
                dve(lambda: V_.tensor_tensor(v3(ys), v3(xtok), b3(Dbc), ALU.mult), r=["xtok", "Dbc"], w=["ys"])

            def ssd3(s):
                ch = j * NSUB + s
                csl = slice(s * 128, (s + 1) * 128)
                bk, bt = bbank()
                for h in range(8):
                    g, r_ = h // 4, h % 4
                    gs = slice(g * 64, (g + 1) * 64)
                    if S.can_yield(): yield
                    pe(lambda: P_.matmul(bk[:, h * 64:(h + 1) * 64], Mt[:, h, :], xdt[:, h * 64:(h + 1) * 64], start=True, stop=(ch == 0)),
                       r=[f"Mt{g}", "xdt"], w=[bt], inc=False)
                    if ch > 0:
                        if S.can_yield(): yield
                        pe(lambda: P_.matmul(bk[:, h * 64:(h + 1) * 64], CsT[gs, r_, :], Sbf[gs, r_, :], start=False, stop=True),
                           r=[f"CsT{g}", "Sbf"], w=[bt], inc=False)
                bk2, bt2 = bbank()
                if S.can_yield(): yield
                pe(lambda: P_.matmul(bk2[:, :], Btok[:], xdw[:], start=True, stop=True), r=["Btok", "xdw"], w=[bt2])
                for h in range(8):
                    g, r_ = h // 4, h % 4
                    gs = slice(g * 64, (g + 1) * 64)
                    if S.can_yield(): yield
                    dve(lambda: V_.scalar_tensor_tensor(Srun[gs, r_, :], Srun[gs, r_, :], eAl[gs, h:h + 1], bk2[gs, h * 64:(h + 1) * 64], ALU.mult, ALU.add),
                        r=["Srun", "eAl", bt2], w=["Srun"])
                if S.can_yield(): yield
                act(lambda: A_.copy(Sbf[:], Srun[:]), r=["Srun"], w=["Sbf"])
                if S.can_yield(): yield
                dve(lambda: V_.tensor_tensor(ys[:], bk[:, :], ys[:], ALU.add), r=[bt, "ys"], w=["ys"])
                if S.can_yield(): yield
                pool(lambda: G_.tensor_tensor(ys[:], ys[:], zs[:, s, :], ALU.mult), r=["ys", f"zs{par}_{s}"], w=["ys"])
                for g in range(2):
                    if S.can_yield(): yield
                    dve(lambda: V_.bn_stats(gst[:, g, :], ys[:, g * 256:(g + 1) * 256]), r=["ys"], w=[f"gst{g}"])
                    if S.can_yield(): yield
                    dve(lambda: V_.bn_aggr(gmv[:, g, :], gst[:, g, :]), r=[f"gst{g}"], w=[f"gmv{g}"])
                    if S.can_yield(): yield
                    dve(lambda: V_.tensor_tensor(grs[:, g:g + 1], gmv[:, g, 0:1], gmv[:, g, 0:1], ALU.mult), r=[f"gmv{g}"], w=["grs"])
                    if S.can_yield(): yield
                    dve(lambda: V_.tensor_tensor(grs[:, g:g + 1], grs[:, g:g + 1], gmv[:, g, 1:2], ALU.add), r=["grs", f"gmv{g}"], w=["grs"])
                if S.can_yield(): yield
                act(lambda: A_.activation(out=grs[:], in_=grs[:], func=AF.Ln, bias=C.eps_rms[:, 0:1]), r=["grs", "eps_rms"], w=["grs"])
                if S.can_yield(): yield
                act(lambda: A_.activation(out=grs[:], in_=grs[:], func=AF.Exp, scale=-0.5), r=["grs"], w=["grs"])
                for g in range(2):
                    if S.can_yield(): yield
                    dve(lambda: V_.scalar_tensor_tensor(ycat[s][:, g * 256:(g + 1) * 256], ys[:, g * 256:(g + 1) * 256], grs[:, g:g + 1], nw[:, g * 256:(g + 1) * 256],
                                                        ALU.mult, ALU.mult), r=["ys", "grs", "nw"], w=[f"ycat{s}"])

            def gmlp(s):
                if S.can_yield(): yield
                dve(lambda: V_.bn_stats(gst[:, 0, :], gg[:, s, 256:512]), r=[f"gg{par}_{s}"], w=["gst0"])
                if S.can_yield(): yield
                dve(lambda: V_.bn_aggr(gmv[:, 0, :], gst[:, 0, :]), r=["gst0"], w=["gmv0"])
                if S.can_yield(): yield
                act(lambda: A_.activation(out=grs[:, 0:1], in_=gmv[:, 0, 1:2], func=AF.Ln, bias=C.eps_ln[:, 0:1]), r=["gmv0", "eps_ln"], w=["grs"])
                if S.can_yield(): yield
                act(lambda: A_.activation(out=grs[:, 0:1], in_=grs[:, 0:1], func=AF.Exp, scale=-0.5), r=["grs"], w=["grs"])
                if S.can_yield(): yield
                dve(lambda: V_.tensor_scalar(vnf[:], gg[:, s, 256:512], gmv[:, 0, 0:1], grs[:, 0:1], ALU.subtract, ALU.mult), r=[f"gg{par}_{s}", "gmv0", "grs"], w=["vnf"])
                if S.can_yield(): yield
                pool(lambda: G_.tensor_tensor(vnf[:], vnf[:], gmg[:], ALU.mult), r=["vnf", "gmg"], w=["vnf"])
                if S.can_yield(): yield
                pool(lambda: G_.tensor_tensor(vnb[:], vnf[:], gmb[:], ALU.add), r=["vnf", "gmb"], w=["vnb"])
                bk, bt = bbank()
                for g in range(4):
                    if S.can_yield(): yield
                    pe(lambda: P_.matmul(bk[:, g * 64:(g + 1) * 64], WT[:, g, :], vnb[:, g * 64:(g + 1) * 64], start=True, stop=True), r=["WT", "vnb"], w=[bt], inc=(g == 3))
                for g in range(4):
                    if S.can_yield(): yield
                    dve(lambda: V_.scalar_tensor_tensor(ycat[s][:, 768 + g * 64:768 + (g + 1) * 64], bk[:, g * 64:(g + 1) * 64], bs[:, g:g + 1], gg[:, s, g * 64:(g + 1) * 64],
                                                        ALU.add, ALU.mult), r=[bt, "bs", f"gg{par}_{s}"], w=[f"ycat{s}"])


            def attn(hd):
                ob, obt = C.bank[6], "b6"
                o3 = ob[:, 0:NSUB * 65].rearrange("p (q c) -> p q c", c=65)

                def a_up(kt):
                    r_ = max(0, kt - NSUB * j)
                    q0 = r_ * 128
                    sbk, sbt = (C.bank[3], "b3") if (kt % 2 == 0) else (C.bank[4], "b4")
                    if S.can_yield(): yield
                    pe(lambda: P_.matmul(sbk[:, q0:TQ], KT[hd][0:96, kt * 128:(kt + 1) * 128], QT[:, hd, q0:TQ], start=True, stop=True),
                       r=[f"KT{hd}_{kt // NSUB}", f"QT{par}_{hd}"], w=[sbt])
                    pb = pt[(hd * nkt + kt) % NPT]
                    pbt = f"pt{(hd * nkt + kt) % NPT}"
                    if S.can_yield(): yield
                    act(lambda: A_.activation(out=pb[:, q0:TQ], in_=sbk[:, q0:TQ], func=AF.Exp, scale=MLA_SCALE), r=[sbt], w=[pbt])
                    if kt >= NSUB * j:
                        if S.can_yield(): yield
                        pool(lambda: G_.tensor_tensor(pb[:, q0:q0 + 128], pb[:, q0:q0 + 128], C.triub[:], ALU.mult), r=[pbt, "triub"], w=[pbt])

                def a_down(kt):
                    r_ = max(0, kt - NSUB * j)
                    pb = pt[(hd * nkt + kt) % NPT]
                    pbt = f"pt{(hd * nkt + kt) % NPT}"
                    for qs in range(r_, NSUB):
                        last = (kt == NSUB * j + qs)
                        if S.can_yield(): yield
                        pe(lambda: P_.matmul(o3[:, qs, :], pb[:, qs * 128:(qs + 1) * 128], Vt[:, kt, hd, :], start=(kt == 0 and qs == 0), stop=last, skip_group_check=True),
                           r=[pbt, f"V{kt}", "Vones"], w=[obt], inc=(qs == NSUB - 1))

                for idx in range(nkt + 1):
                    if idx < nkt:
                        yield from a_up(idx)
                    if idx >= 1:
                        yield from a_down(idx - 1)
                if S.can_yield(): yield
                dve(lambda: V_.reciprocal(rcp[:], o3[:, :, 64]), r=[obt], w=["rcp"])
                for qs in range(NSUB):
                    if S.can_yield(): yield
                    dve(lambda: V_.tensor_scalar(ycat[qs][:, 512 + hd * 64:512 + (hd + 1) * 64], o3[:, qs, 0:64], rcp[:, qs:qs + 1], None, ALU.mult),
                        r=[obt, "rcp"], w=[f"ycat{qs}"])


            for s in range(NSUB):
                yield from ssd1(s)
                yield from gmlp(s)
                yield from attn(2 * s)
                yield from ssd2(s)
                yield from attn(2 * s + 1)
                yield from ssd3(s)
            for s in range(NSUB):
                S.hold = True
                for k in range(8):
                    pe(lambda: P_.transpose(C.bankb[:, k * 128:(k + 1) * 128], ycat[s][:, k * 128:(k + 1) * 128], C.identb[:]), r=[f"ycat{s}", "identb"], w=["bankb"], inc=(k == 7))
                if S.can_yield(): yield
                act(lambda: A_.copy(yT[:].rearrange("p k t -> p (k t)"), C.bankb[:]), r=["bankb"], w=["yT"])
                S.hold = False
                if S.can_yield(): yield
                S.dma("sp", outt[:], u_out_d[T0 + s * 128:T0 + (s + 1) * 128, :], reads=[f"uo{j}_{s}"], writes=["rest"])
                for hlf in range(2):
                    bk, bt = bbank()
                    for k in range(8):
                        if S.can_yield(): yield
                        pe(lambda: P_.matmul(bk[:, :], yT[:, k, :], wout[:, k, hlf * 512:(hlf + 1) * 512], start=(k == 0), stop=(k == 7)), r=["yT", WOUT[k]], w=[bt], inc=(k == 7))
                    if S.can_yield(): yield
                    dve(lambda: V_.tensor_tensor(outt[:, hlf * 512:(hlf + 1) * 512], bk[:, :], outt[:, hlf * 512:(hlf + 1) * 512], ALU.add), r=[bt, "rest"], w=["rest"])
                if S.can_yield(): yield
                S.dma("sp", u_out_d[T0 + s * 128:T0 + (s + 1) * 128, :], outt[:], reads=["rest"], writes=[f"uo{j}_{s}"])

        def merge(ga, gb, ra=1, rb=3):
            live = [ga is not None, gb is not None]
            while live[0] or live[1]:
                for gi, (g_, n_) in enumerate(((ga, ra), (gb, rb))):
                    for _ in range(n_):
                        if live[gi]:
                            try:
                                next(g_)
                            except StopIteration:
                                live[gi] = False

        for j in range(NT + 1):
            merge(front(j) if j < NT else None, back(j - 1) if j >= 1 else None)
        barrier(C)


def modulation_vectors(C, c_d, ada_w_d, ada_b_d, gin_d, bin_d, vec, name):
    nc, S = C.nc, C.S
    V_, A_, G_, P_ = nc.vector, nc.scalar, nc.gpsimd, nc.tensor
    with ExitStack() as es:
        def sb(nm, shape, dt):
            return es.enter_context(nc.sbuf_tensor(f"{name}_{nm}", list(shape), dt))
        sc = sb("sc", [128, 8], F32)
        scb = sb("scb", [128, 8, 128], F32)
        wblk = [sb(f"wblk{i}", [128, 8, 512], F32) for i in range(2)]
        mrow = sb("mrow", [128, 3072], F32)
        brow = sb("brow", [128, 3072], F32)
        gt = sb("gt", [128, 1024], F32)
        bt_ = sb("bt", [128, 1024], F32)
        with nc.allow_non_contiguous_dma(reason="tiny conditioning vector"):
            S.dma("sp", sc[:], c_d.rearrange("(k p) -> p k", p=128), writes=["sc"])
        S.op("act", lambda: A_.activation(out=sc[:], in_=sc[:], func=AF.Silu), reads=["sc"], writes=["sc"])
        for k in range(8):
            S.op("dve", lambda: V_.tensor_scalar(scb[:, k, :], C.ones[:], sc[:, k:k + 1], None, ALU.mult), reads=["sc", "cpack"], writes=["scb"])
        S.dma("sp", brow[:], ada_b_d.partition_broadcast(128), writes=["brow"])
        S.dma("sp", gt[:], gin_d.partition_broadcast(128), writes=["gt"])
        S.dma("sp", bt_[:], bin_d.partition_broadcast(128), writes=["bt"])
        wv = ada_w_d.rearrange("(k p) e -> p k e", p=128)
        for cb in range(6):
            wb = wblk[cb % 2]
            S.dma("sp", wb[:], wv[:, :, cb * 512:(cb + 1) * 512], writes=[f"wblk{cb % 2}"])
            bk, bt = C.bank[cb % 2], f"b{cb % 2}"
            for k in range(8):
                S.op("pe", lambda: P_.matmul(bk[:, :], scb[:, k, :], wb[:, k, :], start=(k == 0), stop=(k == 7)),
                     reads=["scb", f"wblk{cb % 2}"], writes=[bt], inc=(k == 7))
            S.op("dve", lambda: V_.tensor_tensor(mrow[:, cb * 512:(cb + 1) * 512], bk[:, :], brow[:, cb * 512:(cb + 1) * 512], ALU.add),
                 reads=[bt, "brow"], writes=["mrow"])
        shift, scale, gate = mrow[:, 0:1024], mrow[:, 1024:2048], mrow[:, 2048:3072]
        S.op("dve", lambda: V_.tensor_scalar(scale, scale, 1.0, None, ALU.add), reads=["mrow"], writes=["mrow"])
        S.op("dve", lambda: V_.tensor_scalar(gate, gate, 1.0, None, ALU.add), reads=["mrow"], writes=["mrow"])
        S.op("dve", lambda: V_.tensor_tensor(vec["P1"][:], gt[:], scale, ALU.mult), reads=["gt", "mrow"], writes=["P1"])
        S.op("dve", lambda: V_.tensor_tensor(vec["P2"][:], bt_[:], scale, ALU.mult), reads=["bt", "mrow"], writes=["P2"])
        S.op("dve", lambda: V_.tensor_tensor(vec["P2"][:], vec["P2"][:], shift, ALU.add), reads=["P2", "mrow"], writes=["P2"])
        S.op("dve", lambda: V_.tensor_scalar(vec["Q1"][:], gt[:], DN_ALPHA, None, ALU.mult), reads=["gt"], writes=["Q1"])
        S.op("dve", lambda: V_.tensor_scalar(vec["Q2"][:], bt_[:], DN_ALPHA, None, ALU.mult), reads=["bt"], writes=["Q2"])
        S.op("dve", lambda: V_.tensor_copy(vec["Q3b"][:], gate), reads=["mrow"], writes=["Q3b"])
        barrier(C)


def final_ln(C, u_d, out_d, g_d, b_d, Tn, name="fin"):
    nc, S = C.nc, C.S
    with ExitStack() as es:
        def sb(nm, shape, dt):
            return es.enter_context(nc.sbuf_tensor(f"{name}_{nm}", list(shape), dt))
        gt = sb("gt", [128, 1024], F32)
        bt_ = sb("bt", [128, 1024], F32)
        S.dma("sp", gt[:], g_d.partition_broadcast(128), writes=["fgt"])
        S.dma("sp", bt_[:], b_d.partition_broadcast(128), writes=["fbt"])
        ut = [sb(f"ut{i}", [128, 1024], F32) for i in range(3)]
        mv = [sb(f"mv{i}", [128, 2], F32) for i in range(3)]
        rstd = [sb(f"rstd{i}", [128, 1], F32) for i in range(3)]
        st = sb("st", [128, 2, 6], F32)
        for s in range(Tn // 128):
            b = s % 3
            S.dma("sp", ut[b][:], u_d[s * 128:(s + 1) * 128, :], writes=[f"fut{b}"])
            ln_stats(C, ut[b][:], f"fut{b}", mv[b][:], f"fmv{b}", rstd[b][:], f"frstd{b}", st)
            S.op("dve", lambda: nc.vector.tensor_scalar(ut[b][:], ut[b][:], mv[b][:, 0:1], rstd[b][:, 0:1], ALU.subtract, ALU.mult),
                 reads=[f"fut{b}", f"fmv{b}", f"frstd{b}"], writes=[f"fut{b}"])
            S.op("pool", lambda: nc.gpsimd.tensor_tensor(ut[b][:], ut[b][:], gt[:], ALU.mult), reads=[f"fut{b}", "fgt"], writes=[f"fut{b}"])
            S.op("dve", lambda: nc.vector.tensor_tensor(ut[b][:], ut[b][:], bt_[:], ALU.add), reads=[f"fut{b}", "fbt"], writes=[f"fut{b}"])
            S.dma("sp", out_d[s * 128:(s + 1) * 128, :], ut[b][:], reads=[f"fut{b}"])
        barrier(C)


W_SHAPES = {
    "ln0_g": [D], "ln0_b": [D], "ada_w": [2, 2, D, 3 * D], "ada_b": [2, 2, 3 * D], "post_ln_g": [2, 2, D], "post_ln_b": [2, 2, D],
    "w_in": [2, D, D_IN], "ssd_conv_w": [2, 4, 768], "ssd_conv_b": [2, 768], "ssd_dt_bias": [2, 8], "ssd_a_log": [2, 8], "ssd_d": [2, 8],
    "ssd_norm_w": [2, 512], "mla_q_norm": [2, 256], "mla_w_qb": [2, 256, 384], "mla_kv_norm": [2, 128], "mla_w_kvb": [2, 128, 512],
    "gm_ln_g": [2, 256], "gm_ln_b": [2, 256], "gm_w_s": [2, 4, 128, 128], "gm_b_s": [2, 4, 128], "w_out": [2, D, D],
    "ffn_w1": [1, D, DFF], "ffn_w3": [1, D, DFF], "ffn_w2": [1, DFF, D], "moe_router": [1, D, NE],
    "moe_w1": [1, NE, D, DFF], "moe_w3": [1, NE, D, DFF], "moe_w2": [1, NE, DFF, D],
}


def host_consts(Tn):
    i = np.arange(128)
    triu = (i[:, None] <= i[None, :]).astype(np.float32)
    mneg = np.where(i[None, :] >= i[:, None], 0.0, -30000.0).astype(np.float32)
    cpack = np.concatenate([triu, mneg, np.ones((128, 128), np.float32)], axis=1)
    inv = (10000.0 ** (-np.arange(0, 32, 2, dtype=np.float32) / 32)).astype(np.float32)
    ang = (np.arange(Tn, dtype=np.float32)[:, None] * inv[None, :]).astype(np.float32)
    cos, sin = np.cos(ang).T.astype(np.float32), np.sin(ang).T.astype(np.float32)
    rope = np.zeros((2, 96, Tn), np.float32)
    rope[0, 0:64] = 1.0
    rope[0, 64:80] = cos
    rope[0, 80:96] = cos
    rope[1, 64:80] = -sin
    rope[1, 80:96] = sin
    return {"ident": np.eye(128, dtype=np.float32), "cpack": cpack, "rope": rope}


def build_program(Tn=T, n_sub=4):
    nc = bass.Bass("TRN2", target_bir_lowering=False)
    C = Ctx(nc)
    S = C.S
    x_d = nc.dram_tensor("x", [Tn, D], F32, kind="ExternalInput").ap()
    c_d = nc.dram_tensor("c", [D], F32, kind="ExternalInput").ap()
    Wd = {n: nc.dram_tensor(n, sh, F32, kind="ExternalInput").ap() for n, sh in W_SHAPES.items()}
    id_d = nc.dram_tensor("ident", [128, 128], F32, kind="ExternalInput").ap()
    cp_d = nc.dram_tensor("cpack", [128, 384], F32, kind="ExternalInput").ap()
    rope_d = nc.dram_tensor("rope", [2, 96, Tn], F32, kind="ExternalInput").ap()
    out_d = nc.dram_tensor("out", [Tn, D], F32, kind="ExternalOutput").ap()
    scr = [nc.dram_tensor(f"uscr{i}", [Tn, D], F32).ap() for i in range(2)]
    setup_consts(C, id_d, cp_d)
    vec = {n: C.sb("v" + n, [128, 1024], F32) for n in ["P1", "P2", "Q1", "Q2"]}
    vec["Q3b"] = C.sb("vQ3b", [128, 1024], BF16)
    u_in = x_d
    for i in range(n_sub):
        l, sub = i // 2, i % 2
        if i == 0:
            gin, bin_ = Wd["ln0_g"], Wd["ln0_b"]
        else:
            pl, ps = (i - 1) // 2, (i - 1) % 2
            gin, bin_ = Wd["post_ln_g"][pl, ps], Wd["post_ln_b"][pl, ps]
        modulation_vectors(C, c_d, Wd["ada_w"][l, sub], Wd["ada_b"][l, sub], gin, bin_, vec, f"mod{i}")
        u_out = scr[i % 2]
        last = (i == n_sub - 1 and sub == 1)
        fin = None
        if last:
            fin = (C.sb("fing", [128, 1024], F32), C.sb("finb", [128, 1024], F32))
            load_bcast(C, fin[0], Wd["post_ln_g"][l, sub], "fing")
            load_bcast(C, fin[1], Wd["post_ln_b"][l, sub], "finb")
            u_out = out_d
        if sub == 0:
            W = {"w_in": Wd["w_in"][l], "conv_w": Wd["ssd_conv_w"][l], "conv_b": Wd["ssd_conv_b"][l], "dt_bias": Wd["ssd_dt_bias"][l],
                 "a_log": Wd["ssd_a_log"][l], "ssd_d": Wd["ssd_d"][l], "ssd_norm_w": Wd["ssd_norm_w"][l], "q_norm": Wd["mla_q_norm"][l],
                 "w_qb": Wd["mla_w_qb"][l], "kv_norm": Wd["mla_kv_norm"][l], "w_kvb": Wd["mla_w_kvb"][l], "gm_ln_g": Wd["gm_ln_g"][l],
                 "gm_ln_b": Wd["gm_ln_b"][l], "gm_w_s": Wd["gm_w_s"][l], "gm_b_s": Wd["gm_b_s"][l], "w_out": Wd["w_out"][l], "rope": rope_d}
            mixer_sublayer(C, u_in, u_out, vec, W, Tn, name=f"mix{l}")
        elif l % 2 == 0:
            ffn_sublayer(C, u_in, u_out, vec, [(Wd["ffn_w1"][l // 2], Wd["ffn_w3"][l // 2], Wd["ffn_w2"][l // 2])], Tn, name=f"ffn{l}", final=fin)
        else:
            m = l // 2
            experts = [(Wd["moe_w1"][m, e], Wd["moe_w3"][m, e], Wd["moe_w2"][m, e]) for e in range(NE)]
            ffn_sublayer(C, u_in, u_out, vec, experts, Tn, router_d=Wd["moe_router"][m], name=f"moe{l}", final=fin)
        u_in = u_out
    pl, ps = (n_sub - 1) // 2, (n_sub - 1) % 2
    if (n_sub - 1) % 2 == 0:
        final_ln(C, u_in, out_d, Wd["post_ln_g"][pl, ps], Wd["post_ln_b"][pl, ps], Tn)
    S.finish()
    return nc, C


_CACHE = {}


def kernel(**inputs):
    x = np.asarray(inputs["x"], dtype=np.float32)
    B, Tn, _ = x.shape
    if Tn not in _CACHE:
        _CACHE[Tn] = build_program(Tn)
    nc, _ = _CACHE[Tn]
    consts = host_consts(Tn)
    shared = {n: np.ascontiguousarray(np.asarray(inputs[n], dtype=np.float32)) for n in W_SHAPES}
    shared.update(consts)
    c = np.asarray(inputs["c"], dtype=np.float32)
    in_maps = []
    for b in range(B):
        m = dict(shared)
        m["x"] = np.ascontiguousarray(x[b])
        m["c"] = np.ascontiguousarray(c[b])
        in_maps.append(m)
    res = run_bass_kernel_spmd(nc, in_maps, core_ids=list(range(B)))
    return np.stack([np.asarray(r["out"], dtype=np.float32) for r in res.results], axis=0)
```

```python
import math
from contextlib import ExitStack

import numpy as np
import concourse.bass as bass
import concourse.mybir as mybir
from concourse.bass_utils import run_bass_kernel_spmd

F32 = mybir.dt.float32
BF16 = mybir.dt.bfloat16
I32 = mybir.dt.int32
AF = mybir.ActivationFunctionType
ALU = mybir.AluOpType
AX = mybir.AxisListType

T = 4096
D = 1024
DFF = 2816
NE = 8
NCH = DFF // 128
DEPTH = 2
DN_ALPHA = (2 * DEPTH) ** 0.25
LN_EPS = 1e-5
RMS_EPS = 1e-6
D_IN = 2216


class Sched:
    NDSEM = 6

    def __init__(self, nc):
        self.nc = nc
        self.engs = {"pe": nc.tensor, "act": nc.scalar, "dve": nc.vector, "pool": nc.gpsimd, "sp": nc.sync}
        self.sem = {k: nc.alloc_semaphore("prog_" + k) for k in ("pe", "act", "dve", "pool")}
        self.cnt = {k: 0 for k in self.sem}
        self.seen = {k: {} for k in self.engs}
        self.lastw = {}
        self.rd = {}
        self.dsem = {}
        self.dcnt = {}
        self.dnext = {}
        for q in ("sp", "act", "pool"):
            for i in range(self.NDSEM):
                key = ("d", q, i)
                self.dsem[key] = nc.alloc_semaphore(f"dma_{q}{i}")
                self.dcnt[key] = 0
            self.dnext[q] = 0
        self.n_inst = 0
        self.pe_open = False
        self.hold = False

    def can_yield(self):
        return not (self.pe_open or self.hold)

    def _semh(self, key):
        return self.sem[key] if isinstance(key, str) else self.dsem[key]

    def wait(self, eng, ev):
        key, val = ev
        if val <= 0:
            return
        if key == eng and eng in ("pe",):
            return
        if self.seen[eng].get(key, 0) >= val:
            return
        self.engs[eng].wait_ge(self._semh(key), val)
        self.seen[eng][key] = val
        self.n_inst += 1

    def _deps(self, reads, writes):
        evs = []
        for r in reads:
            if r in self.lastw:
                evs.append(self.lastw[r])
        for w in writes:
            if w in self.lastw:
                evs.append(self.lastw[w])
            for k, v in self.rd.get(w, {}).items():
                evs.append((k, v))
        return evs

    def _record(self, ev, reads, writes):
        for r in reads:
            d = self.rd.setdefault(r, {})
            d[ev[0]] = max(d.get(ev[0], 0), ev[1])
        for w in writes:
            self.lastw[w] = ev
            self.rd[w] = {}

    PSUM_TOK = frozenset([f"b{i}" for i in range(8)] + ["bankb"])

    def op(self, eng, fn, reads=(), writes=(), inc=True):
        pr = [r for r in reads if r in self.PSUM_TOK]
        if pr:
            reads = [r for r in reads if r not in self.PSUM_TOK]
            writes = list(writes) + pr
        for ev in self._deps(reads, writes):
            self.wait(eng, ev)
        ins = fn()
        self.n_inst += 1
        if eng == "pe":
            self.pe_open = not inc
        if inc:
            ins.then_inc(self.sem[eng], 1)
            self.cnt[eng] += 1
            ev = (eng, self.cnt[eng])
        else:
            ev = (eng, self.cnt[eng] + 1)
        self._record(ev, reads, writes)
        return ins

    def dma(self, q, out, in_, reads=(), writes=()):
        i = self.dnext[q] % self.NDSEM
        self.dnext[q] += 1
        key = ("d", q, i)
        if self.dcnt[key] > 0:
            self.wait(q, (key, self.dcnt[key]))
        for ev in self._deps(reads, writes):
            self.wait(q, ev)
        ins = self.engs[q].dma_start(out=out, in_=in_)
        ins.then_inc(self.dsem[key], 16)
        self.n_inst += 1
        self.dcnt[key] += 16
        ev = (key, self.dcnt[key])
        self._record(ev, reads, writes)
        return ins

    def finish(self):
        for key, v in self.dcnt.items():
            if v > 0:
                self.wait("sp", (key, v))
        for k, v in self.cnt.items():
            self.wait("sp", (k, v))


class Ctx:
    def __init__(self, nc):
        self.nc = nc
        self.S = Sched(nc)
        self.stack = ExitStack()
        self.bank = [nc.alloc_psum_tensor(f"bank{i}", [128, 512], F32) for i in range(7)]
        self.bankb = nc.alloc_psum_tensor("bankb", [128, 1024], BF16)

    def chk(self, n):
        if getattr(self, "sstop", 99) <= n:
            raise StopIteration

    def sb(self, name, shape, dtype):
        return self.nc.alloc_sbuf_tensor("sb_" + name, list(shape), dtype)


def setup_consts(C, ident_f_d, cpack_d=None):
    nc, S = C.nc, C.S
    C.identf = C.sb("identf", [128, 128], F32)
    C.identb = C.sb("identb", [128, 128], BF16)
    S.dma("sp", C.identf[:], ident_f_d, writes=["identf"])
    if cpack_d is not None:
        C.cpack = C.sb("cpack", [128, 384], F32)
        S.dma("sp", C.cpack[:], cpack_d, writes=["cpack"])
        C.triu = C.cpack[:, 0:128]
        C.mneg = C.cpack[:, 128:256]
        C.ones = C.cpack[:, 256:384]
        C.triub = C.sb("triub", [128, 128], BF16)
        S.op("dve", lambda: nc.vector.tensor_copy(C.triub[:], C.triu), reads=["cpack"], writes=["triub"])
    C.eps_ln = C.sb("eps_ln", [128, 1], F32)
    C.eps_rms = C.sb("eps_rms", [128, 1], F32)
    S.op("pool", lambda: nc.gpsimd.memset(C.eps_ln[:], LN_EPS), writes=["eps_ln"])
    S.op("pool", lambda: nc.gpsimd.memset(C.eps_rms[:], RMS_EPS), writes=["eps_rms"])
    S.op("dve", lambda: nc.vector.tensor_copy(C.identb[:], C.identf[:]), reads=["identf"], writes=["identb"])


def ln_stats(C, u_ap, tok_u, mv, tok_mv, rstd, tok_rstd, st):
    nc, S = C.nc, C.S
    S.op("dve", lambda: nc.vector.bn_stats(st[:, 0, :], u_ap[:, 0:512]), reads=[tok_u], writes=["st0"])
    S.op("dve", lambda: nc.vector.bn_stats(st[:, 1, :], u_ap[:, 512:1024]), reads=[tok_u], writes=["st1"])
    S.op("dve", lambda: nc.vector.bn_aggr(mv, st[:].rearrange("p a b -> p (a b)")), reads=["st0", "st1"], writes=[tok_mv])
    S.op("act", lambda: nc.scalar.activation(out=rstd, in_=mv[:, 1:2], func=AF.Ln, bias=C.eps_ln[:, 0:1]),
         reads=[tok_mv, "eps_ln"], writes=[tok_rstd])
    S.op("act", lambda: nc.scalar.activation(out=rstd, in_=rstd, func=AF.Exp, scale=-0.5),
         reads=[tok_rstd], writes=[tok_rstd])


def barrier(C):
    S = C.S
    evs = [(k, v) for k, v in S.cnt.items()] + [(k, v) for k, v in S.dcnt.items()]
    for eng in ("pe", "act", "dve", "pool", "sp"):
        for ev in evs:
            S.wait(eng, ev)


def load_bcast(C, tile, dram_vec, tok):
    C.S.dma("sp", tile[:], dram_vec.partition_broadcast(128), writes=[tok])


def ln_front(C, ph, u_d, r0, vec, want_f32=False):
    nc, S = C.nc, C.S
    i = ph["i"] = ph.get("i", 0) + 1
    b = 0 if ph.get("single") else i % 2
    ut, xn, hb, hf = ph["ut"][b], ph["xn"][b], ph["hb"][b], ph["hf"]
    mv, rstd, st = ph["mv"][b], ph["rstd"][b], ph["st"]
    tx = f"ut{b}" if ph["xn"] is ph["ut"] else f"xn{b}"
    ph["tx"] = tx
    S.dma("sp", ut[:], u_d[r0:r0 + 128, :], writes=[f"ut{b}"])
    ln_stats(C, ut[:], f"ut{b}", mv[:], f"mv{b}", rstd[:], f"rstd{b}", st)
    S.op("dve", lambda: nc.vector.tensor_scalar(xn[:], ut[:], mv[:, 0:1], rstd[:, 0:1], ALU.subtract, ALU.mult),
         reads=[f"ut{b}", f"mv{b}", f"rstd{b}"], writes=[tx])
    if hf is None:
        S.op("pool", lambda: nc.gpsimd.tensor_tensor(hb[:], xn[:], vec["P1"][:], ALU.mult), reads=[tx, "P1"], writes=[f"hb{b}"])
        S.op("pool", lambda: nc.gpsimd.tensor_tensor(hb[:], hb[:], vec["P2"][:], ALU.add), reads=[f"hb{b}", "P2"], writes=[f"hb{b}"])
        return b
    S.op("pool", lambda: nc.gpsimd.tensor_tensor(hf[:], xn[:], vec["P1"][:], ALU.mult), reads=[tx, "P1"], writes=["hf"])
    if want_f32:
        S.op("dve", lambda: nc.vector.tensor_tensor(hf[:], hf[:], vec["P2"][:], ALU.add), reads=["hf", "P2"], writes=["hf"])
        S.op("act", lambda: nc.scalar.copy(hb[:], hf[:]), reads=["hf"], writes=[f"hb{b}"])
    else:
        S.op("pool", lambda: nc.gpsimd.tensor_tensor(hb[:], hf[:], vec["P2"][:], ALU.add), reads=["hf", "P2"], writes=[f"hb{b}"])
    return b


def transpose_to(C, src, src_tok, dst3, dst_tok, col0):
    nc, S = C.nc, C.S
    for k in range(8):
        S.op("pe", lambda: nc.tensor.transpose(C.bankb[:, k * 128:(k + 1) * 128], src[:, k * 128:(k + 1) * 128], C.identb[:]),
             reads=[src_tok, "identb"], writes=["bankb"], inc=(k == 7))
    S.op("act", lambda: nc.scalar.copy(dst3[:, :, col0:col0 + 128], C.bankb[:].rearrange("p (k t) -> p k t", k=8)),
         reads=["bankb"], writes=[dst_tok])


def ffn_sublayer(C, u_in_d, u_out_d, vec, experts, Tn, router_d=None, name="ffn", final=None):
    nc, S = C.nc, C.S
    HT = min(2048, Tn)
    NS = HT // 128
    moe = router_d is not None
    with ExitStack() as es:
        def sb(nm, shape, dt):
            return es.enter_context(nc.sbuf_tensor(f"{name}_{nm}", list(shape), dt))
        hT = sb("hT", [128, 8, HT], BF16)
        yacc = sb("yacc", [128, NS, 1024], F32)
        ph = {"ut": [sb(f"ut{i}", [128, 1024], F32) for i in range(2)],
              "xn": [sb(f"xn{i}", [128, 1024], F32) for i in range(2)],
              "hb": [sb(f"hb{i}", [128, 1024], BF16) for i in range(2)],
              "hf": sb("hf", [128, 1024], F32),
              "mv": [sb(f"mv{i}", [128, 2], F32) for i in range(2)],
              "rstd": [sb(f"rstd{i}", [128, 1], F32) for i in range(2)],
              "st": sb("st", [128, 2, 6], F32)}
        GC = 4
        groups = [list(range(c, min(c + GC, NCH))) for c in range(0, NCH, GC)]
        NWB = 2
        w1g = [sb(f"w1g{i}", [128, 8, GC * 128], BF16) for i in range(NWB)]
        w3g = [sb(f"w3g{i}", [128, 8, GC * 128], BF16) for i in range(NWB)]
        w2g = [sb(f"w2g{i}", [128, GC, 1024], BF16) for i in range(NWB)]
        NAB = 4
        sil = [sb(f"sil{i}", [128, 256], BF16) for i in range(NAB)]
        actT = [sb(f"actT{i}", [128, 256], BF16) for i in range(NAB)]
        if moe:
            hTf = sb("hTf", [128, 8, 128], F32)
            wr = sb("wr", [128, 8, NE], F32)
            lg = sb("lg", [128, NE], F32)
            m8 = sb("m8", [128, 8], F32)
            gts = sb("gts", [128, 2], F32)
            dlt = sb("dlt", [128, 2], F32)
            cm1 = sb("cm1", [128, NE], F32)
            comb = sb("comb", [128, NS, NE], F32)
            S.dma("sp", wr[:], router_d.rearrange("(k p) e -> p k e", p=128), writes=["wr"])
        abank = [C.bank[0], C.bank[1], C.bank[2]]
        accb = [C.bank[3], C.bank[4], C.bank[5], C.bank[6]]
        wld = 0
        abi = 0
        for half in range(Tn // HT):
            t0 = half * HT
            for s in range(NS):
                b = ln_front(C, ph, u_in_d, t0 + s * 128, vec, want_f32=moe)
                xn = ph["xn"][b]
                S.op("dve", lambda: nc.vector.tensor_tensor(yacc[:, s, :], xn[:], vec["Q1"][:], ALU.mult),
                     reads=[ph["tx"], "Q1"], writes=[f"yacc{s}"])
                S.op("pool", lambda: nc.gpsimd.tensor_tensor(yacc[:, s, :], yacc[:, s, :], vec["Q2"][:], ALU.add),
                     reads=[f"yacc{s}", "Q2"], writes=[f"yacc{s}"])
                transpose_to(C, ph["hb"][b], f"hb{b}", hT, f"hT{s // 2}", s * 128)
                if moe:
                    hf = ph["hf"]
                    for q in range(2):
                        for kk in range(4):
                            k = q * 4 + kk
                            S.op("pe", lambda: nc.tensor.transpose(C.bank[q][:, kk * 128:(kk + 1) * 128], hf[:, k * 128:(k + 1) * 128], C.identf[:]),
                                 reads=["hf", "identf"], writes=[f"b{q}"], inc=(kk == 3))
                        S.op("dve", lambda: nc.vector.tensor_copy(hTf[:, q * 4:(q + 1) * 4, :], C.bank[q][:].rearrange("p (k t) -> p k t", k=4)),
                             reads=[f"b{q}"], writes=["hTf"])
                    for k in range(8):
                        S.op("pe", lambda: nc.tensor.matmul(C.bank[2][:, 0:NE], hTf[:, k, :], wr[:, k, :], start=(k == 0), stop=(k == 7)),
                             reads=["hTf", "wr"], writes=["b2"], inc=(k == 7))
                    S.op("dve", lambda: nc.vector.tensor_copy(lg[:], C.bank[2][:, 0:NE]), reads=["b2"], writes=["lg"])
                    S.op("dve", lambda: nc.vector.max(m8[:], lg[:]), reads=["lg"], writes=["m8"])
                    S.op("dve", lambda: nc.vector.tensor_tensor(dlt[:, 0:1], m8[:, 0:1], m8[:, 1:2], ALU.subtract), reads=["m8"], writes=["dlt"])
                    S.op("dve", lambda: nc.vector.tensor_tensor(dlt[:, 1:2], m8[:, 1:2], m8[:, 0:1], ALU.subtract), reads=["m8", "dlt"], writes=["dlt"])
                    S.op("act", lambda: nc.scalar.activation(out=gts[:], in_=dlt[:], func=AF.Sigmoid), reads=["dlt"], writes=["gts"])
                    S.op("dve", lambda: nc.vector.tensor_scalar(cm1[:], lg[:], m8[:, 0:1], gts[:, 0:1], ALU.is_equal, ALU.mult),
                         reads=["lg", "m8", "gts"], writes=["cm1"])
                    S.op("dve", lambda: nc.vector.tensor_scalar(comb[:, s, :], lg[:], m8[:, 1:2], gts[:, 1:2], ALU.is_equal, ALU.mult),
                         reads=["lg", "m8", "gts"], writes=[f"comb{s}"])
                    S.op("dve", lambda: nc.vector.tensor_tensor(comb[:, s, :], comb[:, s, :], cm1[:], ALU.add),
                         reads=[f"comb{s}", "cm1"], writes=[f"comb{s}"])
            for e, (w1_d, w3_d, w2_d) in enumerate(experts):
                w1v = w1_d.rearrange("(k p) f -> p k f", p=128)
                w3v = w3_d.rearrange("(k p) f -> p k f", p=128)
                w2v = w2_d.rearrange("(c p) d -> p c d", p=128)
                for grp in groups:
                    wb = wld % NWB
                    wld += 1
                    ng = len(grp)
                    f0 = grp[0] * 128
                    S.dma("pool", w1g[wb][:, :, 0:ng * 128], w1v[:, :, f0:f0 + ng * 128], writes=[f"w1g{wb}"])
                    S.dma("pool", w3g[wb][:, :, 0:ng * 128], w3v[:, :, f0:f0 + ng * 128], writes=[f"w3g{wb}"])
                    S.dma("pool", w2g[wb][:, 0:ng, :], w2v[:, grp[0]:grp[0] + ng, :], writes=[f"w2g{wb}"])
                    for cc in range(ng):
                        S.op("pool", lambda: nc.gpsimd.tensor_tensor(w2g[wb][:, cc, :], w2g[wb][:, cc, :], vec["Q3b"][:], ALU.mult),
                             reads=[f"w2g{wb}", "Q3b"], writes=[f"w2g{wb}"])
                    seq = [(j, cc) for j in range(HT // 256) for cc in range(ng)]
                    SKEW = 2
                    st_ = {}

                    def up(j, cc):
                        nonlocal abi
                        c0 = j * 256
                        ab = abank[abi % 3]
                        abt = f"b{abi % 3}"
                        bf = abi % NAB
                        abi += 1
                        st_[(j, cc)] = bf
                        for k in range(8):
                            S.op("pe", lambda: nc.tensor.matmul(ab[:, 0:256], w1g[wb][:, k, cc * 128:(cc + 1) * 128], hT[:, k, c0:c0 + 256],
                                                                start=(k == 0), stop=(k == 7)),
                                 reads=[f"w1g{wb}", f"hT{j}"], writes=[abt], inc=False)
                        for k in range(8):
                            S.op("pe", lambda: nc.tensor.matmul(ab[:, 256:512], w3g[wb][:, k, cc * 128:(cc + 1) * 128], hT[:, k, c0:c0 + 256],
                                                                start=(k == 0), stop=(k == 7)),
                                 reads=[f"w3g{wb}", f"hT{j}"], writes=[abt], inc=(k == 7))
                        S.op("act", lambda: nc.scalar.activation(out=sil[bf][:], in_=ab[:, 0:256], func=AF.Silu),
                             reads=[abt], writes=[f"sil{bf}"])
                        S.op("dve", lambda: nc.vector.tensor_tensor(actT[bf][:], ab[:, 256:512], sil[bf][:], ALU.mult),
                             reads=[abt, f"sil{bf}"], writes=[f"actT{bf}"])

                    def down(j, cc):
                        bf = st_[(j, cc)]
                        for s2 in range(2):
                            for hlf in range(2):
                                bi = s2 * 2 + hlf
                                S.op("pe", lambda: nc.tensor.matmul(accb[bi][:, :], actT[bf][:, s2 * 128:(s2 + 1) * 128],
                                                                    w2g[wb][:, cc, hlf * 512:(hlf + 1) * 512],
                                                                    start=(cc == 0), stop=(cc == ng - 1)),
                                     reads=[f"actT{bf}", f"w2g{wb}"], writes=[f"b{3 + bi}"], inc=(cc == ng - 1 or bi == 3))
                        if cc == ng - 1:
                            for s2 in range(2):
                                s = j * 2 + s2
                                for hlf in range(2):
                                    bi = s2 * 2 + hlf
                                    ysl = yacc[:, s, hlf * 512:(hlf + 1) * 512]
                                    if moe:
                                        S.op("dve", lambda: nc.vector.scalar_tensor_tensor(ysl, accb[bi][:, :], comb[:, s, e:e + 1], ysl, ALU.mult, ALU.add),
                                             reads=[f"b{3 + bi}", f"comb{s}", f"yacc{s}"], writes=[f"yacc{s}"])
                                    else:
                                        S.op("dve", lambda: nc.vector.tensor_tensor(ysl, accb[bi][:, :], ysl, ALU.add),
                                             reads=[f"b{3 + bi}", f"yacc{s}"], writes=[f"yacc{s}"])

                    for idx in range(len(seq) + SKEW):
                        if idx < len(seq):
                            up(*seq[idx])
                        if idx >= SKEW:
                            down(*seq[idx - SKEW])
            for s in range(NS):
                if final is not None:
                    fg, fb = final
                    b = s % 2
                    ln_stats(C, yacc[:, s, :], f"yacc{s}", ph["mv"][b][:], f"mv{b}", ph["rstd"][b][:], f"rstd{b}", ph["st"])
                    S.op("dve", lambda: nc.vector.tensor_scalar(yacc[:, s, :], yacc[:, s, :], ph["mv"][b][:, 0:1], ph["rstd"][b][:, 0:1], ALU.subtract, ALU.mult),
                         reads=[f"yacc{s}", f"mv{b}", f"rstd{b}"], writes=[f"yacc{s}"])
                    S.op("pool", lambda: nc.gpsimd.tensor_tensor(yacc[:, s, :], yacc[:, s, :], fg[:], ALU.mult), reads=[f"yacc{s}", "fing"], writes=[f"yacc{s}"])
                    S.op("dve", lambda: nc.vector.tensor_tensor(yacc[:, s, :], yacc[:, s, :], fb[:], ALU.add), reads=[f"yacc{s}", "finb"], writes=[f"yacc{s}"])
                S.dma("sp", u_out_d[t0 + s * 128:t0 + (s + 1) * 128, :], yacc[:, s, :], reads=[f"yacc{s}"])
        barrier(C)


MLA_SCALE = 96 ** -0.5


def mixer_sublayer(C, u_in_d, u_out_d, vec, W, Tn, name="mix", TQ=256):
    nc, S = C.nc, C.S
    V_, A_, G_, P_ = nc.vector, nc.scalar, nc.gpsimd, nc.tensor
    NT = Tn // TQ
    NSUB = TQ // 128
    NKT = Tn // 128

    def dve(fn, r=(), w=()): return S.op("dve", fn, r, w)
    def act(fn, r=(), w=()): return S.op("act", fn, r, w)
    def pool(fn, r=(), w=()): return S.op("pool", fn, r, w)
    def pe(fn, r=(), w=(), inc=True): return S.op("pe", fn, r, w, inc=inc)

    rb = {"i": 0}

    def nextbank():
        i = rb["i"] % 3
        rb["i"] += 1
        return C.bank[i], f"b{i}"

    fb = {"i": 0}

    def fbank():
        i = (0, 1)[fb["i"] % 2]
        fb["i"] += 1
        return C.bank[i], f"b{i}"

    bb = {"i": 0}

    def bbank():
        i = (2, 5)[bb["i"] % 2]
        bb["i"] += 1
        return C.bank[i], f"b{i}"

    with ExitStack() as es:
        def sb(nm, shape, dt):
            return es.enter_context(nc.sbuf_tensor(f"{name}_{nm}", list(shape), dt))

        win = sb("win", [128, 8, D_IN], BF16)
        w_in_v = W["w_in"].rearrange("(k p) e -> p k e", p=128)
        for k in range(8):
            S.dma("pool", win[:, k, :], w_in_v[:, k, :], writes=[f"win{k}"])
        WIN = [f"win{k}" for k in range(8)]
        wkA = sb("wkA", [128, 8, 96], BF16)
        wkB = sb("wkB", [128, 8, 96], BF16)
        pool(lambda: G_.memset(wkA[:], 0.0), w=["wkA"])
        pool(lambda: G_.memset(wkB[:], 0.0), w=["wkB"])
        pool(lambda: G_.tensor_copy(wkA[:, :, 64:96], win[:, :, 1672:1704]), r=WIN, w=["wkA"])
        pool(lambda: G_.tensor_copy(wkB[:, :, 64:80], win[:, :, 1688:1704]), r=WIN, w=["wkB"])
        pool(lambda: G_.tensor_copy(wkB[:, :, 80:96], win[:, :, 1672:1688]), r=WIN, w=["wkB"])
        wout = sb("wout", [128, 8, 1024], BF16)
        w_out_v = W["w_out"].rearrange("(k p) e -> p k e", p=128)
        for k in range(8):
            S.dma("pool", wout[:, k, :], w_out_v[:, k, :], writes=[f"wout{k}"])
            pool(lambda: G_.tensor_tensor(wout[:, k, :], wout[:, k, :], vec["Q3b"][:], ALU.mult), r=[f"wout{k}", "Q3b"], w=[f"wout{k}"])
        WOUT = [f"wout{k}" for k in range(8)]
        rest = sb("rest", [128, 1024], F32)
        tmpw = rest[:].rearrange("p (a b) -> p a b", a=2)
        qn = sb("qn", [128, 2], F32)
        kvn_g = sb("kvn_g", [128, 1], F32)
        with nc.allow_non_contiguous_dma(reason="tiny param vectors"):
            S.dma("sp", qn[:], W["q_norm"].rearrange("(b p) -> p b", p=128), writes=["qn"])
            S.dma("sp", kvn_g[:], W["kv_norm"].rearrange("(p o) -> p o", o=1), writes=["kvn_g"])
        S.dma("sp", tmpw[:, :, 0:384], W["w_qb"].rearrange("(b p) e -> p b e", p=128), writes=["rest"])
        wqb = sb("wqb", [128, 2, 384], BF16)
        wqs = sb("wqs", [128, 2, 384], BF16)
        for b in range(2):
            dve(lambda: V_.tensor_scalar(wqb[:, b, :], tmpw[:, b, 0:384], qn[:, b:b + 1], None, ALU.mult), r=["rest", "qn"], w=["wqb"])
        pool(lambda: G_.memset(wqs[:], 0.0), w=["wqs"])
        for hd in range(4):
            o = hd * 96
            pool(lambda: G_.tensor_copy(wqs[:, :, o + 64:o + 80], wqb[:, :, o + 80:o + 96]), r=["wqb"], w=["wqs"])
            pool(lambda: G_.tensor_copy(wqs[:, :, o + 80:o + 96], wqb[:, :, o + 64:o + 80]), r=["wqb"], w=["wqs"])
        S.dma("sp", tmpw[:, 0, :], W["w_kvb"], reads=["wqb"], writes=["rest"])
        wkv = sb("wkv", [128, 512], BF16)
        dve(lambda: V_.tensor_scalar(wkv[:], tmpw[:, 0, :], kvn_g[:, 0:1], None, ALU.mult), r=["rest", "kvn_g"], w=["wkv"])
        wkv4 = wkv[:].rearrange("p (h c) -> p h c", c=128)
        cwr = sb("cwr", [5, 768], F32)
        S.dma("sp", cwr[0:4, :], W["conv_w"], writes=["cwr"])
        S.dma("sp", cwr[4:5, :], W["conv_b"].rearrange("(o c) -> o c", o=1), writes=["cwr"])
        cw = sb("cw", [128, 6, 5], F32)
        bk, bt = nextbank()
        for blk in range(6):
            pe(lambda: P_.transpose(bk[:, blk * 5:blk * 5 + 5], cwr[0:5, blk * 128:(blk + 1) * 128], C.identf[0:5, 0:5]),
               r=["cwr", "identf"], w=[bt], inc=(blk == 5))
        dve(lambda: V_.tensor_copy(cw[:].rearrange("p a b -> p (a b)"), bk[:, 0:30]), r=[bt], w=["cw"])
        Abc = sb("Abc", [128, 8], F32)
        dtb = sb("dtb", [128, 8], F32)
        Dbc = sb("Dbc", [128, 8], F32)
        load_bcast(C, Abc, W["a_log"], "Abc")
        load_bcast(C, dtb, W["dt_bias"], "dtb")
        load_bcast(C, Dbc, W["ssd_d"], "Dbc")
        act(lambda: A_.activation(out=Abc[:], in_=Abc[:], func=AF.Exp), r=["Abc"], w=["Abc"])
        dve(lambda: V_.tensor_scalar(Abc[:], Abc[:], -1.0, None, ALU.mult), r=["Abc"], w=["Abc"])
        nw = sb("nw", [128, 512], F32)
        load_bcast(C, nw, W["ssd_norm_w"], "nw")
        gmg = sb("gmg", [128, 256], F32)
        gmb = sb("gmb", [128, 256], F32)
        load_bcast(C, gmg, W["gm_ln_g"], "gmg")
        load_bcast(C, gmb, W["gm_ln_b"], "gmb")
        bs = sb("bs", [128, 4], F32)
        with nc.allow_non_contiguous_dma(reason="tiny param vectors"):
            S.dma("sp", bs[:], W["gm_b_s"].rearrange("g t -> t g"), writes=["bs"])
        WT = sb("WT", [128, 4, 128], BF16)
        for g in range(4):
            S.dma("sp", tmpw[:, 1, 0:128], W["gm_w_s"][g], writes=["rest"])
            bk, bt = nextbank()
            pe(lambda: P_.transpose(bk[:, 0:128], tmpw[:, 1, 0:128], C.identf[:]), r=["rest", "identf"], w=[bt])
            dve(lambda: V_.tensor_tensor(WT[:, g, :], bk[:, 0:128], C.triu[:], ALU.mult), r=[bt, "cpack"], w=["WT"])

        KT = [sb(f"KT{hd}", [96, Tn], BF16) for hd in range(4)]
        Vt = sb("Vt", [128, NKT, 4, 65], BF16)
        pool(lambda: G_.memset(Vt[:, :, :, 64:65], 1.0), w=["Vones"])
        Srun = sb("Srun", [128, 4, 64], F32)
        Sbf = sb("Sbf", [128, 4, 64], BF16)
        pool(lambda: G_.memset(Srun[:], 0.0), w=["Srun"])
        pool(lambda: G_.memset(Sbf[:], 0.0), w=["Sbf"])
        halo = sb("halo", [128, 6, 3], F32)
        pool(lambda: G_.memset(halo[:], 0.0), w=["halo"])
        xct = [sb(f"xct{i}", [128, 3 + TQ], F32) for i in range(2)]

        _ut = sb("ut0", [128, 1024], F32)
        _hb = sb("hb0", [128, 1024], BF16)
        ph = {"ut": [_ut, _ut], "hb": [_hb, _hb], "hf": None, "single": True,
              "mv": [sb(f"mv{i}", [128, 2], F32) for i in range(2)],
              "rstd": [sb(f"rstd{i}", [128, 1], F32) for i in range(2)],
              "st": sb("st", [128, 2, 6], F32)}
        ph["xn"] = ph["ut"]
        hT = sb("hT", [128, 8, TQ], BF16)
        zsD = [sb(f"zs{i}", [128, NSUB, 512], BF16) for i in range(2)]
        ggD = [sb(f"gg{i}", [128, NSUB, 512], BF16) for i in range(2)]
        dtrD = [sb(f"dtr{i}", [128, NSUB, 8], F32) for i in range(2)]
        xaD = [sb(f"xa{i}", [128, 6, TQ], BF16) for i in range(2)]
        cacc = sb("cacc", [128, TQ], F32)
        qlT = sb("qlT", [128, 2, TQ], BF16)
        sqT = sb("sqT", [128, 2, TQ], BF16)
        rq = sb("rq", [128, TQ], F32)
        rkv = sb("rkv", [128, TQ], F32)
        kvnT = sb("kvnT", [128, TQ], BF16)
        QTD = [sb(f"QT{i}", [96, 4, TQ], BF16) for i in range(2)]
        ccs = sb("ccs", [96, 2, TQ], F32)
        qt1 = sb("qt1", [96, TQ], F32)
        qt2 = sb("qt2", [96, TQ], F32)
        NPT = 3
        pt = [sb(f"pt{i}", [128, TQ], BF16) for i in range(NPT)]
        rcp = sb("rcp", [128, NSUB], F32)
        ycat = [sb(f"ycat{i}", [128, 1024], BF16) for i in range(NSUB)]
        yT = sb("yT", [128, 8, 128], BF16)
        dif = sb("dif", [128, 8, 128], F32)
        axT = dif
        eA = sb("eA", [128, 8, 128], F32)
        Mt = sb("Mt", [128, 8, 128], BF16)
        CsT = sb("CsT", [128, 4, 128], BF16)
        xtok = sb("xtok", [128, 512], BF16)
        xdt = sb("xdt", [128, 512], BF16)
        xdw = sb("xdw", [128, 512], BF16)
        Btok = sb("Btok", [128, 128], BF16)
        dts = sb("dts", [128, 8], F32)
        t8a = sb("t8a", [128, 8], F32)
        t8b = sb("t8b", [128, 8], F32)
        av = sb("av", [128, 8], F32)
        acs = sb("acs", [128, 8], F32)
        dend = sb("dend", [128, 8], F32)
        eAl = sb("eAl", [128, 8], F32)
        ys = sb("ys", [128, 512], F32)
        gst = sb("gst", [128, 2, 6], F32)
        gmv = sb("gmv", [128, 2, 2], F32)
        grs = sb("grs", [128, 2], F32)
        vnb = sb("vnb", [128, 256], BF16)
        vnf = sb("vnf", [128, 256], F32)
        outt = rest
        ones_b = sb("ones_b", [128, 128], BF16)
        dve(lambda: V_.tensor_copy(ones_b[:], C.ones[:]), r=["cpack"], w=["ones_b"])
        print("mixer sbuf bytes remaining", nc.sbuf_bytes_remaining)

        def front(j):
            par = j % 2
            zs, gg, dtr, xa, QT = zsD[par], ggD[par], dtrD[par], xaD[par], QTD[par]
            T0 = j * TQ
            if S.can_yield(): yield
            S.dma("sp", ccs[:, 0, :], W["rope"][0, :, T0:T0 + TQ], writes=["cc"])
            if S.can_yield(): yield
            S.dma("sp", ccs[:, 1, :], W["rope"][1, :, T0:T0 + TQ], writes=["ss"])
            for s in range(NSUB):
                if S.can_yield(): yield
                b = ln_front(C, ph, u_in_d, T0 + s * 128, vec)
                xn = ph["xn"][b]
                if S.can_yield(): yield
                dve(lambda: V_.tensor_tensor(rest[:], xn[:], vec["Q1"][:], ALU.mult), r=[ph["tx"], "Q1"], w=["rest"])
                if S.can_yield(): yield
                pool(lambda: G_.tensor_tensor(rest[:], rest[:], vec["Q2"][:], ALU.add), r=["rest", "Q2"], w=["rest"])
                if S.can_yield(): yield
                S.dma("sp", u_out_d[T0 + s * 128:T0 + (s + 1) * 128, :], rest[:], reads=["rest"], writes=[f"uo{j}_{s}"])
                if S.can_yield(): yield
                transpose_to(C, ph["hb"][b], f"hb{b}", hT, "hT", s * 128)
            for s in range(NSUB):
                lh = lambda k: hT[:, k, s * 128:(s + 1) * 128]
                bk, bt = fbank()
                for k in range(8):
                    if S.can_yield(): yield
                    pe(lambda: P_.matmul(bk[:, :], lh(k), win[:, k, 1704:2216], start=(k == 0), stop=(k == 7)), r=["hT", WIN[k]], w=[bt], inc=(k == 7))
                if S.can_yield(): yield
                act(lambda: A_.activation(out=gg[:, s, :], in_=bk[:, :], func=AF.Gelu), r=[bt], w=[f"gg{par}_{s}"])
            for s in range(NSUB):
                lh = lambda k: hT[:, k, s * 128:(s + 1) * 128]
                bk, bt = fbank()
                for k in range(8):
                    if S.can_yield(): yield
                    pe(lambda: P_.matmul(bk[:, 0:8], lh(k), win[:, k, 1280:1288], start=(k == 0), stop=(k == 7)), r=["hT", WIN[k]], w=[bt], inc=(k == 7))
                if S.can_yield(): yield
                dve(lambda: V_.tensor_tensor(dtr[:, s, :], bk[:, 0:8], dtb[:], ALU.add), r=[bt, "dtb"], w=[f"dtr{par}_{s}"])
            for s in range(NSUB):
                lh = lambda k: hT[:, k, s * 128:(s + 1) * 128]
                bk, bt = fbank()
                for k in range(8):
                    if S.can_yield(): yield
                    pe(lambda: P_.matmul(bk[:, :], lh(k), win[:, k, 0:512], start=(k == 0), stop=(k == 7)), r=["hT", WIN[k]], w=[bt], inc=(k == 7))
                if S.can_yield(): yield
                for hz in range(512 // TQ):
                    zsl = slice(hz * TQ, (hz + 1) * TQ)
                    if S.can_yield(): yield
                    act(lambda: A_.activation(out=cacc[:], in_=bk[:, zsl], func=AF.Exp, scale=-1.0), r=[bt], w=["cacc"])
                    if S.can_yield(): yield
                    act(lambda: A_.activation(out=cacc[:], in_=cacc[:], func=AF.Ln, bias=C.ones[:, 0:1]), r=["cacc", "cpack"], w=["cacc"])
                    act(lambda: A_.activation(out=cacc[:], in_=cacc[:], func=AF.Exp, scale=-1.0), r=["cacc"], w=["cacc"])
                    dve(lambda: V_.tensor_tensor(zs[:, s, zsl], bk[:, zsl], cacc[:], ALU.mult), r=[bt, "cacc"], w=[f"zs{par}_{s}"])
            for blk in range(6):
                c0 = 512 + blk * 128
                bk, bt = fbank()
                for k in range(8):
                    if S.can_yield(): yield
                    pe(lambda: P_.matmul(bk[:, 0:TQ], win[:, k, c0:c0 + 128], hT[:, k, :], start=(k == 0), stop=(k == 7)), r=["hT", WIN[k]], w=[bt], inc=(k == 7))
                xb = xct[blk % 2]
                xbt = f"xct{blk % 2}"
                if S.can_yield(): yield
                act(lambda: A_.copy(xb[:, 3:3 + TQ], bk[:, 0:TQ]), r=[bt], w=[xbt])
                if S.can_yield(): yield
                pool(lambda: G_.tensor_copy(xb[:, 0:3], halo[:, blk, :]), r=["halo"], w=[xbt])
                if S.can_yield(): yield
                dve(lambda: V_.tensor_scalar(cacc[:], xb[:, 3:3 + TQ], cw[:, blk, 3:4], cw[:, blk, 4:5], ALU.mult, ALU.add),
                    r=[xbt, "cw"], w=["cacc"])
                for kk in range(3):
                    if S.can_yield(): yield
                    dve(lambda: V_.scalar_tensor_tensor(cacc[:], xb[:, kk:kk + TQ], cw[:, blk, kk:kk + 1], cacc[:], ALU.mult, ALU.add),
                        r=[xbt, "cw", "cacc"], w=["cacc"])
                if S.can_yield(): yield
                pool(lambda: G_.tensor_copy(halo[:, blk, :], xb[:, TQ:TQ + 3]), r=[xbt], w=["halo"])
                if S.can_yield(): yield
                act(lambda: A_.activation(out=xb[:, 0:TQ], in_=cacc[:], func=AF.Exp, scale=-1.0), r=["cacc", "halo"], w=[xbt])
                if S.can_yield(): yield
                act(lambda: A_.activation(out=xb[:, 0:TQ], in_=xb[:, 0:TQ], func=AF.Ln, bias=C.ones[:, 0:1]), r=[xbt, "cpack"], w=[xbt])
                act(lambda: A_.activation(out=xb[:, 0:TQ], in_=xb[:, 0:TQ], func=AF.Exp, scale=-1.0), r=[xbt], w=[xbt])
                dve(lambda: V_.tensor_tensor(xa[:, blk, :], cacc[:], xb[:, 0:TQ], ALU.mult), r=["cacc", xbt], w=[f"xa{par}_{blk}"])
            for b2 in range(2):
                c0 = 1288 + b2 * 128
                bk, bt = fbank()
                for k in range(8):
                    if S.can_yield(): yield
                    pe(lambda: P_.matmul(bk[:, 0:TQ], win[:, k, c0:c0 + 128], hT[:, k, :], start=(k == 0), stop=(k == 7)), r=["hT", WIN[k]], w=[bt], inc=(k == 7))
                if S.can_yield(): yield
                act(lambda: A_.copy(qlT[:, b2, :], bk[:, 0:TQ]), r=[bt], w=["qlT"])
                if S.can_yield(): yield
                act(lambda: A_.activation(out=sqT[:, b2, :], in_=bk[:, 0:TQ], func=AF.Square), r=[bt], w=["sqT"])
            bk, bt = fbank()
            for b2 in range(2):
                if S.can_yield(): yield
                pe(lambda: P_.matmul(bk[:, 0:TQ], ones_b[:], sqT[:, b2, :], start=(b2 == 0), stop=(b2 == 1)), r=["ones_b", "sqT"], w=[bt], inc=(b2 == 1))
            if S.can_yield(): yield
            act(lambda: A_.activation(out=rq[:], in_=bk[:, 0:TQ], func=AF.Ln, scale=1.0 / 256, bias=C.eps_rms[:, 0:1]), r=[bt, "eps_rms"], w=["rq"])
            if S.can_yield(): yield
            act(lambda: A_.activation(out=rq[:], in_=rq[:], func=AF.Exp, scale=-0.5), r=["rq"], w=["rq"])
            bk, bt = fbank()
            for k in range(8):
                if S.can_yield(): yield
                pe(lambda: P_.matmul(bk[:, 0:TQ], win[:, k, 1544:1672], hT[:, k, :], start=(k == 0), stop=(k == 7)), r=["hT", WIN[k]], w=[bt], inc=(k == 7))
            if S.can_yield(): yield
            act(lambda: A_.activation(out=sqT[:, 0, :], in_=bk[:, 0:TQ], func=AF.Square), r=[bt], w=["sqT"])
            bk2, bt2 = fbank()
            if S.can_yield(): yield
            pe(lambda: P_.matmul(bk2[:, 0:TQ], ones_b[:], sqT[:, 0, :], start=True, stop=True), r=["ones_b", "sqT"], w=[bt2])
            if S.can_yield(): yield
            act(lambda: A_.activation(out=rkv[:], in_=bk2[:, 0:TQ], func=AF.Ln, scale=1.0 / 128, bias=C.eps_rms[:, 0:1]), r=[bt2, "eps_rms"], w=["rkv"])
            if S.can_yield(): yield
            act(lambda: A_.activation(out=rkv[:], in_=rkv[:], func=AF.Exp, scale=-0.5), r=["rkv"], w=["rkv"])
            if S.can_yield(): yield
            dve(lambda: V_.tensor_tensor(kvnT[:], bk[:, 0:TQ], rkv[:], ALU.mult), r=[bt, "rkv"], w=["kvnT"])
            bk, bt = fbank()
            for k in range(8):
                if S.can_yield(): yield
                pe(lambda: P_.matmul(bk[0:96, 0:TQ], wkA[:, k, :], hT[:, k, :], start=(k == 0), stop=(k == 7)), r=["hT", "wkA"], w=[bt], inc=(k == 7))
            bk2, bt2 = fbank()
            for k in range(8):
                if S.can_yield(): yield
                pe(lambda: P_.matmul(bk2[0:96, 0:TQ], wkB[:, k, :], hT[:, k, :], start=(k == 0), stop=(k == 7)), r=["hT", "wkB"], w=[bt2], inc=(k == 7))
            if S.can_yield(): yield
            dve(lambda: V_.tensor_tensor(qt1[64:96, :], bk[64:96, 0:TQ], ccs[64:96, 0, :], ALU.mult), r=[bt, "cc"], w=["qt1"])
            if S.can_yield(): yield
            dve(lambda: V_.tensor_tensor(qt2[64:96, :], bk2[64:96, 0:TQ], ccs[64:96, 1, :], ALU.mult), r=[bt2, "ss"], w=["qt2"])
            for hd in range(4):
                if S.can_yield(): yield
                pool(lambda: G_.tensor_tensor(KT[hd][64:96, T0:T0 + TQ], qt1[64:96, :], qt2[64:96, :], ALU.add), r=["qt1", "qt2"], w=[f"KT{hd}_{j}"])
            for hd in range(4):
                bk, bt = fbank()
                if S.can_yield(): yield
                pe(lambda: P_.matmul(bk[0:64, 0:TQ], wkv[:, hd * 128:hd * 128 + 64], kvnT[:], start=True, stop=True), r=["wkv", "kvnT"], w=[bt])
                if S.can_yield(): yield
                act(lambda: A_.copy(KT[hd][0:64, T0:T0 + TQ], bk[0:64, 0:TQ]), r=[bt], w=[f"KT{hd}_{j}"])
            for s in range(NSUB):
                bk, bt = fbank()
                for hd in range(4):
                    if S.can_yield(): yield
                    pe(lambda: P_.matmul(bk[:, hd * 64:(hd + 1) * 64], kvnT[:, s * 128:(s + 1) * 128], wkv[:, hd * 128 + 64:hd * 128 + 128], start=True, stop=True),
                       r=["wkv", "kvnT"], w=[bt], inc=(hd == 3))
                if S.can_yield(): yield
                act(lambda: A_.copy(Vt[:, j * NSUB + s, :, 0:64], bk[:, 0:256].rearrange("p (h c) -> p h c", c=64)), r=[bt], w=[f"V{j * NSUB + s}"])
            for hd in range(4):
                bk, bt = fbank()
                for b2 in range(2):
                    if S.can_yield(): yield
                    pe(lambda: P_.matmul(bk[0:96, 0:TQ], wqb[:, b2, hd * 96:(hd + 1) * 96], qlT[:, b2, :], start=(b2 == 0), stop=(b2 == 1)), r=["wqb", "qlT"], w=[bt], inc=(b2 == 1))
                bk2, bt2 = fbank()
                for b2 in range(2):
                    if S.can_yield(): yield
                    pe(lambda: P_.matmul(bk2[0:96, 0:TQ], wqs[:, b2, hd * 96:(hd + 1) * 96], qlT[:, b2, :], start=(b2 == 0), stop=(b2 == 1)), r=["wqs", "qlT"], w=[bt2], inc=(b2 == 1))
                if S.can_yield(): yield
                dve(lambda: V_.tensor_tensor(qt1[:], bk[0:96, 0:TQ], ccs[:, 0, :], ALU.mult), r=[bt, "cc"], w=["qt1"])
                if S.can_yield(): yield
                dve(lambda: V_.tensor_tensor(qt2[:], bk2[0:96, 0:TQ], ccs[:, 1, :], ALU.mult), r=[bt2, "ss"], w=["qt2"])
                if S.can_yield(): yield
                pool(lambda: G_.tensor_tensor(qt1[:], qt1[:], qt2[:], ALU.add), r=["qt1", "qt2"], w=["qt1"])
                if S.can_yield(): yield
                dve(lambda: V_.tensor_tensor(QT[:, hd, :], qt1[:], rq[0:96, :], ALU.mult), r=["qt1", "rq"], w=[f"QT{par}_{hd}"])


        def back(j):
            par = j % 2
            zs, gg, dtr, xa, QT = zsD[par], ggD[par], dtrD[par], xaD[par], QTD[par]
            T0 = j * TQ
            nkt = NSUB * (j + 1)
            sv = {}
            def ssd1(s):
                ch = j * NSUB + s
                csl = slice(s * 128, (s + 1) * 128)
                if S.can_yield(): yield
                dve(lambda: V_.tensor_scalar(t8a[:], dtr[:, s, :], -1.0, None, ALU.mult), r=[f"dtr{par}_{s}"], w=["t8a"])
                if S.can_yield(): yield
                dve(lambda: V_.tensor_tensor(t8a[:], t8a[:], dtr[:, s, :], ALU.max), r=["t8a", f"dtr{par}_{s}"], w=["t8a"])
                if S.can_yield(): yield
                act(lambda: A_.activation(out=t8a[:], in_=t8a[:], func=AF.Exp, scale=-1.0), r=["t8a"], w=["t8a"])
                if S.can_yield(): yield
                act(lambda: A_.activation(out=t8a[:], in_=t8a[:], func=AF.Ln, bias=C.ones[:, 0:1]), r=["t8a", "cpack"], w=["t8a"])
                if S.can_yield(): yield
                dve(lambda: V_.tensor_scalar(t8b[:], dtr[:, s, :], 0.0, None, ALU.max), r=[f"dtr{par}_{s}"], w=["t8b"])
                if S.can_yield(): yield
                dve(lambda: V_.tensor_tensor(dts[:], t8a[:], t8b[:], ALU.add), r=["t8a", "t8b"], w=["dts"])
                if S.can_yield(): yield
                dve(lambda: V_.tensor_tensor(av[:], dts[:], Abc[:], ALU.mult), r=["dts", "Abc"], w=["av"])
                bk, bt = bbank()
                if S.can_yield(): yield
                pe(lambda: P_.matmul(bk[:, 0:8], C.triu[:], av[:], start=True, stop=True), r=["cpack", "av"], w=[bt], inc=False)
                if S.can_yield(): yield
                pe(lambda: P_.matmul(bk[:, 8:16], C.ones[:], av[:], start=True, stop=True), r=["cpack", "av"], w=[bt])
                if S.can_yield(): yield
                dve(lambda: V_.tensor_copy(acs[:], bk[:, 0:8]), r=[bt], w=["acs"])
                if S.can_yield(): yield
                dve(lambda: V_.tensor_tensor(dend[:], bk[:, 8:16], acs[:], ALU.subtract), r=[bt, "acs"], w=["dend"])
                if S.can_yield(): yield
                act(lambda: A_.activation(out=dend[:], in_=dend[:], func=AF.Exp), r=["dend"], w=["dend"])
                if S.can_yield(): yield
                act(lambda: A_.activation(out=eAl[:], in_=bk[:, 8:16], func=AF.Exp), r=[bt], w=["eAl"])
                if S.can_yield(): yield
                dve(lambda: V_.tensor_tensor(axT[:], C.triu[:].unsqueeze(1).broadcast_to([128, 8, 128]), av[:].unsqueeze(2).broadcast_to([128, 8, 128]), ALU.mult),
                    r=["cpack", "av"], w=["dif0", "dif1"])

            def ssd2(s):
                ch = j * NSUB + s
                csl = slice(s * 128, (s + 1) * 128)
                bkA, btA = bbank()
                bkB, btB = bbank()
                if S.can_yield(): yield
                pe(lambda: P_.matmul(bkA[:, :], C.ones[:], axT[:, 0:4, :].rearrange("p a b -> p (a b)"), start=True, stop=True), r=["cpack", "dif0"], w=[btA])
                if S.can_yield(): yield
                pe(lambda: P_.matmul(bkB[:, :], C.ones[:], axT[:, 4:8, :].rearrange("p a b -> p (a b)"), start=True, stop=True), r=["cpack", "dif1"], w=[btB])
                for hh, (bkx, btx) in enumerate(((bkA, btA), (bkB, btB))):
                    d2 = dif[:, hh * 4:hh * 4 + 4, :].rearrange("p a b -> p (a b)")
                    e2 = eA[:, hh * 4:hh * 4 + 4, :].rearrange("p a b -> p (a b)")
                    if S.can_yield(): yield
                    act(lambda: A_.activation(out=e2, in_=bkx[:, :], func=AF.Exp), r=[btx], w=[f"eA{hh}"])
                    hs = slice(hh * 4, hh * 4 + 4)
                    if S.can_yield(): yield
                    dve(lambda: V_.tensor_tensor(dif[:, hs, :], bkx[:, :].rearrange("p (a b) -> p a b", a=4), acs[:, hs].unsqueeze(2).broadcast_to([128, 4, 128]), ALU.subtract),
                        r=[btx, "acs"], w=[f"dif{hh}"])
                    if S.can_yield(): yield
                    pool(lambda: G_.tensor_tensor(dif[:, hs, :], dif[:, hs, :], C.mneg[:].unsqueeze(1).broadcast_to([128, 4, 128]), ALU.add),
                         r=[f"dif{hh}", "cpack"], w=[f"dif{hh}"])
                    if S.can_yield(): yield
                    act(lambda: A_.activation(out=d2, in_=d2, func=AF.Exp), r=[f"dif{hh}"], w=[f"dif{hh}"])
                cbk = [bbank(), bbank()]
                for g in range(2):
                    gs = slice(g * 64, (g + 1) * 64)
                    if S.can_yield(): yield
                    pe(lambda: P_.matmul(cbk[g][0][:, 0:128], xa[gs, 4, csl], xa[gs, 5, csl], start=True, stop=True), r=[f"xa{par}_4", f"xa{par}_5"], w=[cbk[g][1]])
                for g in range(2):
                    gs = slice(g * 64, (g + 1) * 64)
                    if S.can_yield(): yield
                    dve(lambda: V_.tensor_tensor(Mt[:, g * 4:(g + 1) * 4, :], cbk[g][0][:, 0:128].unsqueeze(1).broadcast_to([128, 4, 128]), dif[:, g * 4:(g + 1) * 4, :], ALU.mult),
                        r=[cbk[g][1], f"dif{g}"], w=[f"Mt{g}"])
                    if S.can_yield(): yield
                    dve(lambda: V_.tensor_tensor(CsT[gs, :, :], xa[gs, 5, csl].unsqueeze(1).broadcast_to([64, 4, 128]), eA[gs, g * 4:(g + 1) * 4, :], ALU.mult),
                        r=[f"xa{par}_5", f"eA{g}"], w=[f"CsT{g}"])
                S.hold = True
                for blk in range(4):
                    if S.can_yield(): yield
                    pe(lambda: P_.transpose(C.bankb[:, blk * 128:(blk + 1) * 128], xa[:, blk, csl], C.identb[:]), r=[f"xa{par}_{blk}", "identb"], w=["bankb"], inc=False)
                if S.can_yield(): yield
                pe(lambda: P_.transpose(C.bankb[:, 512:640], xa[:, 4, csl], C.identb[:]), r=[f"xa{par}_4", "identb"], w=["bankb"])
                if S.can_yield(): yield
                act(lambda: A_.copy(xtok[:], C.bankb[:, 0:512]), r=["bankb"], w=["xtok"])
                if S.can_yield(): yield
                act(lambda: A_.copy(Btok[:], C.bankb[:, 512:640]), r=["bankb"], w=["Btok"])
                S.hold = False
                v3 = lambda t_: t_[:].rearrange("p (h c) -> p h c", c=64)
                b3 = lambda t_: t_[:].unsqueeze(2).broadcast_to([128, 8, 64])
                if S.can_yield(): yield
                dve(lambda: V_.tensor_tensor(v3(xdt), v3(xtok), b3(dts), ALU.mult), r=["xtok", "dts"], w=["xdt"])
                if S.can_yield(): yield
                pool(lambda: G_.tensor_tensor(v3(xdw), v3(xdt), b3(dend), ALU.mult), r=["xdt", "dend"], w=["xdw"])
                if S.can_yield(): yield
                dve(lambda: V_.tensor_tensor(v3(ys), v3(xtok), b3(Dbc), ALU.mult), r=["xtok", "Dbc"], w=["ys"])

            def ssd3(s):
                ch = j * NSUB + s
                csl = slice(s * 128, (s + 1) * 128)
                bk, bt = bbank()
                for h in range(8):
                    g, r_ = h // 4, h % 4
                    gs = slice(g * 64, (g + 1) * 64)
                    if S.can_yield(): yield
                    pe(lambda: P_.matmul(bk[:, h * 64:(h + 1) * 64], Mt[:, h, :], xdt[:, h * 64:(h + 1) * 64], start=True, stop=(ch == 0)),
                       r=[f"Mt{g}", "xdt"], w=[bt], inc=False)
                    if ch > 0:
                        if S.can_yield(): yield
                        pe(lambda: P_.matmul(bk[:, h * 64:(h + 1) * 64], CsT[gs, r_, :], Sbf[gs, r_, :], start=False, stop=True),
                           r=[f"CsT{g}", "Sbf"], w=[bt], inc=False)
                bk2, bt2 = bbank()
                if S.can_yield(): yield
                pe(lambda: P_.matmul(bk2[:, :], Btok[:], xdw[:], start=True, stop=True), r=["Btok", "xdw"], w=[bt2])
                for h in range(8):
                    g, r_ = h // 4, h % 4
                    gs = slice(g * 64, (g + 1) * 64)
                    if S.can_yield(): yield
                    dve(lambda: V_.scalar_tensor_tensor(Srun[gs, r_, :], Srun[gs, r_, :], eAl[gs, h:h + 1], bk2[gs, h * 64:(h + 1) * 64], ALU.mult, ALU.add),
                        r=["Srun", "eAl", bt2], w=["Srun"])
                if S.can_yield(): yield
                act(lambda: A_.copy(Sbf[:], Srun[:]), r=["Srun"], w=["Sbf"])
                if S.can_yield(): yield
                dve(lambda: V_.tensor_tensor(ys[:], bk[:, :], ys[:], ALU.add), r=[bt, "ys"], w=["ys"])
                if S.can_yield(): yield
                pool(lambda: G_.tensor_tensor(ys[:], ys[:], zs[:, s, :], ALU.mult), r=["ys", f"zs{par}_{s}"], w=["ys"])
                for g in range(2):
                    if S.can_yield(): yield
                    dve(lambda: V_.bn_stats(gst[:, g, :], ys[:, g * 256:(g + 1) * 256]), r=["ys"], w=[f"gst{g}"])
                    if S.can_yield(): yield
                    dve(lambda: V_.bn_aggr(gmv[:, g, :], gst[:, g, :]), r=[f"gst{g}"], w=[f"gmv{g}"])
                    if S.can_yield(): yield
                    dve(lambda: V_.tensor_tensor(grs[:, g:g + 1], gmv[:, g, 0:1], gmv[:, g, 0:1], ALU.mult), r=[f"gmv{g}"], w=["grs"])
                    if S.can_yield(): yield
                    dve(lambda: V_.tensor_tensor(grs[:, g:g + 1], grs[:, g:g + 1], gmv[:, g, 1:2], ALU.add), r=["grs", f"gmv{g}"], w=["grs"])
                if S.can_yield(): yield
                act(lambda: A_.activation(out=grs[:], in_=grs[:], func=AF.Ln, bias=C.eps_rms[:, 0:1]), r=["grs", "eps_rms"], w=["grs"])
                if S.can_yield(): yield
                act(lambda: A_.activation(out=grs[:], in_=grs[:], func=AF.Exp, scale=-0.5), r=["grs"], w=["grs"])
                for g in range(2):
                    if S.can_yield(): yield
                    dve(lambda: V_.scalar_tensor_tensor(ycat[s][:, g * 256:(g + 1) * 256], ys[:, g * 256:(g + 1) * 256], grs[:, g:g + 1], nw[:, g * 256:(g + 1) * 256],
                                                        ALU.mult, ALU.mult), r=["ys", "grs", "nw"], w=[f"ycat{s}"])

            def gmlp(s):
                if S.can_yield(): yield
                dve(lambda: V_.bn_stats(gst[:, 0, :], gg[:, s, 256:512]), r=[f"gg{par}_{s}"], w=["gst0"])
                if S.can_yield(): yield
                dve(lambda: V_.bn_aggr(gmv[:, 0, :], gst[:, 0, :]), r=["gst0"], w=["gmv0"])
                if S.can_yield(): yield
                act(lambda: A_.activation(out=grs[:, 0:1], in_=gmv[:, 0, 1:2], func=AF.Ln, bias=C.eps_ln[:, 0:1]), r=["gmv0", "eps_ln"], w=["grs"])
                if S.can_yield(): yield
                act(lambda: A_.activation(out=grs[:, 0:1], in_=grs[:, 0:1], func=AF.Exp, scale=-0.5), r=["grs"], w=["grs"])
                if S.can_yield(): yield
                dve(lambda: V_.tensor_scalar(vnf[:], gg[:, s, 256:512], gmv[:, 0, 0:1], grs[:, 0:1], ALU.subtract, ALU.mult), r=[f"gg{par}_{s}", "gmv0", "grs"], w=["vnf"])
                if S.can_yield(): yield
                pool(lambda: G_.tensor_tensor(vnf[:], vnf[:], gmg[:], ALU.mult), r=["vnf", "gmg"], w=["vnf"])
                if S.can_yield(): yield
                pool(lambda: G_.tensor_tensor(vnb[:], vnf[:], gmb[:], ALU.add), r=["vnf", "gmb"], w=["vnb"])
                bk, bt = bbank()
                for g in range(4):
                    if S.can_yield(): yield
                    pe(lambda: P_.matmul(bk[:, g * 64:(g + 1) * 64], WT[:, g, :], vnb[:, g * 64:(g + 1) * 64], start=True, stop=True), r=["WT", "vnb"], w=[bt], inc=(g == 3))
                for g in range(4):
                    if S.can_yield(): yield
                    dve(lambda: V_.scalar_tensor_tensor(ycat[s][:, 768 + g * 64:768 + (g + 1) * 64], bk[:, g * 64:(g + 1) * 64], bs[:, g:g + 1], gg[:, s, g * 64:(g + 1) * 64],
                                                        ALU.add, ALU.mult), r=[bt, "bs", f"gg{par}_{s}"], w=[f"ycat{s}"])


            def attn(hd):
                ob, obt = C.bank[6], "b6"
                o3 = ob[:, 0:NSUB * 65].rearrange("p (q c) -> p q c", c=65)

                def a_up(kt):
                    r_ = max(0, kt - NSUB * j)
                    q0 = r_ * 128
                    sbk, sbt = (C.bank[3], "b3") if (kt % 2 == 0) else (C.bank[4], "b4")
                    if S.can_yield(): yield
                    pe(lambda: P_.matmul(sbk[:, q0:TQ], KT[hd][0:96, kt * 128:(kt + 1) * 128], QT[:, hd, q0:TQ], start=True, stop=True),
                       r=[f"KT{hd}_{kt // NSUB}", f"QT{par}_{hd}"], w=[sbt])
                    pb = pt[(hd * nkt + kt) % NPT]
                    pbt = f"pt{(hd * nkt + kt) % NPT}"
                    if S.can_yield(): yield
                    act(lambda: A_.activation(out=pb[:, q0:TQ], in_=sbk[:, q0:TQ], func=AF.Exp, scale=MLA_SCALE), r=[sbt], w=[pbt])
                    if kt >= NSUB * j:
                        if S.can_yield(): yield
                        dve(lambda: V_.tensor_tensor(pb[:, q0:q0 + 128], pb[:, q0:q0 + 128], C.triub[:], ALU.mult), r=[pbt, "triub"], w=[pbt])

                def a_down(kt):
                    r_ = max(0, kt - NSUB * j)
                    pb = pt[(hd * nkt + kt) % NPT]
                    pbt = f"pt{(hd * nkt + kt) % NPT}"
                    for qs in range(r_, NSUB):
                        last = (kt == NSUB * j + qs)
                        if S.can_yield(): yield
                        pe(lambda: P_.matmul(o3[:, qs, :], pb[:, qs * 128:(qs + 1) * 128], Vt[:, kt, hd, :], start=(kt == 0 and qs == 0), stop=last, skip_group_check=True),
                           r=[pbt, f"V{kt}", "Vones"], w=[obt], inc=(qs == NSUB - 1))

                for idx in range(nkt + 1):
                    if idx < nkt:
                        yield from a_up(idx)
                    if idx >= 1:
                        yield from a_down(idx - 1)
                if S.can_yield(): yield
                dve(lambda: V_.reciprocal(rcp[:], o3[:, :, 64]), r=[obt], w=["rcp"])
                for qs in range(NSUB):
                    if S.can_yield(): yield
                    dve(lambda: V_.tensor_scalar(ycat[qs][:, 512 + hd * 64:512 + (hd + 1) * 64], o3[:, qs, 0:64], rcp[:, qs:qs + 1], None, ALU.mult),
                        r=[obt, "rcp"], w=[f"ycat{qs}"])


            for s in range(NSUB):
                yield from ssd1(s)
                yield from gmlp(s)
                yield from attn(2 * s)
                yield from ssd2(s)
                yield from attn(2 * s + 1)
                yield from ssd3(s)
            for s in range(NSUB):
                S.hold = True
                for k in range(8):
                    pe(lambda: P_.transpose(C.bankb[:, k * 128:(k + 1) * 128], ycat[s][:, k * 128:(k + 1) * 128], C.identb[:]), r=[f"ycat{s}", "identb"], w=["bankb"], inc=(k == 7))
                if S.can_yield(): yield
                act(lambda: A_.copy(yT[:].rearrange("p k t -> p (k t)"), C.bankb[:]), r=["bankb"], w=["yT"])
                S.hold = False
                if S.can_yield(): yield
                S.dma("sp", outt[:], u_out_d[T0 + s * 128:T0 + (s + 1) * 128, :], reads=[f"uo{j}_{s}"], writes=["rest"])
                for hlf in range(2):
                    bk, bt = bbank()
                    for k in range(8):
                        if S.can_yield(): yield
                        pe(lambda: P_.matmul(bk[:, :], yT[:, k, :], wout[:, k, hlf * 512:(hlf + 1) * 512], start=(k == 0), stop=(k == 7)), r=["yT", WOUT[k]], w=[bt], inc=(k == 7))
                    if S.can_yield(): yield
                    dve(lambda: V_.tensor_tensor(outt[:, hlf * 512:(hlf + 1) * 512], bk[:, :], outt[:, hlf * 512:(hlf + 1) * 512], ALU.add), r=[bt, "rest"], w=["rest"])
                if S.can_yield(): yield
                S.dma("sp", u_out_d[T0 + s * 128:T0 + (s + 1) * 128, :], outt[:], reads=["rest"], writes=[f"uo{j}_{s}"])

        def merge(ga, gb, ra=1, rb=3):
            live = [ga is not None, gb is not None]
            while live[0] or live[1]:
                for gi, (g_, n_) in enumerate(((ga, ra), (gb, rb))):
                    for _ in range(n_):
                        if live[gi]:
                            try:
                                next(g_)
                            except StopIteration:
                                live[gi] = False

        for j in range(NT + 1):
            merge(front(j) if j < NT else None, back(j - 1) if j >= 1 else None)
        barrier(C)


def modulation_vectors(C, c_d, ada_w_d, ada_b_d, gin_d, bin_d, vec, name):
    nc, S = C.nc, C.S
    V_, A_, G_, P_ = nc.vector, nc.scalar, nc.gpsimd, nc.tensor
    with ExitStack() as es:
        def sb(nm, shape, dt):
            return es.enter_context(nc.sbuf_tensor(f"{name}_{nm}", list(shape), dt))
        sc = sb("sc", [128, 8], F32)
        scb = sb("scb", [128, 8, 128], F32)
        wblk = [sb(f"wblk{i}", [128, 8, 512], F32) for i in range(2)]
        mrow = sb("mrow", [128, 3072], F32)
        brow = sb("brow", [128, 3072], F32)
        gt = sb("gt", [128, 1024], F32)
        bt_ = sb("bt", [128, 1024], F32)
        with nc.allow_non_contiguous_dma(reason="tiny conditioning vector"):
            S.dma("sp", sc[:], c_d.rearrange("(k p) -> p k", p=128), writes=["sc"])
        S.op("act", lambda: A_.activation(out=sc[:], in_=sc[:], func=AF.Silu), reads=["sc"], writes=["sc"])
        for k in range(8):
            S.op("dve", lambda: V_.tensor_scalar(scb[:, k, :], C.ones[:], sc[:, k:k + 1], None, ALU.mult), reads=["sc", "cpack"], writes=["scb"])
        S.dma("sp", brow[:], ada_b_d.partition_broadcast(128), writes=["brow"])
        S.dma("sp", gt[:], gin_d.partition_broadcast(128), writes=["gt"])
        S.dma("sp", bt_[:], bin_d.partition_broadcast(128), writes=["bt"])
        wv = ada_w_d.rearrange("(k p) e -> p k e", p=128)
        for cb in range(6):
            wb = wblk[cb % 2]
            S.dma("sp", wb[:], wv[:, :, cb * 512:(cb + 1) * 512], writes=[f"wblk{cb % 2}"])
            bk, bt = C.bank[cb % 2], f"b{cb % 2}"
            for k in range(8):
                S.op("pe", lambda: P_.matmul(bk[:, :], scb[:, k, :], wb[:, k, :], start=(k == 0), stop=(k == 7)),
                     reads=["scb", f"wblk{cb % 2}"], writes=[bt], inc=(k == 7))
            S.op("dve", lambda: V_.tensor_tensor(mrow[:, cb * 512:(cb + 1) * 512], bk[:, :], brow[:, cb * 512:(cb + 1) * 512], ALU.add),
                 reads=[bt, "brow"], writes=["mrow"])
        shift, scale, gate = mrow[:, 0:1024], mrow[:, 1024:2048], mrow[:, 2048:3072]
        S.op("dve", lambda: V_.tensor_scalar(scale, scale, 1.0, None, ALU.add), reads=["mrow"], writes=["mrow"])
        S.op("dve", lambda: V_.tensor_scalar(gate, gate, 1.0, None, ALU.add), reads=["mrow"], writes=["mrow"])
        S.op("dve", lambda: V_.tensor_tensor(vec["P1"][:], gt[:], scale, ALU.mult), reads=["gt", "mrow"], writes=["P1"])
        S.op("dve", lambda: V_.tensor_tensor(vec["P2"][:], bt_[:], scale, ALU.mult), reads=["bt", "mrow"], writes=["P2"])
        S.op("dve", lambda: V_.tensor_tensor(vec["P2"][:], vec["P2"][:], shift, ALU.add), reads=["P2", "mrow"], writes=["P2"])
        S.op("dve", lambda: V_.tensor_scalar(vec["Q1"][:], gt[:], DN_ALPHA, None, ALU.mult), reads=["gt"], writes=["Q1"])
        S.op("dve", lambda: V_.tensor_scalar(vec["Q2"][:], bt_[:], DN_ALPHA, None, ALU.mult), reads=["bt"], writes=["Q2"])
        S.op("dve", lambda: V_.tensor_copy(vec["Q3b"][:], gate), reads=["mrow"], writes=["Q3b"])
        barrier(C)


def final_ln(C, u_d, out_d, g_d, b_d, Tn, name="fin"):
    nc, S = C.nc, C.S
    with ExitStack() as es:
        def sb(nm, shape, dt):
            return es.enter_context(nc.sbuf_tensor(f"{name}_{nm}", list(shape), dt))
        gt = sb("gt", [128, 1024], F32)
        bt_ = sb("bt", [128, 1024], F32)
        S.dma("sp", gt[:], g_d.partition_broadcast(128), writes=["fgt"])
        S.dma("sp", bt_[:], b_d.partition_broadcast(128), writes=["fbt"])
        ut = [sb(f"ut{i}", [128, 1024], F32) for i in range(3)]
        mv = [sb(f"mv{i}", [128, 2], F32) for i in range(3)]
        rstd = [sb(f"rstd{i}", [128, 1], F32) for i in range(3)]
        st = sb("st", [128, 2, 6], F32)
        for s in range(Tn // 128):
            b = s % 3
            S.dma("sp", ut[b][:], u_d[s * 128:(s + 1) * 128, :], writes=[f"fut{b}"])
            ln_stats(C, ut[b][:], f"fut{b}", mv[b][:], f"fmv{b}", rstd[b][:], f"frstd{b}", st)
            S.op("dve", lambda: nc.vector.tensor_scalar(ut[b][:], ut[b][:], mv[b][:, 0:1], rstd[b][:, 0:1], ALU.subtract, ALU.mult),
                 reads=[f"fut{b}", f"fmv{b}", f"frstd{b}"], writes=[f"fut{b}"])
            S.op("pool", lambda: nc.gpsimd.tensor_tensor(ut[b][:], ut[b][:], gt[:], ALU.mult), reads=[f"fut{b}", "fgt"], writes=[f"fut{b}"])
            S.op("dve", lambda: nc.vector.tensor_tensor(ut[b][:], ut[b][:], bt_[:], ALU.add), reads=[f"fut{b}", "fbt"], writes=[f"fut{b}"])
            S.dma("sp", out_d[s * 128:(s + 1) * 128, :], ut[b][:], reads=[f"fut{b}"])
        barrier(C)


W_SHAPES = {
    "ln0_g": [D], "ln0_b": [D], "ada_w": [2, 2, D, 3 * D], "ada_b": [2, 2, 3 * D], "post_ln_g": [2, 2, D], "post_ln_b": [2, 2, D],
    "w_in": [2, D, D_IN], "ssd_conv_w": [2, 4, 768], "ssd_conv_b": [2, 768], "ssd_dt_bias": [2, 8], "ssd_a_log": [2, 8], "ssd_d": [2, 8],
    "ssd_norm_w": [2, 512], "mla_q_norm": [2, 256], "mla_w_qb": [2, 256, 384], "mla_kv_norm": [2, 128], "mla_w_kvb": [2, 128, 512],
    "gm_ln_g": [2, 256], "gm_ln_b": [2, 256], "gm_w_s": [2, 4, 128, 128], "gm_b_s": [2, 4, 128], "w_out": [2, D, D],
    "ffn_w1": [1, D, DFF], "ffn_w3": [1, D, DFF], "ffn_w2": [1, DFF, D], "moe_router": [1, D, NE],
    "moe_w1": [1, NE, D, DFF], "moe_w3": [1, NE, D, DFF], "moe_w2": [1, NE, DFF, D],
}


def host_consts(Tn):
    i = np.arange(128)
    triu = (i[:, None] <= i[None, :]).astype(np.float32)
    mneg = np.where(i[None, :] >= i[:, None], 0.0, -30000.0).astype(np.float32)
    cpack = np.concatenate([triu, mneg, np.ones((128, 128), np.float32)], axis=1)
    inv = (10000.0 ** (-np.arange(0, 32, 2, dtype=np.float32) / 32)).astype(np.float32)
    ang = (np.arange(Tn, dtype=np.float32)[:, None] * inv[None, :]).astype(np.float32)
    cos, sin = np.cos(ang).T.astype(np.float32), np.sin(ang).T.astype(np.float32)
    rope = np.zeros((2, 96, Tn), np.float32)
    rope[0, 0:64] = 1.0
    rope[0, 64:80] = cos
    rope[0, 80:96] = cos
    rope[1, 64:80] = -sin
    rope[1, 80:96] = sin
    return {"ident": np.eye(128, dtype=np.float32), "cpack": cpack, "rope": rope}


def build_program(Tn=T, n_sub=4):
    nc = bass.Bass("TRN2", target_bir_lowering=False)
    C = Ctx(nc)
    S = C.S
    x_d = nc.dram_tensor("x", [Tn, D], F32, kind="ExternalInput").ap()
    c_d = nc.dram_tensor("c", [D], F32, kind="ExternalInput").ap()
    Wd = {n: nc.dram_tensor(n, sh, F32, kind="ExternalInput").ap() for n, sh in W_SHAPES.items()}
    id_d = nc.dram_tensor("ident", [128, 128], F32, kind="ExternalInput").ap()
    cp_d = nc.dram_tensor("cpack", [128, 384], F32, kind="ExternalInput").ap()
    rope_d = nc.dram_tensor("rope", [2, 96, Tn], F32, kind="ExternalInput").ap()
    out_d = nc.dram_tensor("out", [Tn, D], F32, kind="ExternalOutput").ap()
    scr = [nc.dram_tensor(f"uscr{i}", [Tn, D], F32).ap() for i in range(2)]
    setup_consts(C, id_d, cp_d)
    vec = {n: C.sb("v" + n, [128, 1024], F32) for n in ["P1", "P2", "Q1", "Q2"]}
    vec["Q3b"] = C.sb("vQ3b", [128, 1024], BF16)
    u_in = x_d
    for i in range(n_sub):
        l, sub = i // 2, i % 2
        if i == 0:
            gin, bin_ = Wd["ln0_g"], Wd["ln0_b"]
        else:
            pl, ps = (i - 1) // 2, (i - 1) % 2
            gin, bin_ = Wd["post_ln_g"][pl, ps], Wd["post_ln_b"][pl, ps]
        modulation_vectors(C, c_d, Wd["ada_w"][l, sub], Wd["ada_b"][l, sub], gin, bin_, vec, f"mod{i}")
        u_out = scr[i % 2]
        last = (i == n_sub - 1 and sub == 1)
        fin = None
        if last:
            fin = (C.sb("fing", [128, 1024], F32), C.sb("finb", [128, 1024], F32))
            load_bcast(C, fin[0], Wd["post_ln_g"][l, sub], "fing")
            load_bcast(C, fin[1], Wd["post_ln_b"][l, sub], "finb")
            u_out = out_d
        if sub == 0:
            W = {"w_in": Wd["w_in"][l], "conv_w": Wd["ssd_conv_w"][l], "conv_b": Wd["ssd_conv_b"][l], "dt_bias": Wd["ssd_dt_bias"][l],
                 "a_log": Wd["ssd_a_log"][l], "ssd_d": Wd["ssd_d"][l], "ssd_norm_w": Wd["ssd_norm_w"][l], "q_norm": Wd["mla_q_norm"][l],
                 "w_qb": Wd["mla_w_qb"][l], "kv_norm": Wd["mla_kv_norm"][l], "w_kvb": Wd["mla_w_kvb"][l], "gm_ln_g": Wd["gm_ln_g"][l],
                 "gm_ln_b": Wd["gm_ln_b"][l], "gm_w_s": Wd["gm_w_s"][l], "gm_b_s": Wd["gm_b_s"][l], "w_out": Wd["w_out"][l], "rope": rope_d}
            mixer_sublayer(C, u_in, u_out, vec, W, Tn, name=f"mix{l}")
        elif l % 2 == 0:
            ffn_sublayer(C, u_in, u_out, vec, [(Wd["ffn_w1"][l // 2], Wd["ffn_w3"][l // 2], Wd["ffn_w2"][l // 2])], Tn, name=f"ffn{l}", final=fin)
        else:
            m = l // 2
            experts = [(Wd["moe_w1"][m, e], Wd["moe_w3"][m, e], Wd["moe_w2"][m, e]) for e in range(NE)]
            ffn_sublayer(C, u_in, u_out, vec, experts, Tn, router_d=Wd["moe_router"][m], name=f"moe{l}", final=fin)
        u_in = u_out
    pl, ps = (n_sub - 1) // 2, (n_sub - 1) % 2
    if (n_sub - 1) % 2 == 0:
        final_ln(C, u_in, out_d, Wd["post_ln_g"][pl, ps], Wd["post_ln_b"][pl, ps], Tn)
    S.finish()
    return nc, C


_CACHE = {}


def kernel(**inputs):
    x = np.asarray(inputs["x"], dtype=np.float32)
    B, Tn, _ = x.shape
    if Tn not in _CACHE:
        _CACHE[Tn] = build_program(Tn)
    nc, _ = _CACHE[Tn]
    consts = host_consts(Tn)
    shared = {n: np.ascontiguousarray(np.asarray(inputs[n], dtype=np.float32)) for n in W_SHAPES}
    shared.update(consts)
    c = np.asarray(inputs["c"], dtype=np.float32)
    in_maps = []
    for b in range(B):
        m = dict(shared)
        m["x"] = np.ascontiguousarray(x[b])
        m["c"] = np.ascontiguousarray(c[b])
        in_maps.append(m)
    res = run_bass_kernel_spmd(nc, in_maps, core_ids=list(range(B)))
    return np.stack([np.asarray(r["out"], dtype=np.float32) for r in res.results], axis=0)
```
